# Optimizing a Trainium2 kernel written in Bass

```python
import jax, jax.numpy as jnp
from jax import lax
import numpy as np

D_MODEL = 1024
BATCH = 8
SEQ = 4096
DEPTH = 4

GRID_W = 64
CTX_LEN = 256
MIX_WIDTH = D_MODEL
RET_WIDTH = D_MODEL // 2
POOL_WIDTH = MIX_WIDTH - RET_WIDTH
RET_HEADS = 4
RET_HEAD_DIM = RET_WIDTH // RET_HEADS
RET_CHUNK = 128
ROPE_BASE = 10000.0
POOL_WINDOWS = (2, 4, 8, 16)
POOL_GROUPS = len(POOL_WINDOWS)
POOL_CH = POOL_WIDTH // POOL_GROUPS
IN_COLS = 4 * RET_WIDTH + POOL_WIDTH
D_FF = 2816
N_EXPERTS = 8
MOE_TOP_K = 2
MOE_D_FF = 2816
NORM_EPS = 1e-6
GN_EPS = 1e-5

kernel_name = 'hybrid_retention_pool_moe_dit'

F32 = jnp.float32


def rmsnorm(x, gain):
    xf = x.astype(F32)
    y = xf * lax.rsqrt(jnp.mean(xf * xf, axis=-1, keepdims=True) + NORM_EPS)
    return (y * gain.astype(F32)).astype(x.dtype)


def modulate(h, shift, scale):
    return h * (1 + scale) + shift


def rope_2d(t, rows, cols):
    half = t.shape[-1] // 2
    inv_freq = ROPE_BASE ** (-jnp.arange(0, half, 2, dtype=F32) / half)

    def rotate(u, pos):
        ang = pos.astype(F32)[:, None] * inv_freq[None, :]
        cos = jnp.cos(ang)[None, :, None, :]
        sin = jnp.sin(ang)[None, :, None, :]
        u1, u2 = jnp.split(u, 2, axis=-1)
        return jnp.concatenate([u1 * cos - u2 * sin, u1 * sin + u2 * cos], axis=-1)

    return jnp.concatenate([rotate(t[..., :half], rows), rotate(t[..., half:], cols)], axis=-1)


def retention_chunked(q, k, v, log_gamma, s0, include_diag):
    b, n, h, dk = q.shape
    dv = v.shape[-1]
    nc = n // RET_CHUNK
    q = q.reshape(b, nc, RET_CHUNK, h, dk)
    k = k.reshape(b, nc, RET_CHUNK, h, dk)
    v = v.reshape(b, nc, RET_CHUNK, h, dv)
    pos = jnp.arange(RET_CHUNK, dtype=F32)
    diff = pos[:, None] - pos[None, :]
    past_mask = diff >= 0 if include_diag else diff > 0
    decay = jnp.where(past_mask[None], jnp.exp(log_gamma[:, None, None] * jnp.maximum(diff, 0.0)[None]), 0.0)
    scores = jnp.einsum('bcihd,bcjhd->bchij', q, k) * decay[None, None]
    intra = jnp.einsum('bchij,bcjhe->bcihe', scores, v)
    k_tail = k * jnp.exp((RET_CHUNK - 1 - pos)[:, None] * log_gamma[None, :])[:, :, None]
    kv = jnp.einsum('bcjhd,bcjhe->cbhde', k_tail, v)
    gamma_chunk = jnp.exp(log_gamma * RET_CHUNK)[None, :, None, None]

    def step(state, kv_c):
        return gamma_chunk * state + kv_c, state

    s_final, s_prev = lax.scan(step, s0, kv)
    q_head = q * jnp.exp((pos + 1.0)[:, None] * log_gamma[None, :])[:, :, None]
    inter = jnp.einsum('bcihd,cbhde->bcihe', q_head, s_prev)
    return (intra + inter).reshape(b, n, h, dv), s_final


def retention_final_state(k, v, log_gamma, reverse):
    n = k.shape[1]
    pos = jnp.arange(n, dtype=F32)
    dist = pos if reverse else (n - 1 - pos)
    wgt = jnp.exp(dist[:, None] * log_gamma[None, :])
    return jnp.einsum('bnhd,bnhe->bhde', k * wgt[None, :, :, None], v)


def retention_readout(o, g, gain):
    mu = jnp.mean(o, axis=-1, keepdims=True)
    var = jnp.mean(jnp.square(o - mu), axis=-1, keepdims=True)
    on = ((o - mu) * lax.rsqrt(var + GN_EPS)).reshape(o.shape[0], o.shape[1], RET_WIDTH)
    return (on * gain.astype(F32) * jax.nn.silu(g.astype(F32))).astype(g.dtype)


def centred_mean(u, w, axis):
    n = u.shape[axis]
    cs = jnp.cumsum(u.astype(F32), axis=axis)
    pad = [(0, 0)] * u.ndim
    pad[axis] = (1, 0)
    cs = jnp.pad(cs, pad)
    i = jnp.arange(n)
    lo = jnp.clip(i - w // 2, 0, n)
    hi = jnp.clip(i - w // 2 + w, 0, n)
    total = jnp.take(cs, hi, axis=axis) - jnp.take(cs, lo, axis=axis)
    shape = [1] * u.ndim
    shape[axis] = n
    return (total / (hi - lo).astype(F32).reshape(shape)).astype(u.dtype)


def pool_grid_diff(p, rows_n):
    b, l, _ = p.shape
    pg = p.reshape(b, rows_n, GRID_W, POOL_GROUPS, POOL_CH)
    outs = [centred_mean(centred_mean(pg[..., gi, :], w, 1), w, 2) - pg[..., gi, :]
            for gi, w in enumerate(POOL_WINDOWS)]
    return jnp.stack(outs, axis=3).reshape(b, l, POOL_GROUPS, POOL_CH)


def pool_seq_diff(p):
    b, n, _ = p.shape
    pg = p.reshape(b, n, POOL_GROUPS, POOL_CH)
    outs = [centred_mean(pg[:, :, gi, :], w, 1) - pg[:, :, gi, :] for gi, w in enumerate(POOL_WINDOWS)]
    return jnp.stack(outs, axis=2)


def pool_project(d, pool_w, pool_scale):
    b, n = d.shape[0], d.shape[1]
    return jnp.einsum('bngc,gcd->bngd', d, pool_w).reshape(b, n, POOL_WIDTH) * pool_scale


def token_mix(h, hc, w_in, log_gamma, gn_g, pool_w, pool_scale, w_out, with_ctx_out):
    b, l, _ = h.shape
    rows_n = l // GRID_W
    t = jnp.arange(l)
    rows, cols = t // GRID_W, t % GRID_W
    splits = [RET_WIDTH, 2 * RET_WIDTH, 3 * RET_WIDTH, 4 * RET_WIDTH]
    scale = RET_HEAD_DIM ** -0.5

    def heads(u):
        return u.reshape(u.shape[0], u.shape[1], RET_HEADS, RET_HEAD_DIM).astype(F32)

    lg_f, lg_b = log_gamma[0], log_gamma[1]
    zeros = jnp.zeros((b, RET_HEADS, RET_HEAD_DIM, RET_HEAD_DIM), F32)

    if with_ctx_out:
        qc, kc, vc, gc, pc = jnp.split(hc @ w_in, splits, axis=-1)
        qc, kc, vc = heads(qc), heads(kc) * scale, heads(vc)
        oc_f, s_f = retention_chunked(qc, kc, vc, lg_f, zeros, True)
        oc_b, s_b = retention_chunked(qc[:, ::-1], kc[:, ::-1], vc[:, ::-1], lg_b, zeros, False)
        ret_c = retention_readout(oc_f + oc_b[:, ::-1], gc, gn_g)
        pool_c = pool_project(pool_seq_diff(pc), pool_w, pool_scale)
        y_ctx = (jnp.concatenate([ret_c, pool_c.astype(ret_c.dtype)], axis=-1) @ w_out).astype(hc.dtype)
    else:
        kc, vc = jnp.split(hc @ w_in[:, RET_WIDTH:3 * RET_WIDTH], 2, axis=-1)
        kc, vc = heads(kc) * scale, heads(vc)
        s_f = retention_final_state(kc, vc, lg_f, reverse=False)
        s_b = retention_final_state(kc, vc, lg_b, reverse=True)
        y_ctx = None

    q, k, v, g, p = jnp.split(h @ w_in, splits, axis=-1)
    q = rope_2d(heads(q), rows, cols)
    k = rope_2d(heads(k), rows, cols) * scale
    v = heads(v)
    o_f, _ = retention_chunked(q, k, v, lg_f, s_f, True)
    o_b, _ = retention_chunked(q[:, ::-1], k[:, ::-1], v[:, ::-1], lg_b, s_b, False)
    ret = retention_readout(o_f + o_b[:, ::-1], g, gn_g)
    pool = pool_project(pool_grid_diff(p, rows_n), pool_w, pool_scale)
    y = (jnp.concatenate([ret, pool.astype(ret.dtype)], axis=-1) @ w_out).astype(h.dtype)
    return y, y_ctx


def swiglu(h, w13, w2):
    u, gt = jnp.split(h @ w13, 2, axis=-1)
    return (jax.nn.silu(gt) * u) @ w2


def moe_swiglu(h, router_w, w13, w2):
    shp = h.shape
    t = h.reshape(-1, shp[-1])
    logits = (t @ router_w).astype(F32)
    top_val, top_idx = lax.top_k(logits, MOE_TOP_K)
    top_w = jax.nn.softmax(top_val, axis=-1)
    gate = jnp.sum(jax.nn.one_hot(top_idx, N_EXPERTS, dtype=F32) * top_w[..., None], axis=1)
    out = jnp.zeros(t.shape, F32)
    for e in range(N_EXPERTS):
        out = out + gate[:, e:e + 1] * swiglu(t, w13[e], w2[e]).astype(F32)
    return out.reshape(shp).astype(h.dtype)


def setup_inputs(seed: int = 0) -> dict:
    key = jax.random.key(seed)
    ks = jax.random.split(key, 20)
    n_dense = (DEPTH + 1) // 2
    n_moe = DEPTH // 2

    def nrm(k, shape, s=1.0):
        return jax.random.normal(k, shape, F32) * s

    base_logit = jnp.log(2.0 ** (5.0 + jnp.arange(RET_HEADS, dtype=F32)) - 1.0)
    return {
        'x': nrm(ks[0], (BATCH, SEQ, D_MODEL)),
        'c': nrm(ks[1], (BATCH, D_MODEL)),
        'ctx': nrm(ks[2], (BATCH, CTX_LEN, D_MODEL)),
        'c_ctx': nrm(ks[3], (D_MODEL,)),
        'w_ada': nrm(ks[4], (DEPTH, D_MODEL, 6 * D_MODEL), 0.5 * D_MODEL ** -0.5),
        'b_ada': nrm(ks[5], (DEPTH, 6 * D_MODEL), 0.02),
        'norm1_g': 1.0 + nrm(ks[6], (DEPTH, D_MODEL), 0.02),
        'norm2_g': 1.0 + nrm(ks[7], (DEPTH, D_MODEL), 0.02),
        'w_in': nrm(ks[8], (DEPTH, D_MODEL, IN_COLS), D_MODEL ** -0.5),
        'ret_decay_logit': base_logit[None, None, :] + nrm(ks[9], (DEPTH, 2, RET_HEADS), 0.01),
        'ret_gn_g': 1.0 + nrm(ks[10], (DEPTH, RET_WIDTH), 0.02),
        'pool_w': nrm(ks[11], (DEPTH, POOL_GROUPS, POOL_CH, POOL_CH), POOL_CH ** -0.5),
        'pool_scale': 1.0 + nrm(ks[12], (DEPTH, POOL_WIDTH), 0.02),
        'w_out': nrm(ks[13], (DEPTH, MIX_WIDTH, D_MODEL), MIX_WIDTH ** -0.5),
        'ffn_w13': nrm(ks[14], (n_dense, D_MODEL, 2 * D_FF), D_MODEL ** -0.5),
        'ffn_w2': nrm(ks[15], (n_dense, D_FF, D_MODEL), D_FF ** -0.5),
        'router_w': nrm(ks[16], (n_moe, D_MODEL, N_EXPERTS), D_MODEL ** -0.5),
        'moe_w13': nrm(ks[17], (n_moe, N_EXPERTS, D_MODEL, 2 * MOE_D_FF), D_MODEL ** -0.5),
        'moe_w2': nrm(ks[18], (n_moe, N_EXPERTS, MOE_D_FF, D_MODEL), MOE_D_FF ** -0.5),
        'final_norm_g': 1.0 + nrm(ks[19], (D_MODEL,), 0.02),
    }


def reference(x, c, ctx, c_ctx, w_ada, b_ada, norm1_g, norm2_g, w_in, ret_decay_logit, ret_gn_g,
              pool_w, pool_scale, w_out, ffn_w13, ffn_w2, router_w, moe_w13, moe_w2, final_norm_g):
    xc = ctx
    for l in range(DEPTH):
        last = l == DEPTH - 1
        mod_lat = (jax.nn.silu(c) @ w_ada[l] + b_ada[l])[:, None, :]
        sh1, sc1, gt1, sh2, sc2, gt2 = jnp.split(mod_lat, 6, axis=-1)
        mod_ctx = jax.nn.silu(c_ctx) @ w_ada[l] + b_ada[l]
        csh1, csc1, cgt1, csh2, csc2, cgt2 = jnp.split(mod_ctx, 6)
        log_gamma = jax.nn.log_sigmoid(ret_decay_logit[l].astype(F32))

        h = modulate(rmsnorm(x, norm1_g[l]), sh1, sc1)
        hc = modulate(rmsnorm(xc, norm1_g[l]), csh1, csc1)
        y, y_ctx = token_mix(h, hc, w_in[l], log_gamma, ret_gn_g[l], pool_w[l], pool_scale[l],
                             w_out[l], not last)
        x = x + gt1 * y

        if l % 2 == 0:
            def ffn(z, i=l // 2):
                return swiglu(z, ffn_w13[i], ffn_w2[i])
        else:
            def ffn(z, i=l // 2):
                return moe_swiglu(z, router_w[i], moe_w13[i], moe_w2[i])
        x = x + gt2 * ffn(modulate(rmsnorm(x, norm2_g[l]), sh2, sc2))

        if not last:
            xc = xc + cgt1 * y_ctx
            xc = xc + cgt2 * ffn(modulate(rmsnorm(xc, norm2_g[l]), csh2, csc2))
    return rmsnorm(x, final_norm_g)
```

```python
import numpy as np
import concourse.bass as bass
import concourse.mybir as mybir
from concourse.bass_utils import run_bass_kernel_spmd

F32 = mybir.dt.float32
BF16 = mybir.dt.bfloat16
AF = mybir.ActivationFunctionType
ALU = mybir.AluOpType

D = 1024
NCTX = 256
NLAT = 4096
NTOK = NCTX + NLAT
NT = NTOK // 128
DEPTH = 4
DFF = 2816
NFC = DFF // 128
NEXP = 8
GRID = 64
WINS = (2, 4, 8, 16)
NORM_EPS = 1e-6
GN_EPS = 1e-5
SCALE = 128 ** -0.5

QUADS = [(0, 256, True)] + [(256 + 512 * i, 512, False) for i in range(8)]
FFN_BLOCKS = [[0, 1, 2], [3, 4], [5, 6], [7, 8]]
F_HALVES = [(0, 12), (12, 10)]


class Buf:
    __slots__ = ("name", "w", "r")

    def __init__(self, name=""):
        self.name = name
        self.w = None
        self.r = []


class Sched:
    ENGS = ("pe", "act", "dve", "pool", "sp")

    def __init__(self, nc, n_dma_sems=48):
        self.nc = nc
        self.ops = {e: [] for e in self.ENGS}
        self.cnt = {e: 0 for e in self.ENGS}
        self.pending = {e: False for e in self.ENGS}
        self.sem = {e: nc.alloc_semaphore("s_" + e) for e in self.ENGS}
        self.seen = {e: {} for e in self.ENGS}
        self.dma_sems = [nc.alloc_semaphore("d%d" % i) for i in range(n_dma_sems)]
        self.dma_val = [0] * n_dma_sems
        self.dma_rr = 0
        self.dma_rr_q = {}
        self.n_ops = 0
        self.n_wait = 0

    def _deps(self, eng, reads, writes):
        best = {}
        for b in reads:
            t = b.w
            if t is not None and t[1] > best.get(t[0], 0):
                best[t[0]] = t[1]
        for b in writes:
            t = b.w
            if t is not None and t[1] > best.get(t[0], 0):
                best[t[0]] = t[1]
            for t in b.r:
                if t[1] > best.get(t[0], 0):
                    best[t[0]] = t[1]
        waits = []
        seen = self.seen[eng]
        for key, val in best.items():
            if key == "pe" and eng == "pe":
                continue
            if seen.get(key, 0) >= val:
                continue
            seen[key] = val
            waits.append((key, val))
        return waits

    def _mark(self, tok, reads, writes):
        for b in reads:
            b.r.append(tok)
        for b in writes:
            b.w = tok
            b.r = []

    def op(self, eng, fn, reads=(), writes=(), signal=True):
        waits = self._deps(eng, reads, writes)
        if signal:
            self.cnt[eng] += 1
            tok = (eng, self.cnt[eng])
            self.pending[eng] = False
        else:
            tok = (eng, self.cnt[eng] + 1)
            self.pending[eng] = True
        self.ops[eng].append((fn, waits, ("eng", signal)))
        self._mark(tok, reads, writes)
        self.n_ops += 1
        self.n_wait += len(waits)
        return tok

    def dma(self, q, fn, reads=(), writes=()):
        half = len(self.dma_sems) // 2
        base = 0 if q == "sp" else half
        rr = self.dma_rr_q.get(q, 0)
        self.dma_rr_q[q] = (rr + 1) % half
        i = base + rr
        waits = self._deps(q, reads, writes)
        key = ("dma", i)
        prev = self.dma_val[i]
        if prev > 0 and self.seen[q].get(key, 0) < prev:
            self.seen[q][key] = prev
            waits.append((key, prev))
        self.dma_val[i] += 16
        tok = (key, self.dma_val[i])
        self.ops[q].append((fn, waits, ("dma", i)))
        self._mark(tok, reads, writes)
        self.n_ops += 1
        self.n_wait += len(waits)
        return tok

    def barrier(self):
        for e in self.ENGS:
            waits = []
            seen = self.seen[e]
            for e2 in self.ENGS:
                if e2 == e:
                    continue
                if self.pending[e2]:
                    raise RuntimeError("barrier with pending unsignalled op on " + e2)
                v = self.cnt[e2]
                if v > 0 and seen.get(e2, 0) < v:
                    seen[e2] = v
                    waits.append((e2, v))
            for i, v in enumerate(self.dma_val):
                key = ("dma", i)
                if v > 0 and seen.get(key, 0) < v:
                    seen[key] = v
                    waits.append((key, v))
            if waits:
                self.ops[e].append((None, waits, None))
                self.n_wait += len(waits)

    def _semof(self, key):
        if isinstance(key, tuple):
            return self.dma_sems[key[1]]
        return self.sem[key]

    def emit(self):
        nc = self.nc
        engobj = {"pe": "tensor", "act": "scalar", "dve": "vector", "pool": "gpsimd", "sp": "sync"}
        for e in self.ENGS:
            if self.pending[e]:
                raise RuntimeError("engine %s ends with unsignalled op" % e)
        with nc.Block() as block:
            for e in self.ENGS:
                ops = self.ops[e]
                if not ops:
                    continue

                def body(eng, ops=ops, e=e):
                    for fn, waits, kind in ops:
                        for key, val in waits:
                            eng.wait_ge(self._semof(key), val)
                        if fn is None:
                            continue
                        ins = fn(eng)
                        if kind[0] == "eng":
                            if kind[1]:
                                ins.then_inc(self.sem[e], 1)
                        else:
                            ins.then_inc(self.dma_sems[kind[1]], 16)

                getattr(block, engobj[e])(body)


class Arena:
    def __init__(self, nc, nwords):
        self.t = nc.alloc_sbuf_tensor("arena", [128, nwords], F32)
        self.n = nwords
        self.top = 0
        self.marks = []

    def mark(self):
        self.marks.append(self.top)

    def release(self):
        self.top = self.marks.pop()

    def f32(self, n):
        n = int(n)
        a = self.top
        self.top += (n + 7) // 8 * 8
        assert self.top <= self.n, ("SBUF arena overflow", self.top, self.n)
        return self.t[:, a:a + n]

    def bf16(self, n):
        n = int(n)
        w = (n + 1) // 2
        a = self.top
        self.top += (w + 7) // 8 * 8
        assert self.top <= self.n, ("SBUF arena overflow", self.top, self.n)
        return self.t[:, a:a + w].bitcast(BF16)[:, 0:n]


def _win_matrix(n, w):
    A = np.zeros((n, n), np.float64)
    for i in range(n):
        lo = min(max(i - w // 2, 0), n)
        hi = min(max(i - w // 2 + w, 0), n)
        A[i, lo:hi] = 1.0 / (hi - lo)
    return A


def _pool_blocks():
    blocks = []
    index = {}
    plan = [[None] * NT for _ in range(4)]

    def add(M):
        M = np.ascontiguousarray(M.astype(np.float32))
        key = M.tobytes()
        if key not in index:
            index[key] = len(blocks)
            blocks.append(M)
        return index[key]

    eye = np.eye(128)
    for g, w in enumerate(WINS):
        A = _win_matrix(NCTX, w) - np.eye(NCTX)
        for jo in range(2):
            lst = []
            for ji in range(2):
                M = A[jo * 128:(jo + 1) * 128, ji * 128:(ji + 1) * 128].T
                if np.any(M != 0):
                    lst.append((ji, add(M)))
            plan[g][jo] = lst
        Ar = _win_matrix(GRID, w)
        Ac = _win_matrix(GRID, w)
        for jo in range(32):
            lst = []
            for ji in range(32):
                sub = Ar[2 * jo:2 * jo + 2, 2 * ji:2 * ji + 2]
                if not np.any(sub != 0):
                    continue
                M = np.kron(sub, Ac)
                if ji == jo:
                    M = M - eye
                lst.append((2 + ji, add(M.T)))
            plan[g][2 + jo] = lst
    return np.stack(blocks), plan


def _rope_tables():
    half = 64
    inv = 10000.0 ** (-np.arange(0, half, 2, dtype=np.float32) / np.float32(half))
    inv = inv.astype(np.float32)
    cos = np.ones((128, NT, 2, 32), np.float32)
    sin = np.zeros((128, NT, 2, 32), np.float32)
    t = np.arange(NLAT)
    rows = (t // GRID).astype(np.float32)
    cols = (t % GRID).astype(np.float32)
    for hidx, pos in enumerate((rows, cols)):
        ang = (pos[:, None] * inv[None, :]).astype(np.float32)
        c = np.cos(ang).astype(np.float32).reshape(32, 128, 32)
        s = np.sin(ang).astype(np.float32).reshape(32, 128, 32)
        cos[:, 2:, hidx, :] = c.transpose(1, 0, 2)
        sin[:, 2:, hidx, :] = s.transpose(1, 0, 2)
    return cos.reshape(128, -1), sin.reshape(128, -1)


_CONST_CACHE = {}


def _host_consts():
    if "c" in _CONST_CACHE:
        return _CONST_CACHE["c"]
    blocks, plan = _pool_blocks()
    cos, sin = _rope_tables()
    p = np.arange(128, dtype=np.float32)
    i = np.arange(128, dtype=np.float32)
    E = i[None, :] - p[:, None]
    misc = np.zeros((128, 6, 128), np.float32)
    misc[:, 0] = np.eye(128)
    misc[:, 1] = 1.0
    misc[:, 2] = np.maximum(E, 0)
    misc[:, 3] = np.maximum(-E, 0)
    misc[:, 4] = (E >= 0)
    misc[:, 5] = (E < 0)
    small = np.zeros((128, 264), np.float32)
    small[:, 0:128] = i[None, :] + 1.0
    small[:, 128:256] = 128.0 - i[None, :]
    small[:, 256] = 127.0 - p
    small[:, 257] = p
    small[:, 258] = NORM_EPS
    small[:, 259] = GN_EPS
    small[:, 260] = 1.0
    small[:, 261] = 128.0
    small[:, 262] = -0.5
    c = dict(blocks=blocks, plan=plan, cos=cos, sin=sin, misc=misc.reshape(128, -1), small=small)
    _CONST_CACHE["c"] = c
    return c


VEC_PER_LAYER = 72
NVEC = VEC_PER_LAYER * DEPTH + 8


def build_program(n_layers=DEPTH, stop_after_mix=False, nblk=69, plan=None):
    nc = bass.Bass("TRN2", target_bir_lowering=False)
    S = Sched(nc)

    def din(name, shape, dt=F32):
        return nc.dram_tensor(name, list(shape), dt, kind="ExternalInput").ap()

    xT_in = din("xT", [D, NTOK])
    cc_in = din("cc", [128, 16])
    vecs_in = din("vecs", [128, NVEC])
    dlog_in = din("dlog", [1, 32])
    w_ada = din("w_ada", [DEPTH, D, 6 * D])
    w_in = din("w_in", [DEPTH, D, 2560])
    w_out = din("w_out", [DEPTH, D, D])
    pool_w = din("pool_w", [DEPTH, 4, 128, 128])
    ffn_w13 = din("ffn_w13", [2, D, 2 * DFF])
    ffn_w2 = din("ffn_w2", [2, DFF, D])
    router_w = din("router_w", [2, D, NEXP])
    moe_w13 = din("moe_w13", [2, NEXP, D, 2 * DFF])
    moe_w2 = din("moe_w2", [2, NEXP, DFF, D])
    blocks_in = din("blocks", [nblk, 128, 128])
    cos_in = din("cos", [128, NT * 64])
    sin_in = din("sin", [128, NT * 64])
    misc_in = din("misc", [128, 768])
    small_in = din("small", [128, 264])
    outT = nc.dram_tensor("outT", [D, NLAT], F32, kind="ExternalOutput").ap()
    xres = nc.dram_tensor("xres", [D, NTOK], F32, kind="Internal").ap()
    pscr = nc.dram_tensor("pscr", [NT, 128, 512], BF16, kind="Internal").ap()
    sbscr = nc.dram_tensor("sbscr", [NT, 128, 512], BF16, kind="Internal").ap()
    xres_v = xres.rearrange("(k p) t -> p k t", p=128)
    outT_v = outT.rearrange("(k p) t -> p k t", p=128)

    A = Arena(nc, 52000)
    banks = [nc.alloc_psum_tensor("bank%d" % i, [128, 512], F32) for i in range(8)]
    bankb = [Buf("bank%d" % i) for i in range(8)]

    def bk(i):
        return banks[i][:]

    def bkb16(i):
        return banks[i][:].bitcast(BF16)

    xseg_b = [Buf("xseg%d" % s) for s in range(17)]
    pscr_b = [Buf("pscr%d" % t) for t in range(NT)]
    sbscr_b = [Buf("sbscr%d" % t) for t in range(NT)]
    out_b = Buf("out")

    misc = A.f32(768).rearrange("p (a b) -> p a b", a=6)
    misc_b = Buf("misc")
    small = A.f32(264)
    small_b = Buf("small")
    vecs = A.f32(NVEC)
    vecs_b = Buf("vecs")
    ident_bf = A.bf16(128)
    ones_bf = A.bf16(128)
    cbf_b = Buf("cbf")
    mod = A.f32(DEPTH * 96).rearrange("p (l j s) -> p l j s", l=DEPTH, j=48)
    mod_b = Buf("mod")
    lg_all = A.f32(32)
    lg_b = Buf("lg")
    silc = A.f32(16)
    silc_b = Buf("silc")
    ident_f = misc[:, 0, :]
    ones_f = misc[:, 1, :]
    Epos, Eneg, Mf, Mb = misc[:, 2, :], misc[:, 3, :], misc[:, 4, :], misc[:, 5, :]
    eps_n = small[:, 258:259]
    eps_g = small[:, 259:260]

    S.dma("sp", lambda e: e.dma_start(out=misc.rearrange("p a b -> p (a b)"), in_=misc_in[:, :]), writes=[misc_b])
    S.dma("sp", lambda e: e.dma_start(out=small, in_=small_in[:, :]), writes=[small_b])
    S.dma("sp", lambda e: e.dma_start(out=vecs, in_=vecs_in[:, :]), writes=[vecs_b])
    S.dma("sp", lambda e: e.dma_start(out=silc, in_=cc_in[:, :]), writes=[silc_b])
    S.dma("sp", lambda e: e.dma_start(out=lg_all, in_=dlog_in.partition_broadcast(128).rearrange("p a b -> p (a b)")
                                      if len(dlog_in.partition_broadcast(128).shape) == 3 else dlog_in.partition_broadcast(128)),
          writes=[lg_b])
    S.op("dve", lambda e: e.tensor_copy(out=ident_bf, in_=ident_f), reads=[misc_b], writes=[cbf_b])
    S.op("dve", lambda e: e.tensor_copy(out=ones_bf, in_=ones_f), reads=[misc_b], writes=[cbf_b])
    for s in range(17):
        S.dma("sp", lambda e, s=s: e.dma_start(out=xres[:, s * 256:(s + 1) * 256], in_=xT_in[:, s * 256:(s + 1) * 256]),
              writes=[xseg_b[s]])

    S.op("act", lambda e: e.activation(out=lg_all, in_=lg_all, func=AF.Exp, scale=-1.0), reads=[lg_b], writes=[lg_b])
    S.op("dve", lambda e: e.tensor_scalar(out=lg_all, in0=lg_all, scalar1=1.0, scalar2=None, op0=ALU.add),
         reads=[lg_b], writes=[lg_b])
    S.op("act", lambda e: e.activation(out=lg_all, in_=lg_all, func=AF.Ln), reads=[lg_b], writes=[lg_b])
    S.op("dve", lambda e: e.tensor_scalar(out=lg_all, in0=lg_all, scalar1=-1.0, scalar2=None, op0=ALU.mult),
         reads=[lg_b], writes=[lg_b])
    S.op("act", lambda e: e.activation(out=silc, in_=silc, func=AF.Silu), reads=[silc_b], writes=[silc_b])

    A.mark()
    wa_st = [A.f32(8 * 1024).rearrange("p (k n) -> p k n", k=8) for _ in range(2)]
    wa_b = [Buf("wa0"), Buf("wa1")]
    silc_v = silc.rearrange("p (s k) -> p k s", s=2)
    it = 0
    for l in range(n_layers):
        for j6 in range(6):
            i = it % 2
            it += 1
            for half in range(2):
                S.dma("sp", lambda e, i=i, l=l, j6=j6, half=half: e.dma_start(
                    out=wa_st[i][:, half * 4:(half + 1) * 4, :],
                    in_=w_ada[l, half * 512:(half + 1) * 512, j6 * 1024:(j6 + 1) * 1024].rearrange("(k p) n -> p k n", p=128)),
                    writes=[wa_b[i]])
            pb = j6 % 2
            for n in range(8):
                for k in range(8):
                    S.op("pe", lambda e, i=i, n=n, k=k, pb=pb: e.matmul(
                        bk(pb)[:, n * 2:n * 2 + 2], lhsT=wa_st[i][:, k, n * 128:(n + 1) * 128], rhs=silc_v[:, k, :],
                        start=(k == 0), stop=(k == 7)),
                        reads=[wa_b[i], silc_b], writes=[bankb[pb]], signal=(k == 7))
            bofs = l * VEC_PER_LAYER + 24 + j6 * 8
            S.op("dve", lambda e, l=l, j6=j6, pb=pb, bofs=bofs: e.tensor_tensor(
                out=mod[:, l, j6 * 8:(j6 + 1) * 8, :],
                in0=bk(pb)[:, 0:16].rearrange("p (n s) -> p n s", s=2),
                in1=vecs[:, bofs:bofs + 8].unsqueeze(2).broadcast_to([128, 8, 2]), op=ALU.add),
                reads=[bankb[pb], vecs_b], writes=[mod_b])
    S.barrier()
    A.release()

    ctx = dict(nc=nc, S=S, A=A, bk=bk, bkb16=bkb16, bankb=bankb, xres_v=xres_v, xseg_b=xseg_b,
               pscr=pscr, pscr_b=pscr_b, sbscr=sbscr, sbscr_b=sbscr_b, misc=misc, misc_b=misc_b, small=small,
               small_b=small_b, vecs=vecs, vecs_b=vecs_b, ident_bf=ident_bf, ones_bf=ones_bf, cbf_b=cbf_b,
               mod=mod, mod_b=mod_b, lg_all=lg_all, lg_b=lg_b, ident_f=ident_f, ones_f=ones_f,
               Epos=Epos, Eneg=Eneg, Mf=Mf, Mb=Mb, eps_n=eps_n, eps_g=eps_g,
               w_in=w_in, w_out=w_out, pool_w=pool_w, blocks_in=blocks_in, cos_in=cos_in, sin_in=sin_in,
               ffn_w13=ffn_w13, ffn_w2=ffn_w2, router_w=router_w, moe_w13=moe_w13, moe_w2=moe_w2,
               plan=plan, nblk=nblk)

    for l in range(n_layers):
        emit_mixer(ctx, l)
        if stop_after_mix and l == n_layers - 1:
            break
        emit_ffn(ctx, l)

    emit_final(ctx, vecs[:, VEC_PER_LAYER * DEPTH:VEC_PER_LAYER * DEPTH + 8], outT_v, out_b,
               raw=(n_layers < DEPTH or stop_after_mix))
    S.barrier()
    S.emit()
    return nc


def emit_norm_seg(ctx, s, xq, xq_b, gvec, svec, hT_out, hT_b, nb, h32_out=None, h32_b=None, statbank=7):
    S, bk, bankb = ctx["S"], ctx["bk"], ctx["bankb"]
    sq, sq_b, rstd, rstd_b, tmp, tmp_b = nb
    S.op("act", lambda e: e.activation(out=sq, in_=xq, func=AF.Square), reads=[xq_b], writes=[sq_b])
    for k in range(8):
        S.op("pe", lambda e, k=k: e.matmul(bk(statbank)[:, 0:256], lhsT=ctx["ones_bf"], rhs=sq[:, k, :],
                                           start=(k == 0), stop=(k == 7)),
             reads=[sq_b, ctx["cbf_b"]], writes=[bankb[statbank]], signal=(k == 7))
    S.op("act", lambda e: e.activation(out=rstd, in_=bk(statbank)[:, 0:256], func=AF.Ln, bias=ctx["eps_n"], scale=1.0 / D),
         reads=[bankb[statbank], ctx["small_b"]], writes=[rstd_b])
    S.op("act", lambda e: e.activation(out=rstd, in_=rstd, func=AF.Exp, scale=-0.5), reads=[rstd_b], writes=[rstd_b])
    dst, dst_b = (hT_out, hT_b) if h32_out is None else (h32_out, h32_b)
    for k in range(8):
        i = k % 2
        S.op("dve", lambda e, k=k, i=i: e.tensor_tensor(out=tmp[i], in0=xq[:, k, :], in1=rstd, op=ALU.mult),
             reads=[xq_b, rstd_b], writes=[tmp_b[i]])
        if svec is not None:
            S.op("act", lambda e, k=k, i=i: e.activation(out=dst[:, k, :], in_=tmp[i], func=AF.Identity,
                                                         bias=svec[:, k:k + 1], scale=gvec[:, k:k + 1]),
                 reads=[tmp_b[i], ctx["mod_b"], ctx["vecs_b"]], writes=[dst_b])
        else:
            S.op("act", lambda e, k=k, i=i: e.activation(out=dst[:, k, :], in_=tmp[i], func=AF.Identity,
                                                         scale=gvec[:, k:k + 1]),
                 reads=[tmp_b[i], ctx["mod_b"], ctx["vecs_b"]], writes=[dst_b])
    if h32_out is not None and hT_out is not None:
        S.op("pool", lambda e: e.tensor_copy(out=hT_out, in_=h32_out), reads=[h32_b], writes=[hT_b])


def emit_layer_vectors(ctx, l, which):
    S, A = ctx["S"], ctx["A"]
    mod, vecs = ctx["mod"], ctx["vecs"]
    G = A.f32(16).rearrange("p (k s) -> p k s", s=2)
    g_ofs = l * VEC_PER_LAYER + (0 if which == 1 else 8)
    j_sc = 8 if which == 1 else 32
    S.op("dve", lambda e: e.scalar_tensor_tensor(
        out=G, in0=mod[:, l, j_sc:j_sc + 8, :], scalar=1.0,
        in1=vecs[:, g_ofs:g_ofs + 8].unsqueeze(2).broadcast_to([128, 8, 2]), op0=ALU.add, op1=ALU.mult),
        reads=[ctx["mod_b"], ctx["vecs_b"]], writes=[ctx["mod_b"]])
    return G


def emit_rope(ctx, src_view, dst_view, cs, sn, nh, tmps, tmps_b, src_b, dst_b, tab_b):
    S = ctx["S"]
    u1, u2 = src_view[:, :, :, 0, :], src_view[:, :, :, 1, :]
    o1, o2 = dst_view[:, :, :, 0, :], dst_view[:, :, :, 1, :]
    cb = cs.unsqueeze(1).broadcast_to([128, nh, 2, 32])
    sb = sn.unsqueeze(1).broadcast_to([128, nh, 2, 32])
    t = [x[:, 0:nh * 64].rearrange("p (h a f) -> p h a f", h=nh, a=2) for x in tmps]
    S.op("dve", lambda e: e.tensor_tensor(out=t[0], in0=u1, in1=cb, op=ALU.mult), reads=[src_b, tab_b], writes=[tmps_b[0]])
    S.op("dve", lambda e: e.tensor_tensor(out=t[1], in0=u2, in1=sb, op=ALU.mult), reads=[src_b, tab_b], writes=[tmps_b[1]])
    S.op("dve", lambda e: e.tensor_tensor(out=t[2], in0=u1, in1=sb, op=ALU.mult), reads=[src_b, tab_b], writes=[tmps_b[2]])
    S.op("dve", lambda e: e.tensor_tensor(out=t[3], in0=u2, in1=cb, op=ALU.mult), reads=[src_b, tab_b], writes=[tmps_b[3]])
    S.op("pool", lambda e: e.tensor_tensor(out=o1, in0=t[0], in1=t[1], op=ALU.subtract),
         reads=[tmps_b[0], tmps_b[1]], writes=[dst_b])
    S.op("pool", lambda e: e.tensor_tensor(out=o2, in0=t[2], in1=t[3], op=ALU.add),
         reads=[tmps_b[2], tmps_b[3]], writes=[dst_b])


def emit_mixer(ctx, l):
    S, A, bk, bkb16, bankb = ctx["S"], ctx["A"], ctx["bk"], ctx["bkb16"], ctx["bankb"]
    mod, vecs, small, lg_all = ctx["mod"], ctx["vecs"], ctx["small"], ctx["lg_all"]
    plan = ctx["plan"]
    nblk = ctx["nblk"]
    last = (l == DEPTH - 1)
    S.barrier()
    A.mark()
    win = A.bf16(8 * 2560).rearrange("p (k n) -> p k n", k=8)
    win_b = Buf("win")
    wout = A.bf16(8 * 1024).rearrange("p (k n) -> p k n", k=8)
    wout_b = Buf("wout")
    poolw = A.bf16(4 * 128).rearrange("p (g n) -> p g n", g=4)
    poolw_b = Buf("poolw")
    blocks = A.bf16(nblk * 128).rearrange("p (b n) -> p b n", b=nblk)
    blocks_b = Buf("blocks")
    for k in range(8):
        for hf in range(2):
            S.dma("pool", lambda e, k=k, hf=hf: e.dma_start(
                out=win[:, k, hf * 1280:(hf + 1) * 1280], in_=ctx["w_in"][l, k * 128:(k + 1) * 128, hf * 1280:(hf + 1) * 1280]),
                writes=[win_b])
    S.dma("pool", lambda e: e.dma_start(out=wout, in_=ctx["w_out"][l].rearrange("(k p) n -> p k n", p=128)), writes=[wout_b])
    S.dma("pool", lambda e: e.dma_start(out=poolw, in_=ctx["pool_w"][l].rearrange("g c d -> c g d")), writes=[poolw_b])
    for b0 in range(0, nblk, 16):
        b1 = min(nblk, b0 + 16)
        S.dma("pool", lambda e, b0=b0, b1=b1: e.dma_start(
            out=blocks[:, b0:b1, :], in_=ctx["blocks_in"][b0:b1].rearrange("b p n -> p b n")), writes=[blocks_b])

    G1 = emit_layer_vectors(ctx, l, 1)
    S1 = mod[:, l, 0:8, :]
    GT1 = mod[:, l, 16:24, :]
    gn_ofs = l * VEC_PER_LAYER + 16
    rsc = vecs[:, gn_ofs:gn_ofs + 8]

    Dcomb = A.f32(512).rearrange("p (h n) -> p h n", h=4)
    decqf = A.f32(512).rearrange("p (h n) -> p h n", h=4)
    decqb = A.f32(512).rearrange("p (h n) -> p h n", h=4)
    dk = A.f32(16)
    dtmp = [A.f32(128), A.f32(128)]
    dec_b = Buf("dec")
    dtmp_b = [Buf("dtmp0"), Buf("dtmp1")]
    lgf = lg_all[:, l * 8:l * 8 + 4]
    lgb = lg_all[:, l * 8 + 4:l * 8 + 8]
    rd = [ctx["lg_b"], ctx["misc_b"], ctx["small_b"]]
    for h in range(4):
        S.op("act", lambda e, h=h: e.activation(out=dtmp[0], in_=ctx["Epos"], func=AF.Exp, scale=lgf[:, h:h + 1]),
             reads=rd, writes=[dtmp_b[0]])
        S.op("dve", lambda e, h=h: e.scalar_tensor_tensor(out=Dcomb[:, h, :], in0=dtmp[0], scalar=SCALE, in1=ctx["Mf"],
                                                          op0=ALU.mult, op1=ALU.mult),
             reads=[dtmp_b[0]] + rd, writes=[dec_b])
        S.op("act", lambda e, h=h: e.activation(out=dtmp[1], in_=ctx["Eneg"], func=AF.Exp, scale=lgb[:, h:h + 1]),
             reads=rd, writes=[dtmp_b[1]])
        S.op("dve", lambda e, h=h: e.scalar_tensor_tensor(out=dtmp[1], in0=dtmp[1], scalar=SCALE, in1=ctx["Mb"],
                                                          op0=ALU.mult, op1=ALU.mult),
             reads=[dtmp_b[1]] + rd, writes=[dtmp_b[1]])
        S.op("dve", lambda e, h=h: e.tensor_tensor(out=Dcomb[:, h, :], in0=Dcomb[:, h, :], in1=dtmp[1], op=ALU.add),
             reads=[dtmp_b[1], dec_b], writes=[dec_b])
        S.op("act", lambda e, h=h: e.activation(out=decqf[:, h, :], in_=small[:, 0:128], func=AF.Exp, scale=lgf[:, h:h + 1]),
             reads=rd, writes=[dec_b])
        S.op("act", lambda e, h=h: e.activation(out=decqb[:, h, :], in_=small[:, 128:256], func=AF.Exp, scale=lgb[:, h:h + 1]),
             reads=rd, writes=[dec_b])
    S.op("dve", lambda e: e.tensor_scalar(out=dk[:, 0:4], in0=lgf, scalar1=small[:, 256:257], scalar2=None, op0=ALU.mult),
         reads=rd, writes=[dec_b])
    S.op("dve", lambda e: e.tensor_scalar(out=dk[:, 4:8], in0=lgb, scalar1=small[:, 257:258], scalar2=None, op0=ALU.mult),
         reads=rd, writes=[dec_b])
    S.op("dve", lambda e: e.tensor_scalar(out=dk[:, 8:12], in0=lgf, scalar1=128.0, scalar2=None, op0=ALU.mult),
         reads=rd, writes=[dec_b])
    S.op("dve", lambda e: e.tensor_scalar(out=dk[:, 12:16], in0=lgb, scalar1=128.0, scalar2=None, op0=ALU.mult),
         reads=rd, writes=[dec_b])
    S.op("act", lambda e: e.activation(out=dk, in_=dk, func=AF.Exp), reads=[dec_b], writes=[dec_b])
    S.op("dve", lambda e: e.tensor_scalar(out=dk[:, 0:8], in0=dk[:, 0:8], scalar1=SCALE, scalar2=None, op0=ALU.mult),
         reads=[dec_b], writes=[dec_b])
    dkf, dkb, gcf, gcb = dk[:, 0:4], dk[:, 4:8], dk[:, 8:12], dk[:, 12:16]

    xq = [A.f32(8 * 256).rearrange("p (k n) -> p k n", k=8) for _ in range(3)]
    xq_b = [Buf("xq%d" % i) for i in range(3)]
    sq = A.bf16(8 * 256).rearrange("p (k n) -> p k n", k=8)
    nb = (sq, Buf("sq"), A.f32(256), Buf("rstd"), [A.f32(256), A.f32(256)], [Buf("tmp0"), Buf("tmp1")])
    hT = [A.bf16(8 * 256).rearrange("p (k n) -> p k n", k=8) for _ in range(2)]
    hT_b = [Buf("hT0"), Buf("hT1")]
    cst = [A.f32(128).rearrange("p (j a f) -> p j a f", j=2, a=2) for _ in range(3)]
    snt = [A.f32(128).rearrange("p (j a f) -> p j a f", j=2, a=2) for _ in range(3)]
    tab_b = [Buf("tab0"), Buf("tab1"), Buf("tab2")]
    rtmp = [A.f32(512) for _ in range(4)]
    rtmp_b = [Buf("rt%d" % i) for i in range(4)]
    P2 = range(2)
    qk_tm = [A.bf16(1024) for _ in P2]
    qk_b = [Buf("qk_tm%d" % i) for i in P2]
    v_tm = [A.bf16(512) for _ in P2]
    v_b = [Buf("v_tm%d" % i) for i in P2]
    sg = [A.f32(512) for _ in P2]
    sg_b = [Buf("sg%d" % i) for i in P2]
    kT = [A.bf16(512).rearrange("p (h n) -> p h n", h=4) for _ in P2]
    qT = [A.bf16(512).rearrange("p (h n) -> p h n", h=4) for _ in P2]
    qfT = [A.bf16(512).rearrange("p (h n) -> p h n", h=4) for _ in P2]
    qbT = [A.bf16(512).rearrange("p (h n) -> p h n", h=4) for _ in P2]
    qkT_b = [Buf("qkT%d" % i) for i in P2]
    ktil = [A.bf16(512).rearrange("p (h n) -> p h n", h=4) for _ in P2]
    ktil_b = [Buf("ktil%d" % i) for i in P2]
    pst = [A.bf16(512) for _ in P2]
    pst_b = [Buf("pst%d" % i) for i in P2]
    PT = A.bf16(512).rearrange("p (h n) -> p h n", h=4)
    PT_b = Buf("PT")
    St = A.f32(512).rearrange("p (h n) -> p h n", h=4)
    St_bf = A.bf16(512).rearrange("p (h n) -> p h n", h=4)
    St_b = Buf("St")
    Stbf_b = Buf("Stbf")
    sbst = [A.bf16(512) for _ in P2]
    sbst_b = [Buf("sbst%d" % i) for i in P2]
    pslot = [A.bf16(512) for _ in range(12)]
    pslot_b = [Buf("pslot%d" % i) for i in range(12)]
    sbslot = [A.bf16(512).rearrange("p (h n) -> p h n", h=4) for _ in range(4)]
    sbslot_b = [Buf("sbslot%d" % i) for i in range(4)]
    stats = A.f32(24).rearrange("p (h s) -> p h s", h=4)
    mv = A.f32(8).rearrange("p (h s) -> p h s", h=4)
    rs4 = A.f32(4)
    gn_b = Buf("gn")
    on = A.f32(512).rearrange("p (h n) -> p h n", h=4)
    on_b = Buf("on")
    ret_tm = A.bf16(512)
    ret_b = Buf("ret_tm")
    mixT = [A.bf16(1024).rearrange("p (k n) -> p k n", k=8) for _ in P2]
    mixr_b = [Buf("mixr%d" % i) for i in P2]
    mixp_b = [Buf("mixp%d" % i) for i in P2]
    dT = A.bf16(512).rearrange("p (g n) -> p g n", g=4)
    dT_b = Buf("dT")
    xnew = [A.f32(1024).rearrange("p (k n) -> p k n", k=8) for _ in P2]
    xnew_b = [Buf("xnew%d" % i) for i in P2]
    cos_v = ctx["cos_in"].rearrange("p (t a f) -> p t a f", t=NT, a=2)
    sin_v = ctx["sin_in"].rearrange("p (t a f) -> p t a f", t=NT, a=2)
    STATB = 3

    def load_seg(s, n):
        i3 = n % 3
        S.dma("sp", lambda e: e.dma_start(out=xq[i3], in_=ctx["xres_v"][:, :, s * 256:(s + 1) * 256]),
              reads=[ctx["xseg_b"][s]], writes=[xq_b[i3]])
        S.dma("sp", lambda e: e.dma_start(out=cst[i3], in_=cos_v[:, 2 * s:2 * s + 2]), writes=[tab_b[i3]])
        S.dma("sp", lambda e: e.dma_start(out=snt[i3], in_=sin_v[:, 2 * s:2 * s + 2]), writes=[tab_b[i3]])

    def norm_seg(s, n):
        sidx = 1 if s == 0 else 0
        emit_norm_seg(ctx, s, xq[n % 3], xq_b[n % 3], G1[:, :, sidx], S1[:, :, sidx], hT[n % 2], hT_b[n % 2], nb, statbank=STATB)

    def project(n, tl, col0, ncols, bank0):
        h_, hb_ = hT[n % 2], hT_b[n % 2]
        for c in range(ncols // 512):
            for k in range(8):
                S.op("pe", lambda e, c=c, k=k: e.matmul(
                    bk(bank0 + c), lhsT=h_[:, k, tl * 128:(tl + 1) * 128], rhs=win[:, k, col0 + c * 512:col0 + (c + 1) * 512],
                    start=(k == 0), stop=(k == 7)),
                    reads=[hb_, win_b], writes=[bankb[bank0 + c]], signal=(k == 7))

    def make_ktil(p, dkv):
        kview = qk_tm[p][:, 512:1024].rearrange("p (h n) -> p h n", h=4)
        S.op("pool", lambda e: e.tensor_tensor(out=ktil[p], in0=kview, in1=dkv.unsqueeze(2).broadcast_to([128, 4, 128]), op=ALU.mult),
             reads=[qk_b[p], dec_b], writes=[ktil_b[p]])

    def state_update(p, gcv):
        for h in range(4):
            S.op("pe", lambda e, h=h: e.matmul(bk(7)[:, h * 128:(h + 1) * 128], lhsT=ktil[p][:, h, :], rhs=v_tm[p][:, h * 128:(h + 1) * 128],
                                               start=True, stop=True),
                 reads=[ktil_b[p], v_b[p]], writes=[bankb[7]], signal=(h == 3))
        S.op("pool", lambda e: e.tensor_tensor(out=St, in0=St, in1=gcv.unsqueeze(2).broadcast_to([128, 4, 128]), op=ALU.mult),
             reads=[St_b, dec_b], writes=[St_b])
        S.op("dve", lambda e: e.tensor_tensor(out=St, in0=St, in1=bk(7).rearrange("p (h n) -> p h n", h=4), op=ALU.add),
             reads=[St_b, bankb[7]], writes=[St_b])
        S.op("act", lambda e: e.copy(out=St_bf, in_=St), reads=[St_b], writes=[Stbf_b])

    def zero_state():
        S.op("pool", lambda e: e.memset(St.rearrange("p h n -> p (h n)"), 0.0), writes=[St_b])
        S.op("pool", lambda e: e.memset(St_bf.rearrange("p h n -> p (h n)"), 0.0), writes=[Stbf_b])

    zero_state()
    seg_order = [0] + list(range(16, 0, -1))
    tiles = []
    for n, s in enumerate(seg_order):
        for tl in (1, 0):
            tiles.append((2 * s + tl, n, s, tl))

    def pre_front(j):
        t, n, s, tl = tiles[j]
        p = j % 2
        project(n, tl, 512, 512, 0)
        project(n, tl, 1024, 512, 2)
        project(n, tl, 2048, 512, 3)
        yield
        kv5 = bk(0).rearrange("p (h a b f) -> p h a b f", h=4, a=2, b=2)
        kd5 = qk_tm[p][:, 512:1024].rearrange("p (h a b f) -> p h a b f", h=4, a=2, b=2)
        emit_rope(ctx, kv5, kd5, cst[n % 3][:, tl], snt[n % 3][:, tl], 4, rtmp, rtmp_b, bankb[0], qk_b[p], tab_b[n % 3])
        S.op("act", lambda e: e.copy(out=v_tm[p], in_=bk(2)), reads=[bankb[2]], writes=[v_b[p]])
        S.op("act", lambda e: e.copy(out=pst[p], in_=bk(3)), reads=[bankb[3]], writes=[pst_b[p]])
        S.dma("pool", lambda e: e.dma_start(out=ctx["pscr"][t], in_=pst[p]), reads=[pst_b[p]], writes=[ctx["pscr_b"][t]])
        make_ktil(p, dkb)
        if tl == 0:
            if n + 2 < len(seg_order):
                load_seg(seg_order[n + 2], n + 2)
            if n + 1 < len(seg_order):
                norm_seg(seg_order[n + 1], n + 1)

    def pre_back(j):
        t, n, s, tl = tiles[j]
        p = j % 2
        S.op("act", lambda e: e.copy(out=sbst[p], in_=St_bf.rearrange("p h n -> p (h n)")), reads=[Stbf_b], writes=[sbst_b[p]])
        S.dma("pool", lambda e: e.dma_start(out=ctx["sbscr"][t], in_=sbst[p]), reads=[sbst_b[p]], writes=[ctx["sbscr_b"][t]])
        state_update(p, gcb)
        yield

    def interleave(gens):
        active = list(gens)
        while active:
            for g_ in list(active):
                try:
                    next(g_)
                except StopIteration:
                    active.remove(g_)

    load_seg(seg_order[0], 0)
    load_seg(seg_order[1], 1)
    norm_seg(seg_order[0], 0)
    interleave([pre_front(0)])
    for j in range(len(tiles)):
        gens = [pre_back(j)]
        if j + 1 < len(tiles):
            gens.append(pre_front(j + 1))
        interleave(gens)

    zero_state()

    def p_needed(t):
        r = set()
        for g in range(4):
            for (ti, _) in plan[g][t]:
                r.add(ti)
        return r

    loaded_p = set()

    def prefetch_p(t):
        if t in loaded_p or t >= NT:
            return
        loaded_p.add(t)
        S.dma("sp", lambda e: e.dma_start(out=pslot[t % 12], in_=ctx["pscr"][t]), reads=[ctx["pscr_b"][t]], writes=[pslot_b[t % 12]])

    def prefetch_sb(t):
        if t < NT:
            S.dma("sp", lambda e: e.dma_start(out=sbslot[t % 4].rearrange("p h n -> p (h n)"), in_=ctx["sbscr"][t]),
                  reads=[ctx["sbscr_b"][t]], writes=[sbslot_b[t % 4]])

    def main_front(t):
        s, tl = t // 2, t % 2
        n = s
        p = t % 2
        prefetch_sb(t + 2)
        for tt in sorted(p_needed(t) | (p_needed(t + 1) if t + 1 < NT else set())):
            prefetch_p(tt)
        project(n, tl, 0, 1024, 0)
        project(n, tl, 1024, 1024, 2)
        yield
        for qi in range(2):
            sv = bk(qi).rearrange("p (h a b f) -> p h a b f", h=4, a=2, b=2)
            dv = qk_tm[p][:, qi * 512:(qi + 1) * 512].rearrange("p (h a b f) -> p h a b f", h=4, a=2, b=2)
            emit_rope(ctx, sv, dv, cst[n % 3][:, tl], snt[n % 3][:, tl], 4, rtmp, rtmp_b, bankb[qi], qk_b[p], tab_b[n % 3])
        S.op("act", lambda e: e.copy(out=v_tm[p], in_=bk(2)), reads=[bankb[2]], writes=[v_b[p]])
        S.op("act", lambda e: e.activation(out=sg[p], in_=bk(3), func=AF.Silu), reads=[bankb[3]], writes=[sg_b[p]])
        make_ktil(p, dkf)
        yield
        if tl == 1:
            if s + 2 < 17:
                load_seg(s + 2, s + 2)
            if s + 1 < 17:
                norm_seg(s + 1, s + 1)
        b4 = bkb16(4).rearrange("p (j n) -> p j n", j=8)
        for j in range(8):
            S.op("pe", lambda e, j=j: e.transpose(out=b4[:, j, :], in_=qk_tm[p][:, j * 128:(j + 1) * 128], identity=ctx["ident_bf"]),
                 reads=[qk_b[p], ctx["cbf_b"]], writes=[bankb[4]], signal=(j == 7))
        S.op("act", lambda e: e.copy(out=kT[p], in_=b4[:, 4:8, :]), reads=[bankb[4]], writes=[qkT_b[p]])
        S.op("act", lambda e: e.copy(out=qT[p], in_=b4[:, 0:4, :]), reads=[bankb[4]], writes=[qkT_b[p]])
        S.op("dve", lambda e: e.tensor_tensor(out=qfT[p], in0=b4[:, 0:4, :], in1=decqf, op=ALU.mult),
             reads=[bankb[4], dec_b], writes=[qkT_b[p]])
        S.op("dve", lambda e: e.tensor_tensor(out=qbT[p], in0=b4[:, 0:4, :], in1=decqb, op=ALU.mult),
             reads=[bankb[4], dec_b], writes=[qkT_b[p]])
        yield
        for g in range(4):
            lst = plan[g][t]
            for n_i, (ti, bi) in enumerate(lst):
                S.op("pe", lambda e, g=g, ti=ti, bi=bi, n_i=n_i, L=len(lst): e.matmul(
                    bk(0)[:, g * 128:(g + 1) * 128], lhsT=pslot[ti % 12][:, g * 128:(g + 1) * 128], rhs=blocks[:, bi, :],
                    start=(n_i == 0), stop=(n_i == L - 1)),
                    reads=[pslot_b[ti % 12], blocks_b], writes=[bankb[0]], signal=(g == 3 and n_i == len(lst) - 1))
        S.op("act", lambda e: e.copy(out=dT, in_=bk(0).rearrange("p (g n) -> p g n", g=4)), reads=[bankb[0]], writes=[dT_b])
        yield
        for g in range(4):
            S.op("pe", lambda e, g=g: e.matmul(bk(1)[:, g * 128:(g + 1) * 128], lhsT=poolw[:, g, :], rhs=dT[:, g, :], start=True, stop=True),
                 reads=[poolw_b, dT_b], writes=[bankb[1]], signal=(g == 3))
        S.op("dve", lambda e: e.tensor_tensor(out=mixT[p][:, 4:8, :], in0=bk(1).rearrange("p (g n) -> p g n", g=4),
                                              in1=rsc[:, 4:8].unsqueeze(2).broadcast_to([128, 4, 128]), op=ALU.mult),
             reads=[bankb[1], ctx["vecs_b"]], writes=[mixp_b[p]])

    def main_back(t):
        s, tl = t // 2, t % 2
        p = t % 2
        sidx = 1 if s == 0 else 0
        xi = s % 3
        for h in range(4):
            S.op("pe", lambda e, h=h: e.matmul(bk(5)[:, h * 128:(h + 1) * 128], lhsT=kT[p][:, h, :], rhs=qT[p][:, h, :], start=True, stop=True),
                 reads=[qkT_b[p]], writes=[bankb[5]], signal=(h == 3))
        S.op("dve", lambda e: e.tensor_tensor(out=PT, in0=bk(5).rearrange("p (h n) -> p h n", h=4), in1=Dcomb, op=ALU.mult),
             reads=[bankb[5], dec_b], writes=[PT_b])
        yield
        sbs = sbslot[t % 4]
        for h in range(4):
            o_h = bk(6)[:, h * 128:(h + 1) * 128]
            S.op("pe", lambda e, h=h, o_h=o_h: e.matmul(o_h, lhsT=PT[:, h, :], rhs=v_tm[p][:, h * 128:(h + 1) * 128], start=True, stop=False),
                 reads=[PT_b, v_b[p]], writes=[bankb[6]], signal=False)
            S.op("pe", lambda e, h=h, o_h=o_h: e.matmul(o_h, lhsT=qfT[p][:, h, :], rhs=St_bf[:, h, :], start=False, stop=False),
                 reads=[qkT_b[p], Stbf_b], writes=[bankb[6]], signal=False)
            S.op("pe", lambda e, h=h, o_h=o_h: e.matmul(o_h, lhsT=qbT[p][:, h, :], rhs=sbs[:, h, :], start=False, stop=True),
                 reads=[qkT_b[p], sbslot_b[t % 4]], writes=[bankb[6]], signal=(h == 3))
        state_update(p, gcf)
        yield
        if last and s == 0:
            return
        o3 = bk(6).rearrange("p (h n) -> p h n", h=4)
        for h in range(4):
            S.op("dve", lambda e, h=h: e.bn_stats(out=stats[:, h, :], in_=o3[:, h, :]), reads=[bankb[6]], writes=[gn_b])
        for h in range(4):
            S.op("dve", lambda e, h=h: e.bn_aggr(out=mv[:, h, :], in_=stats[:, h, :]), reads=[gn_b], writes=[gn_b])
        S.op("dve", lambda e: e.tensor_scalar(out=rs4, in0=mv[:, :, 1], scalar1=GN_EPS, scalar2=None, op0=ALU.add),
             reads=[gn_b], writes=[gn_b])
        S.op("pool", lambda e: e.tensor_tensor(out=rs4, in0=rs4, in1=ctx["small"][:, 262:263].broadcast_to([128, 4]), op=ALU.pow),
             reads=[gn_b, ctx["small_b"]], writes=[gn_b])
        for h in range(4):
            S.op("dve", lambda e, h=h: e.tensor_scalar(out=on[:, h, :], in0=o3[:, h, :], scalar1=mv[:, h, 0:1], scalar2=rs4[:, h:h + 1],
                                                       op0=ALU.subtract, op1=ALU.mult),
                 reads=[bankb[6], gn_b], writes=[on_b])
        S.op("pool", lambda e: e.tensor_tensor(out=ret_tm, in0=on.rearrange("p h n -> p (h n)"), in1=sg[p], op=ALU.mult),
             reads=[on_b, sg_b[p]], writes=[ret_b])
        yield
        b7r = bkb16(7)[:, 0:512].rearrange("p (j n) -> p j n", j=4)
        for h in range(4):
            S.op("pe", lambda e, h=h: e.transpose(out=b7r[:, h, :], in_=ret_tm[:, h * 128:(h + 1) * 128], identity=ctx["ident_bf"]),
                 reads=[ret_b, ctx["cbf_b"]], writes=[bankb[7]], signal=(h == 3))
        S.op("dve", lambda e: e.tensor_tensor(out=mixT[p][:, 0:4, :], in0=b7r, in1=rsc[:, 0:4].unsqueeze(2).broadcast_to([128, 4, 128]), op=ALU.mult),
             reads=[bankb[7], ctx["vecs_b"]], writes=[mixr_b[p]])
        yield
        for n_ in range(8):
            for k in range(8):
                S.op("pe", lambda e, n_=n_, k=k: e.matmul(
                    bk(5 + n_ // 4)[:, (n_ % 4) * 128:(n_ % 4 + 1) * 128], lhsT=wout[:, k, n_ * 128:(n_ + 1) * 128], rhs=mixT[p][:, k, :],
                    start=(k == 0), stop=(k == 7)),
                    reads=[wout_b, mixr_b[p], mixp_b[p]], writes=[bankb[5 + n_ // 4]], signal=(k == 7 and n_ % 4 == 3))
        if not (last and s == 0):
            for n_ in range(8):
                S.op("dve", lambda e, n_=n_: e.scalar_tensor_tensor(
                    out=xnew[p][:, n_, :], in0=bk(5 + n_ // 4)[:, (n_ % 4) * 128:(n_ % 4 + 1) * 128], scalar=GT1[:, n_, sidx:sidx + 1],
                    in1=xq[xi][:, n_, tl * 128:(tl + 1) * 128], op0=ALU.mult, op1=ALU.add),
                    reads=[bankb[5 + n_ // 4], xq_b[xi], ctx["mod_b"]], writes=[xnew_b[p]])
            S.dma("pool", lambda e: e.dma_start(out=ctx["xres_v"][:, :, t * 128:(t + 1) * 128], in_=xnew[p]),
                  reads=[xnew_b[p]], writes=[ctx["xseg_b"][s]])

    load_seg(0, 0)
    load_seg(1, 1)
    norm_seg(0, 0)
    prefetch_sb(0)
    prefetch_sb(1)
    for t0 in (0, 1):
        prefetch_p(t0)
    interleave([main_front(0)])
    for t in range(NT):
        gens = [main_back(t)]
        if t + 1 < NT:
            gens.append(main_front(t + 1))
        interleave(gens)
    S.barrier()
    A.release()


FFN_SEG_BLOCKS = [list(range(0, 5)), list(range(5, 9)), list(range(9, 13)), list(range(13, 17))]


def emit_ffn(ctx, l):
    S, A, bk, bankb = ctx["S"], ctx["A"], ctx["bk"], ctx["bankb"]
    mod, vecs = ctx["mod"], ctx["vecs"]
    is_moe = (l % 2 == 1)
    li = l // 2
    last = (l == DEPTH - 1)
    S.barrier()
    A.mark()
    G2 = emit_layer_vectors(ctx, l, 2)
    S2 = mod[:, l, 24:32, :]
    GT2 = mod[:, l, 40:48, :]
    TBMAX = 1280
    h2T = A.bf16(8 * TBMAX).rearrange("p (k n) -> p k n", k=8)
    h2T_b = Buf("h2T")
    abuf = A.bf16(12 * TBMAX).rearrange("p (c n) -> p c n", c=12)
    a_b = Buf("a")
    acc = A.f32(8 * TBMAX).rearrange("p (k n) -> p k n", k=8)
    acc_b = Buf("acc")
    wst = [A.bf16(2 * 8 * 512).rearrange("p (u k n) -> p u k n", u=2, k=8) for _ in range(2)]
    wst_b = [Buf("wst%d" % i) for i in range(2)]
    w2sb = A.bf16(12 * 1024).rearrange("p (c n) -> p c n", c=12)
    w2_b = Buf("w2sb")
    xq = [A.f32(8 * 256).rearrange("p (k n) -> p k n", k=8) for _ in range(2)]
    xq_b = [Buf("fxq0"), Buf("fxq1")]
    sq = A.bf16(8 * 256).rearrange("p (k n) -> p k n", k=8)
    nb = (sq, Buf("fsq"), A.f32(256), Buf("frstd"), [A.f32(256), A.f32(256)], [Buf("ftmp0"), Buf("ftmp1")])
    sgb = [A.f32(512), A.f32(512)]
    sgb_b = [Buf("sgb0"), Buf("sgb1")]
    xnew = A.f32(8 * 256).rearrange("p (k n) -> p k n", k=8)
    xnew_b = Buf("fxnew")
    if is_moe:
        h2f = xnew
        h2f_b = xnew_b
        rw = A.f32(64).rearrange("p (k e) -> p k e", k=8)
        rw_b = Buf("rw")
        Gt = A.f32(10 * 8).rearrange("p (t e) -> p t e", t=10)
        Gt_b = Buf("Gt")
        gsm = A.f32(48)
        gsm_b = Buf("gsm")
        dg = [A.f32(128), A.f32(128)]
        dg_b = [Buf("dg0"), Buf("dg1")]
        gbc = A.f32(512)
        gbc_b = Buf("gbc")
        tmpg = [A.f32(512), A.f32(512)]
        tmpg_b = [Buf("tmpg0"), Buf("tmpg1")]
        S.dma("sp", lambda e: e.dma_start(out=rw, in_=ctx["router_w"][li].rearrange("(k p) e -> p k e", p=128)), writes=[rw_b])
        items = [(e_, hf) for e_ in range(NEXP) for hf in range(2)]
    else:
        items = [(None, 0), (None, 1)]

    def w13_of(e_):
        return ctx["moe_w13"][li, e_] if is_moe else ctx["ffn_w13"][li]

    def w2_of(e_):
        return ctx["moe_w2"][li, e_] if is_moe else ctx["ffn_w2"][li]

    ugrot = [0]
    w2rot = [0]
    strot = [0]

    for segs in FFN_SEG_BLOCKS:
        if last and segs[0] == 0:
            segs = segs[1:]
        TB = 256 * len(segs)
        tok0 = segs[0] * 256
        if segs[0] == 0:
            cgs = [(0, 256), (256, 512), (768, 512)]
        else:
            cgs = [(0, 512), (512, 512)]
        for n, s in enumerate(segs):
            i = n % 2
            sidx = 1 if s == 0 else 0
            S.dma("sp", lambda e, s=s, i=i: e.dma_start(out=xq[i], in_=ctx["xres_v"][:, :, s * 256:(s + 1) * 256]),
                  reads=[ctx["xseg_b"][s]], writes=[xq_b[i]])
            c0 = (s - segs[0]) * 256
            if is_moe:
                emit_norm_seg(ctx, s, xq[i], xq_b[i], G2[:, :, sidx], S2[:, :, sidx], h2T[:, :, c0:c0 + 256], h2T_b, nb,
                              h32_out=h2f, h32_b=h2f_b)
                for tl in range(2):
                    tb = (s - segs[0]) * 2 + tl
                    lgp = bk(7)[:, 256 + tl * 8:256 + tl * 8 + 8]
                    for k in range(8):
                        S.op("pe", lambda e, k=k, tl=tl, lgp=lgp: e.matmul(lgp, lhsT=h2f[:, k, tl * 128:(tl + 1) * 128], rhs=rw[:, k, :],
                                                                           start=(k == 0), stop=(k == 7)),
                             reads=[h2f_b, rw_b], writes=[bankb[7]], signal=(k == 7))
                    lg8, mx, dlt, w1, w2_, g1 = gsm[:, 0:8], gsm[:, 8:16], gsm[:, 16:17], gsm[:, 17:18], gsm[:, 18:19], gsm[:, 24:32]
                    S.op("act", lambda e, lgp=lgp: e.copy(out=lg8, in_=lgp), reads=[bankb[7]], writes=[gsm_b])
                    S.op("dve", lambda e: e.max(out=mx, in_=lg8), reads=[gsm_b], writes=[gsm_b])
                    S.op("dve", lambda e: e.tensor_tensor(out=dlt, in0=mx[:, 1:2], in1=mx[:, 0:1], op=ALU.subtract), reads=[gsm_b], writes=[gsm_b])
                    S.op("act", lambda e: e.activation(out=dlt, in_=dlt, func=AF.Exp), reads=[gsm_b], writes=[gsm_b])
                    S.op("dve", lambda e: e.tensor_scalar(out=w1, in0=dlt, scalar1=1.0, scalar2=None, op0=ALU.add), reads=[gsm_b], writes=[gsm_b])
                    S.op("dve", lambda e: e.reciprocal(out=w1, in_=w1), reads=[gsm_b], writes=[gsm_b])
                    S.op("dve", lambda e: e.tensor_tensor(out=w2_, in0=dlt, in1=w1, op=ALU.mult), reads=[gsm_b], writes=[gsm_b])
                    S.op("dve", lambda e: e.tensor_scalar(out=g1, in0=lg8, scalar1=mx[:, 0:1], scalar2=w1, op0=ALU.is_equal, op1=ALU.mult),
                         reads=[gsm_b], writes=[gsm_b])
                    S.op("dve", lambda e, tb=tb: e.tensor_scalar(out=Gt[:, tb, :], in0=lg8, scalar1=mx[:, 1:2], scalar2=w2_, op0=ALU.is_equal, op1=ALU.mult),
                         reads=[gsm_b], writes=[Gt_b])
                    S.op("dve", lambda e, tb=tb: e.tensor_tensor(out=Gt[:, tb, :], in0=Gt[:, tb, :], in1=g1, op=ALU.add),
                         reads=[gsm_b, Gt_b], writes=[Gt_b])
            else:
                emit_norm_seg(ctx, s, xq[i], xq_b[i], G2[:, :, sidx], S2[:, :, sidx], h2T[:, :, c0:c0 + 256], h2T_b, nb)

        flat = []
        for it_i, (e_, hf) in enumerate(items):
            fc0, nfc = F_HALVES[hf]
            for c4 in range(0, nfc, 4):
                flat.append((it_i, e_, hf, c4, min(4, nfc - c4)))

        def w13_dma(gi):
            it_i, e_, hf, c4, ng = flat[gi]
            fc0, nfc = F_HALVES[hf]
            w13 = w13_of(e_)
            si = gi % 2
            col_u = (fc0 + c4) * 128
            col_g = DFF + (fc0 + c4) * 128
            for uu, col in enumerate((col_u, col_g)):
                S.dma("pool", lambda e, si=si, uu=uu, col=col, w13=w13, ng=ng: e.dma_start(
                    out=wst[si][:, uu, :, 0:ng * 128], in_=w13[:, col:col + ng * 128].rearrange("(k p) n -> p k n", p=128)),
                    writes=[wst_b[si]])

        def w2_dma(it_i):
            e_, hf = items[it_i]
            fc0, nfc = F_HALVES[hf]
            w2 = w2_of(e_)
            for c2 in range(0, nfc, 2):
                S.dma("pool", lambda e, c2=c2, fc0=fc0, w2=w2: e.dma_start(
                    out=w2sb[:, c2:c2 + 2, :], in_=w2[(fc0 + c2) * 128:(fc0 + c2 + 2) * 128, :].rearrange("(c p) n -> p c n", p=128)),
                    writes=[w2_b])

        w13_dma(0)
        w2_dma(0)
        if len(flat) > 1:
            w13_dma(1)
        for gi, (it_i, e_, hf, c4, ng) in enumerate(flat):
            fc0, nfc = F_HALVES[hf]
            si = gi % 2
            for (cc0, cn) in cgs:
                for cl in range(ng):
                    pr = (ugrot[0] % 2) * 2
                    ugrot[0] += 1
                    for uu in range(2):
                        for k in range(8):
                            S.op("pe", lambda e, si=si, uu=uu, k=k, cl=cl, pr=pr, cc0=cc0, cn=cn: e.matmul(
                                bk(pr + uu)[:, 0:cn], lhsT=wst[si][:, uu, k, cl * 128:(cl + 1) * 128], rhs=h2T[:, k, cc0:cc0 + cn],
                                start=(k == 0), stop=(k == 7)),
                                reads=[wst_b[si], h2T_b], writes=[bankb[pr + uu]], signal=(k == 7))
                    sgi = (ugrot[0]) % 2
                    S.op("act", lambda e, pr=pr, cn=cn, sgi=sgi: e.activation(out=sgb[sgi][:, 0:cn], in_=bk(pr + 1)[:, 0:cn], func=AF.Silu),
                         reads=[bankb[pr + 1]], writes=[sgb_b[sgi]])
                    S.op("dve", lambda e, pr=pr, cn=cn, sgi=sgi, c4=c4, cl=cl, cc0=cc0: e.tensor_tensor(
                        out=abuf[:, c4 + cl, cc0:cc0 + cn], in0=bk(pr)[:, 0:cn], in1=sgb[sgi][:, 0:cn], op=ALU.mult),
                        reads=[bankb[pr], sgb_b[sgi]], writes=[a_b])
            if gi + 2 < len(flat):
                w13_dma(gi + 2)
            last_of_item = (gi + 1 == len(flat)) or (flat[gi + 1][0] != it_i)
            if not last_of_item:
                continue
            for (cc0, cn) in cgs:
                if is_moe:
                    ntl = cn // 128
                    for tl in range(ntl):
                        tb = cc0 // 128 + tl
                        di = tl % 2
                        S.op("dve", lambda e, tb=tb, di=di, e_=e_: e.tensor_scalar(out=dg[di], in0=ctx["ident_f"], scalar1=Gt[:, tb, e_:e_ + 1],
                                                                                   scalar2=None, op0=ALU.mult),
                             reads=[Gt_b, ctx["misc_b"]], writes=[dg_b[di]])
                        S.op("pe", lambda e, tl=tl, di=di: e.matmul(bk(7)[:, tl * 128:(tl + 1) * 128], lhsT=ctx["ones_f"], rhs=dg[di], start=True, stop=True),
                             reads=[dg_b[di], ctx["misc_b"]], writes=[bankb[7]], signal=True)
                    S.op("act", lambda e, cn=cn: e.copy(out=gbc[:, 0:cn], in_=bk(7)[:, 0:cn]), reads=[bankb[7]], writes=[gbc_b])
                for n_ in range(8):
                    ob = 4 + (w2rot[0] % 3)
                    w2rot[0] += 1
                    for c in range(nfc):
                        S.op("pe", lambda e, ob=ob, c=c, n_=n_, cc0=cc0, cn=cn, nfc=nfc: e.matmul(
                            bk(ob)[:, 0:cn], lhsT=w2sb[:, c, n_ * 128:(n_ + 1) * 128], rhs=abuf[:, c, cc0:cc0 + cn],
                            start=(c == 0), stop=(c == nfc - 1)),
                            reads=[w2_b, a_b], writes=[bankb[ob]], signal=(c == nfc - 1))
                    dst = acc[:, n_, cc0:cc0 + cn]
                    if not is_moe:
                        if it_i == 0:
                            S.op("act", lambda e, ob=ob, cn=cn, dst=dst: e.copy(out=dst, in_=bk(ob)[:, 0:cn]), reads=[bankb[ob]], writes=[acc_b])
                        else:
                            S.op("dve", lambda e, ob=ob, cn=cn, dst=dst: e.tensor_tensor(out=dst, in0=dst, in1=bk(ob)[:, 0:cn], op=ALU.add),
                                 reads=[bankb[ob], acc_b], writes=[acc_b])
                    else:
                        if it_i == 0:
                            S.op("dve", lambda e, ob=ob, cn=cn, dst=dst: e.tensor_tensor(out=dst, in0=bk(ob)[:, 0:cn], in1=gbc[:, 0:cn], op=ALU.mult),
                                 reads=[bankb[ob], gbc_b], writes=[acc_b])
                        else:
                            tg = tmpg[w2rot[0] % 2]
                            tg_b = tmpg_b[w2rot[0] % 2]
                            S.op("dve", lambda e, ob=ob, cn=cn, tg=tg: e.tensor_tensor(out=tg[:, 0:cn], in0=bk(ob)[:, 0:cn], in1=gbc[:, 0:cn], op=ALU.mult),
                                 reads=[bankb[ob], gbc_b], writes=[tg_b])
                            S.op("pool", lambda e, cn=cn, dst=dst, tg=tg: e.tensor_tensor(out=dst, in0=dst, in1=tg[:, 0:cn], op=ALU.add),
                                 reads=[tg_b, acc_b], writes=[acc_b])
            if it_i + 1 < len(items):
                w2_dma(it_i + 1)
        for n, s in enumerate(segs):
            if last and s == 0:
                continue
            i = n % 2
            sidx = 1 if s == 0 else 0
            c0 = (s - segs[0]) * 256
            S.dma("sp", lambda e, s=s, i=i: e.dma_start(out=xq[i], in_=ctx["xres_v"][:, :, s * 256:(s + 1) * 256]),
                  reads=[ctx["xseg_b"][s]], writes=[xq_b[i]])
            for n_ in range(8):
                S.op("dve", lambda e, n_=n_, i=i, c0=c0, sidx=sidx: e.scalar_tensor_tensor(
                    out=xnew[:, n_, :], in0=acc[:, n_, c0:c0 + 256], scalar=GT2[:, n_, sidx:sidx + 1], in1=xq[i][:, n_, :],
                    op0=ALU.mult, op1=ALU.add),
                    reads=[acc_b, xq_b[i], ctx["mod_b"]], writes=[xnew_b])
            S.dma("sp", lambda e, s=s: e.dma_start(out=ctx["xres_v"][:, :, s * 256:(s + 1) * 256], in_=xnew),
                  reads=[xnew_b], writes=[ctx["xseg_b"][s]])
    S.barrier()
    A.release()


def emit_final(ctx, gfin, outT_v, out_b, raw=False):
    S, A = ctx["S"], ctx["A"]
    S.barrier()
    A.mark()
    xq = [A.f32(8 * 256).rearrange("p (k n) -> p k n", k=8) for _ in range(2)]
    xq_b = [Buf("oxq0"), Buf("oxq1")]
    sq = A.bf16(8 * 256).rearrange("p (k n) -> p k n", k=8)
    nb = (sq, Buf("osq"), A.f32(256), Buf("orstd"), [A.f32(256), A.f32(256)], [Buf("otmp0"), Buf("otmp1")])
    yo = [A.f32(8 * 256).rearrange("p (k n) -> p k n", k=8) for _ in range(2)]
    yo_b = [Buf("yo0"), Buf("yo1")]
    for s in range(1, 17):
        i = s % 2
        S.dma("sp", lambda e, s=s, i=i: e.dma_start(out=xq[i], in_=ctx["xres_v"][:, :, s * 256:(s + 1) * 256]),
              reads=[ctx["xseg_b"][s]], writes=[xq_b[i]])
        if raw:
            S.dma("sp", lambda e, s=s, i=i: e.dma_start(out=outT_v[:, :, (s - 1) * 256:s * 256], in_=xq[i]),
                  reads=[xq_b[i]], writes=[out_b])
            continue
        emit_norm_seg(ctx, s, xq[i], xq_b[i], gfin, None, None, None, nb, h32_out=yo[i], h32_b=yo_b[i])
        S.dma("sp", lambda e, s=s, i=i: e.dma_start(out=outT_v[:, :, (s - 1) * 256:s * 256], in_=yo[i]),
              reads=[yo_b[i]], writes=[out_b])
    A.release()


def _pk(v):
    v = np.asarray(v, np.float32)
    return np.ascontiguousarray(v.reshape(-1, 128).T)


_PROG_CACHE = {}


def _prepare_inputs(x, c, ctx, c_ctx, w_ada, b_ada, norm1_g, norm2_g, w_in, ret_decay_logit, ret_gn_g,
                    pool_w, pool_scale, w_out, ffn_w13, ffn_w2, router_w, moe_w13, moe_w2, final_norm_g):
    hc = _host_consts()
    f = lambda a: np.ascontiguousarray(np.asarray(a, np.float32))
    vecs = np.zeros((128, NVEC), np.float32)
    for l in range(DEPTH):
        o = l * VEC_PER_LAYER
        vecs[:, o:o + 8] = _pk(norm1_g[l])
        vecs[:, o + 8:o + 16] = _pk(norm2_g[l])
        vecs[:, o + 16:o + 20] = _pk(ret_gn_g[l])
        vecs[:, o + 20:o + 24] = _pk(pool_scale[l])
        vecs[:, o + 24:o + 72] = _pk(b_ada[l])
    vecs[:, VEC_PER_LAYER * DEPTH:] = _pk(final_norm_g)
    shared = dict(vecs=vecs, dlog=f(ret_decay_logit).reshape(1, 32), w_ada=f(w_ada), w_in=f(w_in), w_out=f(w_out),
                  pool_w=f(pool_w), ffn_w13=f(ffn_w13), ffn_w2=f(ffn_w2), router_w=f(router_w), moe_w13=f(moe_w13),
                  moe_w2=f(moe_w2), blocks=hc["blocks"], cos=hc["cos"], sin=hc["sin"], misc=hc["misc"], small=hc["small"])
    x = np.asarray(x, np.float32)
    ctx = np.asarray(ctx, np.float32)
    c = np.asarray(c, np.float32)
    c_ctx = np.asarray(c_ctx, np.float32)
    in_maps = []
    for b in range(x.shape[0]):
        xT = np.ascontiguousarray(np.concatenate([ctx[b], x[b]], axis=0).T)
        cc = np.ascontiguousarray(np.concatenate([_pk(c[b]), _pk(c_ctx)], axis=1))
        m = dict(shared)
        m["xT"] = xT
        m["cc"] = cc
        in_maps.append(m)
    return in_maps


def kernel(x, c, ctx, c_ctx, w_ada, b_ada, norm1_g, norm2_g, w_in, ret_decay_logit, ret_gn_g,
           pool_w, pool_scale, w_out, ffn_w13, ffn_w2, router_w, moe_w13, moe_w2, final_norm_g):
    hc = _host_consts()
    in_maps = _prepare_inputs(x, c, ctx, c_ctx, w_ada, b_ada, norm1_g, norm2_g, w_in, ret_decay_logit, ret_gn_g,
                              pool_w, pool_scale, w_out, ffn_w13, ffn_w2, router_w, moe_w13, moe_w2, final_norm_g)
    if "nc" not in _PROG_CACHE:
        _PROG_CACHE["nc"] = build_program(plan=hc["plan"], nblk=hc["blocks"].shape[0])
    nc = _PROG_CACHE["nc"]
    res = run_bass_kernel_spmd(nc, in_maps, core_ids=list(range(len(in_maps))))
    out = np.stack([np.ascontiguousarray(np.asarray(r["outT"], np.float32).T) for r in res.results], axis=0)
    return out.astype(np.float32)
```

```python
import numpy as np
import concourse.bass as bass
import concourse.mybir as mybir
from concourse.bass_utils import run_bass_kernel_spmd

F32 = mybir.dt.float32
BF16 = mybir.dt.bfloat16
AF = mybir.ActivationFunctionType
ALU = mybir.AluOpType

D = 1024
NCTX = 256
NLAT = 4096
NTOK = NCTX + NLAT
NT = NTOK // 128
DEPTH = 4
DFF = 2816
NFC = DFF // 128
NEXP = 8
GRID = 64
WINS = (2, 4, 8, 16)
NORM_EPS = 1e-6
GN_EPS = 1e-5
SCALE = 128 ** -0.5

QUADS = [(0, 256, True)] + [(256 + 512 * i, 512, False) for i in range(8)]
FFN_BLOCKS = [[0, 1, 2], [3, 4], [5, 6], [7, 8]]
F_HALVES = [(0, 12), (12, 10)]


class Buf:
    __slots__ = ("name", "w", "r")

    def __init__(self, name=""):
        self.name = name
        self.w = None
        self.r = []


class Sched:
    ENGS = ("pe", "act", "dve", "pool", "sp")

    def __init__(self, nc, n_dma_sems=48):
        self.nc = nc
        self.ops = {e: [] for e in self.ENGS}
        self.cnt = {e: 0 for e in self.ENGS}
        self.pending = {e: False for e in self.ENGS}
        self.sem = {e: nc.alloc_semaphore("s_" + e) for e in self.ENGS}
        self.seen = {e: {} for e in self.ENGS}
        self.dma_sems = [nc.alloc_semaphore("d%d" % i) for i in range(n_dma_sems)]
        self.dma_val = [0] * n_dma_sems
        self.dma_rr = 0
        self.dma_rr_q = {}
        self.n_ops = 0
        self.n_wait = 0

    def _deps(self, eng, reads, writes):
        best = {}
        for b in reads:
            t = b.w
            if t is not None and t[1] > best.get(t[0], 0):
                best[t[0]] = t[1]
        for b in writes:
            t = b.w
            if t is not None and t[1] > best.get(t[0], 0):
                best[t[0]] = t[1]
            for t in b.r:
                if t[1] > best.get(t[0], 0):
                    best[t[0]] = t[1]
        waits = []
        seen = self.seen[eng]
        for key, val in best.items():
            if key == "pe" and eng == "pe":
                continue
            if seen.get(key, 0) >= val:
                continue
            seen[key] = val
            waits.append((key, val))
        return waits

    def _mark(self, tok, reads, writes):
        for b in reads:
            b.r.append(tok)
        for b in writes:
            b.w = tok
            b.r = []

    def op(self, eng, fn, reads=(), writes=(), signal=True):
        waits = self._deps(eng, reads, writes)
        if signal:
            self.cnt[eng] += 1
            tok = (eng, self.cnt[eng])
            self.pending[eng] = False
        else:
            tok = (eng, self.cnt[eng] + 1)
            self.pending[eng] = True
        self.ops[eng].append((fn, waits, ("eng", signal)))
        self._mark(tok, reads, writes)
        self.n_ops += 1
        self.n_wait += len(waits)
        return tok

    def dma(self, q, fn, reads=(), writes=()):
        half = len(self.dma_sems) // 2
        base = 0 if q == "sp" else half
        rr = self.dma_rr_q.get(q, 0)
        self.dma_rr_q[q] = (rr + 1) % half
        i = base + rr
        waits = self._deps(q, reads, writes)
        key = ("dma", i)
        prev = self.dma_val[i]
        if prev > 0 and self.seen[q].get(key, 0) < prev:
            self.seen[q][key] = prev
            waits.append((key, prev))
        self.dma_val[i] += 16
        tok = (key, self.dma_val[i])
        self.ops[q].append((fn, waits, ("dma", i)))
        self._mark(tok, reads, writes)
        self.n_ops += 1
        self.n_wait += len(waits)
        return tok

    def barrier(self):
        for e in self.ENGS:
            waits = []
            seen = self.seen[e]
            for e2 in self.ENGS:
                if e2 == e:
                    continue
                if self.pending[e2]:
                    raise RuntimeError("barrier with pending unsignalled op on " + e2)
                v = self.cnt[e2]
                if v > 0 and seen.get(e2, 0) < v:
                    seen[e2] = v
                    waits.append((e2, v))
            for i, v in enumerate(self.dma_val):
                key = ("dma", i)
                if v > 0 and seen.get(key, 0) < v:
                    seen[key] = v
                    waits.append((key, v))
            if waits:
                self.ops[e].append((None, waits, None))
                self.n_wait += len(waits)

    def _semof(self, key):
        if isinstance(key, tuple):
            return self.dma_sems[key[1]]
        return self.sem[key]

    def emit(self):
        nc = self.nc
        engobj = {"pe": "tensor", "act": "scalar", "dve": "vector", "pool": "gpsimd", "sp": "sync"}
        for e in self.ENGS:
            if self.pending[e]:
                raise RuntimeError("engine %s ends with unsignalled op" % e)
        with nc.Block() as block:
            for e in self.ENGS:
                ops = self.ops[e]
                if not ops:
                    continue

                def body(eng, ops=ops, e=e):
                    for fn, waits, kind in ops:
                        for key, val in waits:
                            eng.wait_ge(self._semof(key), val)
                        if fn is None:
                            continue
                        ins = fn(eng)
                        if kind[0] == "eng":
                            if kind[1]:
                                ins.then_inc(self.sem[e], 1)
                        else:
                            ins.then_inc(self.dma_sems[kind[1]], 16)

                getattr(block, engobj[e])(body)


class Arena:
    def __init__(self, nc, nwords):
        self.t = nc.alloc_sbuf_tensor("arena", [128, nwords], F32)
        self.n = nwords
        self.top = 0
        self.marks = []

    def mark(self):
        self.marks.append(self.top)

    def release(self):
        self.top = self.marks.pop()

    def f32(self, n):
        n = int(n)
        a = self.top
        self.top += (n + 7) // 8 * 8
        assert self.top <= self.n, ("SBUF arena overflow", self.top, self.n)
        return self.t[:, a:a + n]

    def bf16(self, n):
        n = int(n)
        w = (n + 1) // 2
        a = self.top
        self.top += (w + 7) // 8 * 8
        assert self.top <= self.n, ("SBUF arena overflow", self.top, self.n)
        return self.t[:, a:a + w].bitcast(BF16)[:, 0:n]


def _win_matrix(n, w):
    A = np.zeros((n, n), np.float64)
    for i in range(n):
        lo = min(max(i - w // 2, 0), n)
        hi = min(max(i - w // 2 + w, 0), n)
        A[i, lo:hi] = 1.0 / (hi - lo)
    return A


def _pool_blocks():
    blocks = []
    index = {}
    plan = [[None] * NT for _ in range(4)]

    def add(M):
        M = np.ascontiguousarray(M.astype(np.float32))
        key = M.tobytes()
        if key not in index:
            index[key] = len(blocks)
            blocks.append(M)
        return index[key]

    eye = np.eye(128)
    for g, w in enumerate(WINS):
        A = _win_matrix(NCTX, w) - np.eye(NCTX)
        for jo in range(2):
            lst = []
            for ji in range(2):
                M = A[jo * 128:(jo + 1) * 128, ji * 128:(ji + 1) * 128].T
                if np.any(M != 0):
                    lst.append((ji, add(M)))
            plan[g][jo] = lst
        Ar = _win_matrix(GRID, w)
        Ac = _win_matrix(GRID, w)
        for jo in range(32):
            lst = []
            for ji in range(32):
                sub = Ar[2 * jo:2 * jo + 2, 2 * ji:2 * ji + 2]
                if not np.any(sub != 0):
                    continue
                M = np.kron(sub, Ac)
                if ji == jo:
                    M = M - eye
                lst.append((2 + ji, add(M.T)))
            plan[g][2 + jo] = lst
    return np.stack(blocks), plan


def _rope_tables():
    half = 64
    inv = 10000.0 ** (-np.arange(0, half, 2, dtype=np.float32) / np.float32(half))
    inv = inv.astype(np.float32)
    cos = np.ones((128, NT, 2, 32), np.float32)
    sin = np.zeros((128, NT, 2, 32), np.float32)
    t = np.arange(NLAT)
    rows = (t // GRID).astype(np.float32)
    cols = (t % GRID).astype(np.float32)
    for hidx, pos in enumerate((rows, cols)):
        ang = (pos[:, None] * inv[None, :]).astype(np.float32)
        c = np.cos(ang).astype(np.float32).reshape(32, 128, 32)
        s = np.sin(ang).astype(np.float32).reshape(32, 128, 32)
        cos[:, 2:, hidx, :] = c.transpose(1, 0, 2)
        sin[:, 2:, hidx, :] = s.transpose(1, 0, 2)
    return cos.reshape(128, -1), sin.reshape(128, -1)


_CONST_CACHE = {}


def _host_consts():
    if "c" in _CONST_CACHE:
        return _CONST_CACHE["c"]
    blocks, plan = _pool_blocks()
    cos, sin = _rope_tables()
    p = np.arange(128, dtype=np.float32)
    i = np.arange(128, dtype=np.float32)
    E = i[None, :] - p[:, None]
    misc = np.zeros((128, 6, 128), np.float32)
    misc[:, 0] = np.eye(128)
    misc[:, 1] = 1.0
    misc[:, 2] = np.maximum(E, 0)
    misc[:, 3] = np.maximum(-E, 0)
    misc[:, 4] = (E >= 0)
    misc[:, 5] = (E < 0)
    small = np.zeros((128, 264), np.float32)
    small[:, 0:128] = i[None, :] + 1.0
    small[:, 128:256] = 128.0 - i[None, :]
    small[:, 256] = 127.0 - p
    small[:, 257] = p
    small[:, 258] = NORM_EPS
    small[:, 259] = GN_EPS
    small[:, 260] = 1.0
    small[:, 261] = 128.0
    small[:, 262] = -0.5
    c = dict(blocks=blocks, plan=plan, cos=cos, sin=sin, misc=misc.reshape(128, -1), small=small)
    _CONST_CACHE["c"] = c
    return c


VEC_PER_LAYER = 72
NVEC = VEC_PER_LAYER * DEPTH + 8


def build_program(n_layers=DEPTH, stop_after_mix=False, nblk=69, plan=None):
    nc = bass.Bass("TRN2", target_bir_lowering=False)
    S = Sched(nc)

    def din(name, shape, dt=F32):
        return nc.dram_tensor(name, list(shape), dt, kind="ExternalInput").ap()

    xT_in = din("xT", [D, NTOK])
    cc_in = din("cc", [128, 16])
    vecs_in = din("vecs", [128, NVEC])
    dlog_in = din("dlog", [1, 32])
    w_ada = din("w_ada", [DEPTH, D, 6 * D])
    w_in = din("w_in", [DEPTH, D, 2560])
    w_out = din("w_out", [DEPTH, D, D])
    pool_w = din("pool_w", [DEPTH, 4, 128, 128])
    ffn_w13 = din("ffn_w13", [2, D, 2 * DFF])
    ffn_w2 = din("ffn_w2", [2, DFF, D])
    router_w = din("router_w", [2, D, NEXP])
    moe_w13 = din("moe_w13", [2, NEXP, D, 2 * DFF])
    moe_w2 = din("moe_w2", [2, NEXP, DFF, D])
    blocks_in = din("blocks", [nblk, 128, 128])
    cos_in = din("cos", [128, NT * 64])
    sin_in = din("sin", [128, NT * 64])
    misc_in = din("misc", [128, 768])
    small_in = din("small", [128, 264])
    outT = nc.dram_tensor("outT", [D, NLAT], F32, kind="ExternalOutput").ap()
    xres = nc.dram_tensor("xres", [D, NTOK], F32, kind="Internal").ap()
    pscr = nc.dram_tensor("pscr", [NT, 128, 512], BF16, kind="Internal").ap()
    sbscr = nc.dram_tensor("sbscr", [NT, 128, 512], BF16, kind="Internal").ap()
    kscr = nc.dram_tensor("kscr", [NT, 128, 512], BF16, kind="Internal").ap()
    vscr = nc.dram_tensor("vscr", [NT, 128, 512], BF16, kind="Internal").ap()
    xres_v = xres.rearrange("(k p) t -> p k t", p=128)
    outT_v = outT.rearrange("(k p) t -> p k t", p=128)

    A = Arena(nc, 52000)
    banks = [nc.alloc_psum_tensor("bank%d" % i, [128, 512], F32) for i in range(8)]
    bankb = [Buf("bank%d" % i) for i in range(8)]

    def bk(i):
        return banks[i][:]

    def bkb16(i):
        return banks[i][:].bitcast(BF16)

    xseg_b = [Buf("xseg%d" % s) for s in range(17)]
    pscr_b = [Buf("pscr%d" % t) for t in range(NT)]
    sbscr_b = [Buf("sbscr%d" % t) for t in range(NT)]
    kscr_b = [Buf("kscr%d" % t) for t in range(NT)]
    vscr_b = [Buf("vscr%d" % t) for t in range(NT)]
    out_b = Buf("out")

    misc = A.f32(768).rearrange("p (a b) -> p a b", a=6)
    misc_b = Buf("misc")
    small = A.f32(264)
    small_b = Buf("small")
    vecs = A.f32(NVEC)
    vecs_b = Buf("vecs")
    ident_bf = A.bf16(128)
    ones_bf = A.bf16(128)
    cbf_b = Buf("cbf")
    mod = A.f32(DEPTH * 96).rearrange("p (l j s) -> p l j s", l=DEPTH, j=48)
    mod_b = Buf("mod")
    lg_all = A.f32(32)
    lg_b = Buf("lg")
    silc = A.f32(16)
    silc_b = Buf("silc")
    ident_f = misc[:, 0, :]
    ones_f = misc[:, 1, :]
    Epos, Eneg, Mf, Mb = misc[:, 2, :], misc[:, 3, :], misc[:, 4, :], misc[:, 5, :]
    eps_n = small[:, 258:259]
    eps_g = small[:, 259:260]

    S.dma("sp", lambda e: e.dma_start(out=misc.rearrange("p a b -> p (a b)"), in_=misc_in[:, :]), writes=[misc_b])
    S.dma("sp", lambda e: e.dma_start(out=small, in_=small_in[:, :]), writes=[small_b])
    S.dma("sp", lambda e: e.dma_start(out=vecs, in_=vecs_in[:, :]), writes=[vecs_b])
    S.dma("sp", lambda e: e.dma_start(out=silc, in_=cc_in[:, :]), writes=[silc_b])
    S.dma("sp", lambda e: e.dma_start(out=lg_all, in_=dlog_in.partition_broadcast(128).rearrange("p a b -> p (a b)")
                                      if len(dlog_in.partition_broadcast(128).shape) == 3 else dlog_in.partition_broadcast(128)),
          writes=[lg_b])
    S.op("dve", lambda e: e.tensor_copy(out=ident_bf, in_=ident_f), reads=[misc_b], writes=[cbf_b])
    S.op("dve", lambda e: e.tensor_copy(out=ones_bf, in_=ones_f), reads=[misc_b], writes=[cbf_b])
    for s in range(17):
        S.dma("sp", lambda e, s=s: e.dma_start(out=xres[:, s * 256:(s + 1) * 256], in_=xT_in[:, s * 256:(s + 1) * 256]),
              writes=[xseg_b[s]])

    S.op("act", lambda e: e.activation(out=lg_all, in_=lg_all, func=AF.Exp, scale=-1.0), reads=[lg_b], writes=[lg_b])
    S.op("dve", lambda e: e.tensor_scalar(out=lg_all, in0=lg_all, scalar1=1.0, scalar2=None, op0=ALU.add),
         reads=[lg_b], writes=[lg_b])
    S.op("act", lambda e: e.activation(out=lg_all, in_=lg_all, func=AF.Ln), reads=[lg_b], writes=[lg_b])
    S.op("dve", lambda e: e.tensor_scalar(out=lg_all, in0=lg_all, scalar1=-1.0, scalar2=None, op0=ALU.mult),
         reads=[lg_b], writes=[lg_b])
    S.op("act", lambda e: e.activation(out=silc, in_=silc, func=AF.Silu), reads=[silc_b], writes=[silc_b])

    A.mark()
    wa_st = [A.f32(8 * 1024).rearrange("p (k n) -> p k n", k=8) for _ in range(2)]
    wa_b = [Buf("wa0"), Buf("wa1")]
    silc_v = silc.rearrange("p (s k) -> p k s", s=2)
    it = 0
    for l in range(n_layers):
        for j6 in range(6):
            i = it % 2
            it += 1
            for half in range(2):
                S.dma("sp", lambda e, i=i, l=l, j6=j6, half=half: e.dma_start(
                    out=wa_st[i][:, half * 4:(half + 1) * 4, :],
                    in_=w_ada[l, half * 512:(half + 1) * 512, j6 * 1024:(j6 + 1) * 1024].rearrange("(k p) n -> p k n", p=128)),
                    writes=[wa_b[i]])
            pb = j6 % 2
            for n in range(8):
                for k in range(8):
                    S.op("pe", lambda e, i=i, n=n, k=k, pb=pb: e.matmul(
                        bk(pb)[:, n * 2:n * 2 + 2], lhsT=wa_st[i][:, k, n * 128:(n + 1) * 128], rhs=silc_v[:, k, :],
                        start=(k == 0), stop=(k == 7)),
                        reads=[wa_b[i], silc_b], writes=[bankb[pb]], signal=(k == 7))
            bofs = l * VEC_PER_LAYER + 24 + j6 * 8
            S.op("dve", lambda e, l=l, j6=j6, pb=pb, bofs=bofs: e.tensor_tensor(
                out=mod[:, l, j6 * 8:(j6 + 1) * 8, :],
                in0=bk(pb)[:, 0:16].rearrange("p (n s) -> p n s", s=2),
                in1=vecs[:, bofs:bofs + 8].unsqueeze(2).broadcast_to([128, 8, 2]), op=ALU.add),
                reads=[bankb[pb], vecs_b], writes=[mod_b])
    S.barrier()
    A.release()

    ctx = dict(nc=nc, S=S, A=A, bk=bk, bkb16=bkb16, bankb=bankb, xres_v=xres_v, xseg_b=xseg_b,
               pscr=pscr, pscr_b=pscr_b, sbscr=sbscr, sbscr_b=sbscr_b, kscr=kscr, kscr_b=kscr_b, vscr=vscr, vscr_b=vscr_b, misc=misc, misc_b=misc_b, small=small,
               small_b=small_b, vecs=vecs, vecs_b=vecs_b, ident_bf=ident_bf, ones_bf=ones_bf, cbf_b=cbf_b,
               mod=mod, mod_b=mod_b, lg_all=lg_all, lg_b=lg_b, ident_f=ident_f, ones_f=ones_f,
               Epos=Epos, Eneg=Eneg, Mf=Mf, Mb=Mb, eps_n=eps_n, eps_g=eps_g,
               w_in=w_in, w_out=w_out, pool_w=pool_w, blocks_in=blocks_in, cos_in=cos_in, sin_in=sin_in,
               ffn_w13=ffn_w13, ffn_w2=ffn_w2, router_w=router_w, moe_w13=moe_w13, moe_w2=moe_w2,
               plan=plan, nblk=nblk)

    for l in range(n_layers):
        emit_mixer(ctx, l)
        if stop_after_mix and l == n_layers - 1:
            break
        emit_ffn(ctx, l)

    emit_final(ctx, vecs[:, VEC_PER_LAYER * DEPTH:VEC_PER_LAYER * DEPTH + 8], outT_v, out_b,
               raw=(n_layers < DEPTH or stop_after_mix))
    S.barrier()
    S.emit()
    return nc


def emit_norm_seg(ctx, s, xq, xq_b, gvec, svec, hT_out, hT_b, nb, h32_out=None, h32_b=None, statbank=7):
    S, bk, bankb = ctx["S"], ctx["bk"], ctx["bankb"]
    sq, sq_b, rstd, rstd_b, tmp, tmp_b = nb
    S.op("act", lambda e: e.activation(out=sq, in_=xq, func=AF.Square), reads=[xq_b], writes=[sq_b])
    for k in range(8):
        S.op("pe", lambda e, k=k: e.matmul(bk(statbank)[:, 0:256], lhsT=ctx["ones_bf"], rhs=sq[:, k, :],
                                           start=(k == 0), stop=(k == 7)),
             reads=[sq_b, ctx["cbf_b"]], writes=[bankb[statbank]], signal=(k == 7))
    S.op("act", lambda e: e.activation(out=rstd, in_=bk(statbank)[:, 0:256], func=AF.Ln, bias=ctx["eps_n"], scale=1.0 / D),
         reads=[bankb[statbank], ctx["small_b"]], writes=[rstd_b])
    S.op("act", lambda e: e.activation(out=rstd, in_=rstd, func=AF.Exp, scale=-0.5), reads=[rstd_b], writes=[rstd_b])
    dst, dst_b = (hT_out, hT_b) if h32_out is None else (h32_out, h32_b)
    for k in range(8):
        i = k % 2
        S.op("dve", lambda e, k=k, i=i: e.tensor_tensor(out=tmp[i], in0=xq[:, k, :], in1=rstd, op=ALU.mult),
             reads=[xq_b, rstd_b], writes=[tmp_b[i]])
        if svec is not None:
            S.op("act", lambda e, k=k, i=i: e.activation(out=dst[:, k, :], in_=tmp[i], func=AF.Identity,
                                                         bias=svec[:, k:k + 1], scale=gvec[:, k:k + 1]),
                 reads=[tmp_b[i], ctx["mod_b"], ctx["vecs_b"]], writes=[dst_b])
        else:
            S.op("act", lambda e, k=k, i=i: e.activation(out=dst[:, k, :], in_=tmp[i], func=AF.Identity,
                                                         scale=gvec[:, k:k + 1]),
                 reads=[tmp_b[i], ctx["mod_b"], ctx["vecs_b"]], writes=[dst_b])
    if h32_out is not None and hT_out is not None:
        S.op("pool", lambda e: e.tensor_copy(out=hT_out, in_=h32_out), reads=[h32_b], writes=[hT_b])


def emit_layer_vectors(ctx, l, which):
    S, A = ctx["S"], ctx["A"]
    mod, vecs = ctx["mod"], ctx["vecs"]
    G = A.f32(16).rearrange("p (k s) -> p k s", s=2)
    g_ofs = l * VEC_PER_LAYER + (0 if which == 1 else 8)
    j_sc = 8 if which == 1 else 32
    S.op("dve", lambda e: e.scalar_tensor_tensor(
        out=G, in0=mod[:, l, j_sc:j_sc + 8, :], scalar=1.0,
        in1=vecs[:, g_ofs:g_ofs + 8].unsqueeze(2).broadcast_to([128, 8, 2]), op0=ALU.add, op1=ALU.mult),
        reads=[ctx["mod_b"], ctx["vecs_b"]], writes=[ctx["mod_b"]])
    return G


def emit_rope(ctx, src_view, dst_view, cs, sn, nh, tmps, tmps_b, src_b, dst_b, tab_b):
    S = ctx["S"]
    u1, u2 = src_view[:, :, :, 0, :], src_view[:, :, :, 1, :]
    o1, o2 = dst_view[:, :, :, 0, :], dst_view[:, :, :, 1, :]
    cb = cs.unsqueeze(1).broadcast_to([128, nh, 2, 32])
    sb = sn.unsqueeze(1).broadcast_to([128, nh, 2, 32])
    t = [x[:, 0:nh * 64].rearrange("p (h a f) -> p h a f", h=nh, a=2) for x in tmps]
    S.op("dve", lambda e: e.tensor_tensor(out=t[0], in0=u1, in1=cb, op=ALU.mult), reads=[src_b, tab_b], writes=[tmps_b[0]])
    S.op("dve", lambda e: e.tensor_tensor(out=t[1], in0=u2, in1=sb, op=ALU.mult), reads=[src_b, tab_b], writes=[tmps_b[1]])
    S.op("dve", lambda e: e.tensor_tensor(out=t[2], in0=u1, in1=sb, op=ALU.mult), reads=[src_b, tab_b], writes=[tmps_b[2]])
    S.op("dve", lambda e: e.tensor_tensor(out=t[3], in0=u2, in1=cb, op=ALU.mult), reads=[src_b, tab_b], writes=[tmps_b[3]])
    S.op("pool", lambda e: e.tensor_tensor(out=o1, in0=t[0], in1=t[1], op=ALU.subtract),
         reads=[tmps_b[0], tmps_b[1]], writes=[dst_b])
    S.op("pool", lambda e: e.tensor_tensor(out=o2, in0=t[2], in1=t[3], op=ALU.add),
         reads=[tmps_b[2], tmps_b[3]], writes=[dst_b])


def emit_mixer(ctx, l):
    S, A, bk, bkb16, bankb = ctx["S"], ctx["A"], ctx["bk"], ctx["bkb16"], ctx["bankb"]
    mod, vecs, small, lg_all = ctx["mod"], ctx["vecs"], ctx["small"], ctx["lg_all"]
    plan = ctx["plan"]
    nblk = ctx["nblk"]
    last = (l == DEPTH - 1)
    S.barrier()
    A.mark()
    win = A.bf16(8 * 2560).rearrange("p (k n) -> p k n", k=8)
    win_b = Buf("win")
    wout = A.bf16(8 * 1024).rearrange("p (k n) -> p k n", k=8)
    wout_b = Buf("wout")
    poolw = A.bf16(4 * 128).rearrange("p (g n) -> p g n", g=4)
    poolw_b = Buf("poolw")
    blocks = A.bf16(nblk * 128).rearrange("p (b n) -> p b n", b=nblk)
    blocks_b = Buf("blocks")
    for k in range(8):
        for hf in range(2):
            S.dma("pool", lambda e, k=k, hf=hf: e.dma_start(
                out=win[:, k, hf * 1280:(hf + 1) * 1280], in_=ctx["w_in"][l, k * 128:(k + 1) * 128, hf * 1280:(hf + 1) * 1280]),
                writes=[win_b])
    S.dma("pool", lambda e: e.dma_start(out=wout, in_=ctx["w_out"][l].rearrange("(k p) n -> p k n", p=128)), writes=[wout_b])
    S.dma("pool", lambda e: e.dma_start(out=poolw, in_=ctx["pool_w"][l].rearrange("g c d -> c g d")), writes=[poolw_b])
    for b0 in range(0, nblk, 16):
        b1 = min(nblk, b0 + 16)
        S.dma("pool", lambda e, b0=b0, b1=b1: e.dma_start(
            out=blocks[:, b0:b1, :], in_=ctx["blocks_in"][b0:b1].rearrange("b p n -> p b n")), writes=[blocks_b])

    G1 = emit_layer_vectors(ctx, l, 1)
    S1 = mod[:, l, 0:8, :]
    GT1 = mod[:, l, 16:24, :]
    gn_ofs = l * VEC_PER_LAYER + 16
    rsc = vecs[:, gn_ofs:gn_ofs + 8]

    Dcomb = A.f32(512).rearrange("p (h n) -> p h n", h=4)
    decqf = A.f32(512).rearrange("p (h n) -> p h n", h=4)
    decqb = A.f32(512).rearrange("p (h n) -> p h n", h=4)
    dk = A.f32(16)
    dtmp = [A.f32(128), A.f32(128)]
    dec_b = Buf("dec")
    dtmp_b = [Buf("dtmp0"), Buf("dtmp1")]
    lgf = lg_all[:, l * 8:l * 8 + 4]
    lgb = lg_all[:, l * 8 + 4:l * 8 + 8]
    rd = [ctx["lg_b"], ctx["misc_b"], ctx["small_b"]]
    for h in range(4):
        S.op("act", lambda e, h=h: e.activation(out=dtmp[0], in_=ctx["Epos"], func=AF.Exp, scale=lgf[:, h:h + 1]),
             reads=rd, writes=[dtmp_b[0]])
        S.op("dve", lambda e, h=h: e.scalar_tensor_tensor(out=Dcomb[:, h, :], in0=dtmp[0], scalar=SCALE, in1=ctx["Mf"],
                                                          op0=ALU.mult, op1=ALU.mult),
             reads=[dtmp_b[0]] + rd, writes=[dec_b])
        S.op("act", lambda e, h=h: e.activation(out=dtmp[1], in_=ctx["Eneg"], func=AF.Exp, scale=lgb[:, h:h + 1]),
             reads=rd, writes=[dtmp_b[1]])
        S.op("dve", lambda e, h=h: e.scalar_tensor_tensor(out=dtmp[1], in0=dtmp[1], scalar=SCALE, in1=ctx["Mb"],
                                                          op0=ALU.mult, op1=ALU.mult),
             reads=[dtmp_b[1]] + rd, writes=[dtmp_b[1]])
        S.op("dve", lambda e, h=h: e.tensor_tensor(out=Dcomb[:, h, :], in0=Dcomb[:, h, :], in1=dtmp[1], op=ALU.add),
             reads=[dtmp_b[1], dec_b], writes=[dec_b])
        S.op("act", lambda e, h=h: e.activation(out=decqf[:, h, :], in_=small[:, 0:128], func=AF.Exp, scale=lgf[:, h:h + 1]),
             reads=rd, writes=[dec_b])
        S.op("act", lambda e, h=h: e.activation(out=decqb[:, h, :], in_=small[:, 128:256], func=AF.Exp, scale=lgb[:, h:h + 1]),
             reads=rd, writes=[dec_b])
    S.op("dve", lambda e: e.tensor_scalar(out=dk[:, 0:4], in0=lgf, scalar1=small[:, 256:257], scalar2=None, op0=ALU.mult),
         reads=rd, writes=[dec_b])
    S.op("dve", lambda e: e.tensor_scalar(out=dk[:, 4:8], in0=lgb, scalar1=small[:, 257:258], scalar2=None, op0=ALU.mult),
         reads=rd, writes=[dec_b])
    S.op("dve", lambda e: e.tensor_scalar(out=dk[:, 8:12], in0=lgf, scalar1=128.0, scalar2=None, op0=ALU.mult),
         reads=rd, writes=[dec_b])
    S.op("dve", lambda e: e.tensor_scalar(out=dk[:, 12:16], in0=lgb, scalar1=128.0, scalar2=None, op0=ALU.mult),
         reads=rd, writes=[dec_b])
    S.op("act", lambda e: e.activation(out=dk, in_=dk, func=AF.Exp), reads=[dec_b], writes=[dec_b])
    S.op("dve", lambda e: e.tensor_scalar(out=dk[:, 0:8], in0=dk[:, 0:8], scalar1=SCALE, scalar2=None, op0=ALU.mult),
         reads=[dec_b], writes=[dec_b])
    dkf, dkb, gcf, gcb = dk[:, 0:4], dk[:, 4:8], dk[:, 8:12], dk[:, 12:16]

    xq = [A.f32(8 * 256).rearrange("p (k n) -> p k n", k=8) for _ in range(3)]
    xq_b = [Buf("xq%d" % i) for i in range(3)]
    sq = A.bf16(8 * 256).rearrange("p (k n) -> p k n", k=8)
    nb = (sq, Buf("sq"), A.f32(256), Buf("rstd"), [A.f32(256), A.f32(256)], [Buf("tmp0"), Buf("tmp1")])
    hT = [A.bf16(8 * 256).rearrange("p (k n) -> p k n", k=8) for _ in range(2)]
    hT_b = [Buf("hT0"), Buf("hT1")]
    cst = [A.f32(128).rearrange("p (j a f) -> p j a f", j=2, a=2) for _ in range(3)]
    snt = [A.f32(128).rearrange("p (j a f) -> p j a f", j=2, a=2) for _ in range(3)]
    tab_b = [Buf("tab0"), Buf("tab1"), Buf("tab2")]
    rtmp = [A.f32(512) for _ in range(4)]
    rtmp_b = [Buf("rt%d" % i) for i in range(4)]
    P2 = range(2)
    qk_tm = [A.bf16(1024) for _ in P2]
    qk_b = [Buf("qk_tm%d" % i) for i in P2]
    v_tm = [A.bf16(512) for _ in P2]
    v_b = [Buf("v_tm%d" % i) for i in P2]
    sg = [A.f32(512) for _ in P2]
    sg_b = [Buf("sg%d" % i) for i in P2]
    kT = [A.bf16(512).rearrange("p (h n) -> p h n", h=4) for _ in P2]
    qT = [A.bf16(512).rearrange("p (h n) -> p h n", h=4) for _ in P2]
    qfT = [A.bf16(512).rearrange("p (h n) -> p h n", h=4) for _ in P2]
    qbT = [A.bf16(512).rearrange("p (h n) -> p h n", h=4) for _ in P2]
    qkT_b = [Buf("qkT%d" % i) for i in P2]
    ktil = [A.bf16(512).rearrange("p (h n) -> p h n", h=4) for _ in P2]
    ktil_b = [Buf("ktil%d" % i) for i in P2]
    pst = [A.bf16(512) for _ in P2]
    pst_b = [Buf("pst%d" % i) for i in P2]
    sbst = [A.bf16(512) for _ in P2]
    sbst_b = [Buf("sbst%d" % i) for i in P2]
    kt3 = [pst[0], pst[1], sbst[0]]
    kt3_b = [pst_b[0], pst_b[1], sbst_b[0]]
    vt3 = [sbst[1], A.bf16(512), A.bf16(512)]
    vt3_b = [sbst_b[1], Buf("vt3_1"), Buf("vt3_2")]
    PT = A.bf16(512).rearrange("p (h n) -> p h n", h=4)
    PT_b = Buf("PT")
    St = A.f32(512).rearrange("p (h n) -> p h n", h=4)
    St_bf = A.bf16(512).rearrange("p (h n) -> p h n", h=4)
    St_b = Buf("St")
    Stbf_b = Buf("Stbf")
    pslot = [A.bf16(512) for _ in range(12)]
    pslot_b = [Buf("pslot%d" % i) for i in range(12)]
    sbslot = [A.bf16(512).rearrange("p (h n) -> p h n", h=4) for _ in range(4)]
    sbslot_b = [Buf("sbslot%d" % i) for i in range(4)]
    stats = A.f32(24).rearrange("p (h s) -> p h s", h=4)
    mv = A.f32(8).rearrange("p (h s) -> p h s", h=4)
    rs4 = A.f32(4)
    gn_b = Buf("gn")
    on = A.f32(512).rearrange("p (h n) -> p h n", h=4)
    on_b = Buf("on")
    ret_tm = A.bf16(512)
    ret_b = Buf("ret_tm")
    mixT = [A.bf16(1024).rearrange("p (k n) -> p k n", k=8) for _ in P2]
    mixr_b = [Buf("mixr%d" % i) for i in P2]
    mixp_b = [Buf("mixp%d" % i) for i in P2]
    dT = A.bf16(512).rearrange("p (g n) -> p g n", g=4)
    dT_b = Buf("dT")
    xnew = [A.f32(1024).rearrange("p (k n) -> p k n", k=8) for _ in P2]
    xnew_b = [Buf("xnew%d" % i) for i in P2]
    cos_v = ctx["cos_in"].rearrange("p (t a f) -> p t a f", t=NT, a=2)
    sin_v = ctx["sin_in"].rearrange("p (t a f) -> p t a f", t=NT, a=2)
    STATB = 3

    def load_seg(s, n):
        i3 = n % 3
        S.dma("sp", lambda e: e.dma_start(out=xq[i3], in_=ctx["xres_v"][:, :, s * 256:(s + 1) * 256]),
              reads=[ctx["xseg_b"][s]], writes=[xq_b[i3]])
        S.dma("sp", lambda e: e.dma_start(out=cst[i3], in_=cos_v[:, 2 * s:2 * s + 2]), writes=[tab_b[i3]])
        S.dma("sp", lambda e: e.dma_start(out=snt[i3], in_=sin_v[:, 2 * s:2 * s + 2]), writes=[tab_b[i3]])

    def norm_seg(s, n):
        sidx = 1 if s == 0 else 0
        emit_norm_seg(ctx, s, xq[n % 3], xq_b[n % 3], G1[:, :, sidx], S1[:, :, sidx], hT[n % 2], hT_b[n % 2], nb, statbank=STATB)

    def project(n, tl, col0, ncols, bank0):
        h_, hb_ = hT[n % 2], hT_b[n % 2]
        for c in range(ncols // 512):
            for k in range(8):
                S.op("pe", lambda e, c=c, k=k: e.matmul(
                    bk(bank0 + c), lhsT=h_[:, k, tl * 128:(tl + 1) * 128], rhs=win[:, k, col0 + c * 512:col0 + (c + 1) * 512],
                    start=(k == 0), stop=(k == 7)),
                    reads=[hb_, win_b], writes=[bankb[bank0 + c]], signal=(k == 7))

    def make_ktil(p, dkv, ksrc=None, ksrc_b=None):
        if ksrc is None:
            ksrc, ksrc_b = qk_tm[p][:, 512:1024], qk_b[p]
        kview = ksrc.rearrange("p (h n) -> p h n", h=4)
        S.op("pool", lambda e: e.tensor_tensor(out=ktil[p], in0=kview, in1=dkv.unsqueeze(2).broadcast_to([128, 4, 128]), op=ALU.mult),
             reads=[ksrc_b, dec_b], writes=[ktil_b[p]])

    def state_update(p, gcv, vsrc=None, vsrc_b=None):
        if vsrc is None:
            vsrc, vsrc_b = v_tm[p], v_b[p]
        for h in range(4):
            S.op("pe", lambda e, h=h: e.matmul(bk(7)[:, h * 128:(h + 1) * 128], lhsT=ktil[p][:, h, :], rhs=vsrc[:, h * 128:(h + 1) * 128],
                                               start=True, stop=True),
                 reads=[ktil_b[p], vsrc_b], writes=[bankb[7]], signal=(h == 3))
        S.op("pool", lambda e: e.tensor_tensor(out=St, in0=St, in1=gcv.unsqueeze(2).broadcast_to([128, 4, 128]), op=ALU.mult),
             reads=[St_b, dec_b], writes=[St_b])
        S.op("dve", lambda e: e.tensor_tensor(out=St, in0=St, in1=bk(7).rearrange("p (h n) -> p h n", h=4), op=ALU.add),
             reads=[St_b, bankb[7]], writes=[St_b])
        S.op("act", lambda e: e.copy(out=St_bf, in_=St), reads=[St_b], writes=[Stbf_b])

    def zero_state():
        S.op("pool", lambda e: e.memset(St.rearrange("p h n -> p (h n)"), 0.0), writes=[St_b])
        S.op("pool", lambda e: e.memset(St_bf.rearrange("p h n -> p (h n)"), 0.0), writes=[Stbf_b])

    zero_state()
    seg_order = [0] + list(range(16, 0, -1))
    tiles = []
    for n, s in enumerate(seg_order):
        for tl in (1, 0):
            tiles.append((2 * s + tl, n, s, tl))

    def pre_front(j):
        t, n, s, tl = tiles[j]
        p = j % 2
        project(n, tl, 512, 512, 0)
        project(n, tl, 1024, 512, 2)
        project(n, tl, 2048, 512, 3)
        yield
        kv5 = bk(0).rearrange("p (h a b f) -> p h a b f", h=4, a=2, b=2)
        kd5 = qk_tm[p][:, 512:1024].rearrange("p (h a b f) -> p h a b f", h=4, a=2, b=2)
        emit_rope(ctx, kv5, kd5, cst[n % 3][:, tl], snt[n % 3][:, tl], 4, rtmp, rtmp_b, bankb[0], qk_b[p], tab_b[n % 3])
        S.op("act", lambda e: e.copy(out=v_tm[p], in_=bk(2)), reads=[bankb[2]], writes=[v_b[p]])
        S.op("act", lambda e: e.copy(out=pst[p], in_=bk(3)), reads=[bankb[3]], writes=[pst_b[p]])
        S.dma("pool", lambda e: e.dma_start(out=ctx["pscr"][t], in_=pst[p]), reads=[pst_b[p]], writes=[ctx["pscr_b"][t]])
        S.dma("pool", lambda e: e.dma_start(out=ctx["vscr"][t], in_=v_tm[p]), reads=[v_b[p]], writes=[ctx["vscr_b"][t]])
        S.dma("pool", lambda e: e.dma_start(out=ctx["kscr"][t], in_=qk_tm[p][:, 512:1024]), reads=[qk_b[p]], writes=[ctx["kscr_b"][t]])
        make_ktil(p, dkb)
        if tl == 0:
            if n + 2 < len(seg_order):
                load_seg(seg_order[n + 2], n + 2)
            if n + 1 < len(seg_order):
                norm_seg(seg_order[n + 1], n + 1)

    def pre_back(j):
        t, n, s, tl = tiles[j]
        p = j % 2
        S.op("act", lambda e: e.copy(out=sbst[p], in_=St_bf.rearrange("p h n -> p (h n)")), reads=[Stbf_b], writes=[sbst_b[p]])
        S.dma("pool", lambda e: e.dma_start(out=ctx["sbscr"][t], in_=sbst[p]), reads=[sbst_b[p]], writes=[ctx["sbscr_b"][t]])
        state_update(p, gcb)
        yield

    def interleave(gens):
        active = list(gens)
        while active:
            for g_ in list(active):
                try:
                    next(g_)
                except StopIteration:
                    active.remove(g_)

    load_seg(seg_order[0], 0)
    load_seg(seg_order[1], 1)
    norm_seg(seg_order[0], 0)
    interleave([pre_front(0)])
    for j in range(len(tiles)):
        gens = [pre_back(j)]
        if j + 1 < len(tiles):
            gens.append(pre_front(j + 1))
        interleave(gens)

    zero_state()

    def p_needed(t):
        r = set()
        for g in range(4):
            for (ti, _) in plan[g][t]:
                r.add(ti)
        return r

    loaded_p = set()

    def prefetch_p(t):
        if t in loaded_p or t >= NT:
            return
        loaded_p.add(t)
        S.dma("sp", lambda e: e.dma_start(out=pslot[t % 12], in_=ctx["pscr"][t]), reads=[ctx["pscr_b"][t]], writes=[pslot_b[t % 12]])

    def prefetch_sb(t):
        if t < NT:
            S.dma("sp", lambda e: e.dma_start(out=sbslot[t % 4].rearrange("p h n -> p (h n)"), in_=ctx["sbscr"][t]),
                  reads=[ctx["sbscr_b"][t]], writes=[sbslot_b[t % 4]])

    def main_front(t):
        s, tl = t // 2, t % 2
        n = s
        p = t % 2
        prefetch_sb(t + 2)
        for tt in sorted(p_needed(t) | (p_needed(t + 1) if t + 1 < NT else set())):
            prefetch_p(tt)
        k3, k3b, v3, v3b = kt3[t % 3], kt3_b[t % 3], vt3[t % 3], vt3_b[t % 3]
        if t + 1 < NT:
            S.dma("sp", lambda e: e.dma_start(out=kt3[(t + 1) % 3], in_=ctx["kscr"][t + 1]), reads=[ctx["kscr_b"][t + 1]], writes=[kt3_b[(t + 1) % 3]])
            S.dma("sp", lambda e: e.dma_start(out=vt3[(t + 1) % 3], in_=ctx["vscr"][t + 1]), reads=[ctx["vscr_b"][t + 1]], writes=[vt3_b[(t + 1) % 3]])
        project(n, tl, 0, 512, 0)
        project(n, tl, 1536, 512, 3)
        yield
        sv = bk(0).rearrange("p (h a b f) -> p h a b f", h=4, a=2, b=2)
        dv = qk_tm[p][:, 0:512].rearrange("p (h a b f) -> p h a b f", h=4, a=2, b=2)
        emit_rope(ctx, sv, dv, cst[n % 3][:, tl], snt[n % 3][:, tl], 4, rtmp, rtmp_b, bankb[0], qk_b[p], tab_b[n % 3])
        S.op("act", lambda e: e.activation(out=sg[p], in_=bk(3), func=AF.Silu), reads=[bankb[3]], writes=[sg_b[p]])
        make_ktil(p, dkf, k3, k3b)
        yield
        if tl == 1:
            if s + 2 < 17:
                load_seg(s + 2, s + 2)
            if s + 1 < 17:
                norm_seg(s + 1, s + 1)
        b4 = bkb16(4).rearrange("p (j n) -> p j n", j=8)
        for j in range(8):
            src_j = qk_tm[p][:, j * 128:(j + 1) * 128] if j < 4 else k3[:, (j - 4) * 128:(j - 3) * 128]
            S.op("pe", lambda e, j=j, src_j=src_j: e.transpose(out=b4[:, j, :], in_=src_j, identity=ctx["ident_bf"]),
                 reads=[qk_b[p], k3b, ctx["cbf_b"]], writes=[bankb[4]], signal=(j == 7))
        S.op("act", lambda e: e.copy(out=kT[p], in_=b4[:, 4:8, :]), reads=[bankb[4]], writes=[qkT_b[p]])
        S.op("act", lambda e: e.copy(out=qT[p], in_=b4[:, 0:4, :]), reads=[bankb[4]], writes=[qkT_b[p]])
        S.op("dve", lambda e: e.tensor_tensor(out=qfT[p], in0=b4[:, 0:4, :], in1=decqf, op=ALU.mult),
             reads=[bankb[4], dec_b], writes=[qkT_b[p]])
        S.op("dve", lambda e: e.tensor_tensor(out=qbT[p], in0=b4[:, 0:4, :], in1=decqb, op=ALU.mult),
             reads=[bankb[4], dec_b], writes=[qkT_b[p]])
        yield
        for g in range(4):
            lst = plan[g][t]
            for n_i, (ti, bi) in enumerate(lst):
                S.op("pe", lambda e, g=g, ti=ti, bi=bi, n_i=n_i, L=len(lst): e.matmul(
                    bk(0)[:, g * 128:(g + 1) * 128], lhsT=pslot[ti % 12][:, g * 128:(g + 1) * 128], rhs=blocks[:, bi, :],
                    start=(n_i == 0), stop=(n_i == L - 1)),
                    reads=[pslot_b[ti % 12], blocks_b], writes=[bankb[0]], signal=(g == 3 and n_i == len(lst) - 1))
        S.op("act", lambda e: e.copy(out=dT, in_=bk(0).rearrange("p (g n) -> p g n", g=4)), reads=[bankb[0]], writes=[dT_b])
        yield
        for g in range(4):
            S.op("pe", lambda e, g=g: e.matmul(bk(1)[:, g * 128:(g + 1) * 128], lhsT=poolw[:, g, :], rhs=dT[:, g, :], start=True, stop=True),
                 reads=[poolw_b, dT_b], writes=[bankb[1]], signal=(g == 3))
        S.op("dve", lambda e: e.tensor_tensor(out=mixT[p][:, 4:8, :], in0=bk(1).rearrange("p (g n) -> p g n", g=4),
                                              in1=rsc[:, 4:8].unsqueeze(2).broadcast_to([128, 4, 128]), op=ALU.mult),
             reads=[bankb[1], ctx["vecs_b"]], writes=[mixp_b[p]])

    def main_back(t):
        s, tl = t // 2, t % 2
        p = t % 2
        sidx = 1 if s == 0 else 0
        xi = s % 3
        for h in range(4):
            S.op("pe", lambda e, h=h: e.matmul(bk(5)[:, h * 128:(h + 1) * 128], lhsT=kT[p][:, h, :], rhs=qT[p][:, h, :], start=True, stop=True),
                 reads=[qkT_b[p]], writes=[bankb[5]], signal=(h == 3))
        S.op("dve", lambda e: e.tensor_tensor(out=PT, in0=bk(5).rearrange("p (h n) -> p h n", h=4), in1=Dcomb, op=ALU.mult),
             reads=[bankb[5], dec_b], writes=[PT_b])
        yield
        sbs = sbslot[t % 4]
        for h in range(4):
            o_h = bk(6)[:, h * 128:(h + 1) * 128]
            S.op("pe", lambda e, h=h, o_h=o_h: e.matmul(o_h, lhsT=PT[:, h, :], rhs=vt3[t % 3][:, h * 128:(h + 1) * 128], start=True, stop=False),
                 reads=[PT_b, vt3_b[t % 3]], writes=[bankb[6]], signal=False)
            S.op("pe", lambda e, h=h, o_h=o_h: e.matmul(o_h, lhsT=qfT[p][:, h, :], rhs=St_bf[:, h, :], start=False, stop=False),
                 reads=[qkT_b[p], Stbf_b], writes=[bankb[6]], signal=False)
            S.op("pe", lambda e, h=h, o_h=o_h: e.matmul(o_h, lhsT=qbT[p][:, h, :], rhs=sbs[:, h, :], start=False, stop=True),
                 reads=[qkT_b[p], sbslot_b[t % 4]], writes=[bankb[6]], signal=(h == 3))
        state_update(p, gcf, vt3[t % 3], vt3_b[t % 3])
        yield
        if last and s == 0:
            return
        o3 = bk(6).rearrange("p (h n) -> p h n", h=4)
        for h in range(4):
            S.op("dve", lambda e, h=h: e.bn_stats(out=stats[:, h, :], in_=o3[:, h, :]), reads=[bankb[6]], writes=[gn_b])
        for h in range(4):
            S.op("dve", lambda e, h=h: e.bn_aggr(out=mv[:, h, :], in_=stats[:, h, :]), reads=[gn_b], writes=[gn_b])
        S.op("dve", lambda e: e.tensor_scalar(out=rs4, in0=mv[:, :, 1], scalar1=GN_EPS, scalar2=None, op0=ALU.add),
             reads=[gn_b], writes=[gn_b])
        S.op("pool", lambda e: e.tensor_tensor(out=rs4, in0=rs4, in1=ctx["small"][:, 262:263].broadcast_to([128, 4]), op=ALU.pow),
             reads=[gn_b, ctx["small_b"]], writes=[gn_b])
        for h in range(4):
            S.op("dve", lambda e, h=h: e.tensor_scalar(out=on[:, h, :], in0=o3[:, h, :], scalar1=mv[:, h, 0:1], scalar2=rs4[:, h:h + 1],
                                                       op0=ALU.subtract, op1=ALU.mult),
                 reads=[bankb[6], gn_b], writes=[on_b])
        S.op("pool", lambda e: e.tensor_tensor(out=ret_tm, in0=on.rearrange("p h n -> p (h n)"), in1=sg[p], op=ALU.mult),
             reads=[on_b, sg_b[p]], writes=[ret_b])
        yield
        b7r = bkb16(7)[:, 0:512].rearrange("p (j n) -> p j n", j=4)
        for h in range(4):
            S.op("pe", lambda e, h=h: e.transpose(out=b7r[:, h, :], in_=ret_tm[:, h * 128:(h + 1) * 128], identity=ctx["ident_bf"]),
                 reads=[ret_b, ctx["cbf_b"]], writes=[bankb[7]], signal=(h == 3))
        S.op("dve", lambda e: e.tensor_tensor(out=mixT[p][:, 0:4, :], in0=b7r, in1=rsc[:, 0:4].unsqueeze(2).broadcast_to([128, 4, 128]), op=ALU.mult),
             reads=[bankb[7], ctx["vecs_b"]], writes=[mixr_b[p]])
        yield
        for n_ in range(8):
            for k in range(8):
                S.op("pe", lambda e, n_=n_, k=k: e.matmul(
                    bk(5 + n_ // 4)[:, (n_ % 4) * 128:(n_ % 4 + 1) * 128], lhsT=wout[:, k, n_ * 128:(n_ + 1) * 128], rhs=mixT[p][:, k, :],
                    start=(k == 0), stop=(k == 7)),
                    reads=[wout_b, mixr_b[p], mixp_b[p]], writes=[bankb[5 + n_ // 4]], signal=(k == 7 and n_ % 4 == 3))
        if not (last and s == 0):
            for n_ in range(8):
                S.op("dve", lambda e, n_=n_: e.scalar_tensor_tensor(
                    out=xnew[p][:, n_, :], in0=bk(5 + n_ // 4)[:, (n_ % 4) * 128:(n_ % 4 + 1) * 128], scalar=GT1[:, n_, sidx:sidx + 1],
                    in1=xq[xi][:, n_, tl * 128:(tl + 1) * 128], op0=ALU.mult, op1=ALU.add),
                    reads=[bankb[5 + n_ // 4], xq_b[xi], ctx["mod_b"]], writes=[xnew_b[p]])
            S.dma("pool", lambda e: e.dma_start(out=ctx["xres_v"][:, :, t * 128:(t + 1) * 128], in_=xnew[p]),
                  reads=[xnew_b[p]], writes=[ctx["xseg_b"][s]])

    load_seg(0, 0)
    load_seg(1, 1)
    norm_seg(0, 0)
    S.dma("sp", lambda e: e.dma_start(out=kt3[0], in_=ctx["kscr"][0]), reads=[ctx["kscr_b"][0]], writes=[kt3_b[0]])
    S.dma("sp", lambda e: e.dma_start(out=vt3[0], in_=ctx["vscr"][0]), reads=[ctx["vscr_b"][0]], writes=[vt3_b[0]])
    prefetch_sb(0)
    prefetch_sb(1)
    for t0 in (0, 1):
        prefetch_p(t0)
    interleave([main_front(0)])
    for t in range(NT):
        gens = [main_back(t)]
        if t + 1 < NT:
            gens.append(main_front(t + 1))
        interleave(gens)
    S.barrier()
    A.release()


FFN_SEG_BLOCKS = [list(range(0, 5)), list(range(5, 9)), list(range(9, 13)), list(range(13, 17))]


def emit_ffn(ctx, l):
    S, A, bk, bankb = ctx["S"], ctx["A"], ctx["bk"], ctx["bankb"]
    mod, vecs = ctx["mod"], ctx["vecs"]
    is_moe = (l % 2 == 1)
    li = l // 2
    last = (l == DEPTH - 1)
    S.barrier()
    A.mark()
    G2 = emit_layer_vectors(ctx, l, 2)
    S2 = mod[:, l, 24:32, :]
    GT2 = mod[:, l, 40:48, :]
    TBMAX = 1280
    h2T = A.bf16(8 * TBMAX).rearrange("p (k n) -> p k n", k=8)
    h2T_b = Buf("h2T")
    abuf = A.bf16(12 * TBMAX).rearrange("p (c n) -> p c n", c=12)
    a_b = Buf("a")
    acc = A.f32(8 * TBMAX).rearrange("p (k n) -> p k n", k=8)
    acc_b = Buf("acc")
    wst = [A.bf16(2 * 8 * 512).rearrange("p (u k n) -> p u k n", u=2, k=8) for _ in range(2)]
    wst_b = [Buf("wst%d" % i) for i in range(2)]
    w2sb = A.bf16(12 * 1024).rearrange("p (c n) -> p c n", c=12)
    w2_b = Buf("w2sb")
    xq = [A.f32(8 * 256).rearrange("p (k n) -> p k n", k=8) for _ in range(2)]
    xq_b = [Buf("fxq0"), Buf("fxq1")]
    sq = A.bf16(8 * 256).rearrange("p (k n) -> p k n", k=8)
    nb = (sq, Buf("fsq"), A.f32(256), Buf("frstd"), [A.f32(256), A.f32(256)], [Buf("ftmp0"), Buf("ftmp1")])
    sgb = [A.f32(512), A.f32(512)]
    sgb_b = [Buf("sgb0"), Buf("sgb1")]
    xnew = A.f32(8 * 256).rearrange("p (k n) -> p k n", k=8)
    xnew_b = Buf("fxnew")
    if is_moe:
        h2f = xnew
        h2f_b = xnew_b
        rw = A.f32(64).rearrange("p (k e) -> p k e", k=8)
        rw_b = Buf("rw")
        Gt = [A.f32(10 * 8).rearrange("p (t e) -> p t e", t=10) for _ in range(2)]
        Gt_b = [Buf("Gt0"), Buf("Gt1")]
        gsm = A.f32(48)
        gsm_b = Buf("gsm")
        dg = [A.f32(128), A.f32(128)]
        dg_b = [Buf("dg0"), Buf("dg1")]
        gbc = A.f32(512)
        gbc_b = Buf("gbc")
        tmpg = [A.f32(512), A.f32(512)]
        tmpg_b = [Buf("tmpg0"), Buf("tmpg1")]
        S.dma("sp", lambda e: e.dma_start(out=rw, in_=ctx["router_w"][li].rearrange("(k p) e -> p k e", p=128)), writes=[rw_b])
        items = [(e_, hf) for e_ in range(NEXP) for hf in range(2)]
    else:
        items = [(None, 0), (None, 1)]

    def w13_of(e_):
        return ctx["moe_w13"][li, e_] if is_moe else ctx["ffn_w13"][li]

    def w2_of(e_):
        return ctx["moe_w2"][li, e_] if is_moe else ctx["ffn_w2"][li]

    ugrot = [0]
    w2rot = [0]
    strot = [0]

    blocks_list = []
    for segs in FFN_SEG_BLOCKS:
        if last and segs[0] == 0:
            segs = segs[1:]
        blocks_list.append(segs)

    def interleave(gens):
        active = list(gens)
        while active:
            for g_ in list(active):
                try:
                    next(g_)
                except StopIteration:
                    active.remove(g_)

    def norm_block(bi):
        segs = blocks_list[bi]
        for n, s in enumerate(segs):
            i = n % 2
            sidx = 1 if s == 0 else 0
            S.dma("sp", lambda e, s=s, i=i: e.dma_start(out=xq[i], in_=ctx["xres_v"][:, :, s * 256:(s + 1) * 256]),
                  reads=[ctx["xseg_b"][s]], writes=[xq_b[i]])
            c0 = (s - segs[0]) * 256
            if not is_moe:
                emit_norm_seg(ctx, s, xq[i], xq_b[i], G2[:, :, sidx], S2[:, :, sidx], h2T[:, :, c0:c0 + 256], h2T_b, nb)
                yield
                continue
            Gtb = Gt[bi % 2]
            emit_norm_seg(ctx, s, xq[i], xq_b[i], G2[:, :, sidx], S2[:, :, sidx], h2T[:, :, c0:c0 + 256], h2T_b, nb,
                          h32_out=h2f, h32_b=h2f_b)
            yield
            for tl in range(2):
                tb = (s - segs[0]) * 2 + tl
                lgp = bk(7)[:, 256 + tl * 8:256 + tl * 8 + 8]
                for k in range(8):
                    S.op("pe", lambda e, k=k, tl=tl, lgp=lgp: e.matmul(lgp, lhsT=h2f[:, k, tl * 128:(tl + 1) * 128], rhs=rw[:, k, :],
                                                                       start=(k == 0), stop=(k == 7)),
                         reads=[h2f_b, rw_b], writes=[bankb[7]], signal=(k == 7))
                lg8, mx, dlt, w1, w2_, g1 = gsm[:, 0:8], gsm[:, 8:16], gsm[:, 16:17], gsm[:, 17:18], gsm[:, 18:19], gsm[:, 24:32]
                S.op("act", lambda e, lgp=lgp: e.copy(out=lg8, in_=lgp), reads=[bankb[7]], writes=[gsm_b])
                yield
                S.op("dve", lambda e: e.max(out=mx, in_=lg8), reads=[gsm_b], writes=[gsm_b])
                S.op("dve", lambda e: e.tensor_tensor(out=dlt, in0=mx[:, 1:2], in1=mx[:, 0:1], op=ALU.subtract), reads=[gsm_b], writes=[gsm_b])
                S.op("act", lambda e: e.activation(out=dlt, in_=dlt, func=AF.Exp), reads=[gsm_b], writes=[gsm_b])
                S.op("dve", lambda e: e.tensor_scalar(out=w1, in0=dlt, scalar1=1.0, scalar2=None, op0=ALU.add), reads=[gsm_b], writes=[gsm_b])
                S.op("dve", lambda e: e.reciprocal(out=w1, in_=w1), reads=[gsm_b], writes=[gsm_b])
                S.op("dve", lambda e: e.tensor_tensor(out=w2_, in0=dlt, in1=w1, op=ALU.mult), reads=[gsm_b], writes=[gsm_b])
                S.op("dve", lambda e: e.tensor_scalar(out=g1, in0=lg8, scalar1=mx[:, 0:1], scalar2=w1, op0=ALU.is_equal, op1=ALU.mult),
                     reads=[gsm_b], writes=[gsm_b])
                S.op("dve", lambda e, tb=tb, Gtb=Gtb: e.tensor_scalar(out=Gtb[:, tb, :], in0=lg8, scalar1=mx[:, 1:2], scalar2=w2_, op0=ALU.is_equal, op1=ALU.mult),
                     reads=[gsm_b], writes=[Gt_b[bi % 2]])
                S.op("dve", lambda e, tb=tb, Gtb=Gtb: e.tensor_tensor(out=Gtb[:, tb, :], in0=Gtb[:, tb, :], in1=g1, op=ALU.add),
                     reads=[gsm_b, Gt_b[bi % 2]], writes=[Gt_b[bi % 2]])
                yield

    def w2_phase(bi, it_i, e_, nfc, cgs):
        for (cc0, cn) in cgs:
            if is_moe:
                ntl = cn // 128
                for tl in range(ntl):
                    tb = cc0 // 128 + tl
                    di = tl % 2
                    S.op("dve", lambda e, tb=tb, di=di: e.tensor_scalar(out=dg[di], in0=ctx["ident_f"], scalar1=Gt[bi % 2][:, tb, e_:e_ + 1],
                                                                        scalar2=None, op0=ALU.mult),
                         reads=[Gt_b[bi % 2], ctx["misc_b"]], writes=[dg_b[di]])
                    S.op("pe", lambda e, tl=tl, di=di: e.matmul(bk(7)[:, tl * 128:(tl + 1) * 128], lhsT=ctx["ones_f"], rhs=dg[di], start=True, stop=True),
                         reads=[dg_b[di], ctx["misc_b"]], writes=[bankb[7]], signal=True)
                S.op("act", lambda e, cn=cn: e.copy(out=gbc[:, 0:cn], in_=bk(7)[:, 0:cn]), reads=[bankb[7]], writes=[gbc_b])
            for n_ in range(8):
                ob = 4 + (w2rot[0] % 3)
                w2rot[0] += 1
                for c in range(nfc):
                    S.op("pe", lambda e, ob=ob, c=c, n_=n_, cc0=cc0, cn=cn: e.matmul(
                        bk(ob)[:, 0:cn], lhsT=w2sb[:, c, n_ * 128:(n_ + 1) * 128], rhs=abuf[:, c, cc0:cc0 + cn],
                        start=(c == 0), stop=(c == nfc - 1)),
                        reads=[w2_b, a_b], writes=[bankb[ob]], signal=(c == nfc - 1))
                dst = acc[:, n_, cc0:cc0 + cn]
                if not is_moe:
                    if it_i == 0:
                        S.op("act", lambda e, ob=ob, cn=cn, dst=dst: e.copy(out=dst, in_=bk(ob)[:, 0:cn]), reads=[bankb[ob]], writes=[acc_b])
                    else:
                        S.op("dve", lambda e, ob=ob, cn=cn, dst=dst: e.tensor_tensor(out=dst, in0=dst, in1=bk(ob)[:, 0:cn], op=ALU.add),
                             reads=[bankb[ob], acc_b], writes=[acc_b])
                else:
                    if it_i == 0:
                        S.op("dve", lambda e, ob=ob, cn=cn, dst=dst: e.tensor_tensor(out=dst, in0=bk(ob)[:, 0:cn], in1=gbc[:, 0:cn], op=ALU.mult),
                             reads=[bankb[ob], gbc_b], writes=[acc_b])
                    else:
                        tg = tmpg[w2rot[0] % 2]
                        tg_b = tmpg_b[w2rot[0] % 2]
                        S.op("dve", lambda e, ob=ob, cn=cn, tg=tg: e.tensor_tensor(out=tg[:, 0:cn], in0=bk(ob)[:, 0:cn], in1=gbc[:, 0:cn], op=ALU.mult),
                             reads=[bankb[ob], gbc_b], writes=[tg_b])
                        S.op("pool", lambda e, cn=cn, dst=dst, tg=tg: e.tensor_tensor(out=dst, in0=dst, in1=tg[:, 0:cn], op=ALU.add),
                             reads=[tg_b, acc_b], writes=[acc_b])
                yield

    interleave([norm_block(0)])
    for bi, segs in enumerate(blocks_list):
        TB = 256 * len(segs)
        if segs[0] == 0:
            cgs = [(0, 256), (256, 512), (768, 512)]
        else:
            cgs = [(0, 512), (512, 512)]

        flat = []
        for it_i, (e_, hf) in enumerate(items):
            fc0, nfc = F_HALVES[hf]
            for c4 in range(0, nfc, 4):
                flat.append((it_i, e_, hf, c4, min(4, nfc - c4)))

        def w13_dma(gi):
            it_i, e_, hf, c4, ng = flat[gi]
            fc0, nfc = F_HALVES[hf]
            w13 = w13_of(e_)
            si = gi % 2
            col_u = (fc0 + c4) * 128
            col_g = DFF + (fc0 + c4) * 128
            for uu, col in enumerate((col_u, col_g)):
                S.dma("pool", lambda e, si=si, uu=uu, col=col, w13=w13, ng=ng: e.dma_start(
                    out=wst[si][:, uu, :, 0:ng * 128], in_=w13[:, col:col + ng * 128].rearrange("(k p) n -> p k n", p=128)),
                    writes=[wst_b[si]])

        def w2_dma(it_i):
            e_, hf = items[it_i]
            fc0, nfc = F_HALVES[hf]
            w2 = w2_of(e_)
            for c2 in range(0, nfc, 2):
                S.dma("pool", lambda e, c2=c2, fc0=fc0, w2=w2: e.dma_start(
                    out=w2sb[:, c2:c2 + 2, :], in_=w2[(fc0 + c2) * 128:(fc0 + c2 + 2) * 128, :].rearrange("(c p) n -> p c n", p=128)),
                    writes=[w2_b])

        w13_dma(0)
        w2_dma(0)
        if len(flat) > 1:
            w13_dma(1)
        for gi, (it_i, e_, hf, c4, ng) in enumerate(flat):
            fc0, nfc = F_HALVES[hf]
            si = gi % 2
            for (cc0, cn) in cgs:
                for cl in range(ng):
                    pr = (ugrot[0] % 2) * 2
                    ugrot[0] += 1
                    for uu in range(2):
                        for k in range(8):
                            S.op("pe", lambda e, si=si, uu=uu, k=k, cl=cl, pr=pr, cc0=cc0, cn=cn: e.matmul(
                                bk(pr + uu)[:, 0:cn], lhsT=wst[si][:, uu, k, cl * 128:(cl + 1) * 128], rhs=h2T[:, k, cc0:cc0 + cn],
                                start=(k == 0), stop=(k == 7)),
                                reads=[wst_b[si], h2T_b], writes=[bankb[pr + uu]], signal=(k == 7))
                    sgi = (ugrot[0]) % 2
                    S.op("act", lambda e, pr=pr, cn=cn, sgi=sgi: e.activation(out=sgb[sgi][:, 0:cn], in_=bk(pr + 1)[:, 0:cn], func=AF.Silu),
                         reads=[bankb[pr + 1]], writes=[sgb_b[sgi]])
                    S.op("dve", lambda e, pr=pr, cn=cn, sgi=sgi, c4=c4, cl=cl, cc0=cc0: e.tensor_tensor(
                        out=abuf[:, c4 + cl, cc0:cc0 + cn], in0=bk(pr)[:, 0:cn], in1=sgb[sgi][:, 0:cn], op=ALU.mult),
                        reads=[bankb[pr], sgb_b[sgi]], writes=[a_b])
            if gi + 2 < len(flat):
                w13_dma(gi + 2)
            last_of_item = (gi + 1 == len(flat)) or (flat[gi + 1][0] != it_i)
            if not last_of_item:
                continue
            gens = [w2_phase(bi, it_i, e_, nfc, cgs)]
            if it_i + 1 == len(items) and bi + 1 < len(blocks_list):
                gens.append(norm_block(bi + 1))
            interleave(gens)
            if it_i + 1 < len(items):
                w2_dma(it_i + 1)
        for n, s in enumerate(segs):
            if last and s == 0:
                continue
            i = n % 2
            sidx = 1 if s == 0 else 0
            c0 = (s - segs[0]) * 256
            S.dma("sp", lambda e, s=s, i=i: e.dma_start(out=xq[i], in_=ctx["xres_v"][:, :, s * 256:(s + 1) * 256]),
                  reads=[ctx["xseg_b"][s]], writes=[xq_b[i]])
            for n_ in range(8):
                S.op("dve", lambda e, n_=n_, i=i, c0=c0, sidx=sidx: e.scalar_tensor_tensor(
                    out=xnew[:, n_, :], in0=acc[:, n_, c0:c0 + 256], scalar=GT2[:, n_, sidx:sidx + 1], in1=xq[i][:, n_, :],
                    op0=ALU.mult, op1=ALU.add),
                    reads=[acc_b, xq_b[i], ctx["mod_b"]], writes=[xnew_b])
            S.dma("sp", lambda e, s=s: e.dma_start(out=ctx["xres_v"][:, :, s * 256:(s + 1) * 256], in_=xnew),
                  reads=[xnew_b], writes=[ctx["xseg_b"][s]])
    S.barrier()
    A.release()


def emit_final(ctx, gfin, outT_v, out_b, raw=False):
    S, A = ctx["S"], ctx["A"]
    S.barrier()
    A.mark()
    xq = [A.f32(8 * 256).rearrange("p (k n) -> p k n", k=8) for _ in range(2)]
    xq_b = [Buf("oxq0"), Buf("oxq1")]
    sq = A.bf16(8 * 256).rearrange("p (k n) -> p k n", k=8)
    nb = (sq, Buf("osq"), A.f32(256), Buf("orstd"), [A.f32(256), A.f32(256)], [Buf("otmp0"), Buf("otmp1")])
    yo = [A.f32(8 * 256).rearrange("p (k n) -> p k n", k=8) for _ in range(2)]
    yo_b = [Buf("yo0"), Buf("yo1")]
    for s in range(1, 17):
        i = s % 2
        S.dma("sp", lambda e, s=s, i=i: e.dma_start(out=xq[i], in_=ctx["xres_v"][:, :, s * 256:(s + 1) * 256]),
              reads=[ctx["xseg_b"][s]], writes=[xq_b[i]])
        if raw:
            S.dma("sp", lambda e, s=s, i=i: e.dma_start(out=outT_v[:, :, (s - 1) * 256:s * 256], in_=xq[i]),
                  reads=[xq_b[i]], writes=[out_b])
            continue
        emit_norm_seg(ctx, s, xq[i], xq_b[i], gfin, None, None, None, nb, h32_out=yo[i], h32_b=yo_b[i])
        S.dma("sp", lambda e, s=s, i=i: e.dma_start(out=outT_v[:, :, (s - 1) * 256:s * 256], in_=yo[i]),
              reads=[yo_b[i]], writes=[out_b])
    A.release()


def _pk(v):
    v = np.asarray(v, np.float32)
    return np.ascontiguousarray(v.reshape(-1, 128).T)


_PROG_CACHE = {}


def _prepare_inputs(x, c, ctx, c_ctx, w_ada, b_ada, norm1_g, norm2_g, w_in, ret_decay_logit, ret_gn_g,
                    pool_w, pool_scale, w_out, ffn_w13, ffn_w2, router_w, moe_w13, moe_w2, final_norm_g):
    hc = _host_consts()
    f = lambda a: np.ascontiguousarray(np.asarray(a, np.float32))
    vecs = np.zeros((128, NVEC), np.float32)
    for l in range(DEPTH):
        o = l * VEC_PER_LAYER
        vecs[:, o:o + 8] = _pk(norm1_g[l])
        vecs[:, o + 8:o + 16] = _pk(norm2_g[l])
        vecs[:, o + 16:o + 20] = _pk(ret_gn_g[l])
        vecs[:, o + 20:o + 24] = _pk(pool_scale[l])
        vecs[:, o + 24:o + 72] = _pk(b_ada[l])
    vecs[:, VEC_PER_LAYER * DEPTH:] = _pk(final_norm_g)
    shared = dict(vecs=vecs, dlog=f(ret_decay_logit).reshape(1, 32), w_ada=f(w_ada), w_in=f(w_in), w_out=f(w_out),
                  pool_w=f(pool_w), ffn_w13=f(ffn_w13), ffn_w2=f(ffn_w2), router_w=f(router_w), moe_w13=f(moe_w13),
                  moe_w2=f(moe_w2), blocks=hc["blocks"], cos=hc["cos"], sin=hc["sin"], misc=hc["misc"], small=hc["small"])
    x = np.asarray(x, np.float32)
    ctx = np.asarray(ctx, np.float32)
    c = np.asarray(c, np.float32)
    c_ctx = np.asarray(c_ctx, np.float32)
    in_maps = []
    for b in range(x.shape[0]):
        xT = np.ascontiguousarray(np.concatenate([ctx[b], x[b]], axis=0).T)
        cc = np.ascontiguousarray(np.concatenate([_pk(c[b]), _pk(c_ctx)], axis=1))
        m = dict(shared)
        m["xT"] = xT
        m["cc"] = cc
        in_maps.append(m)
    return in_maps


def kernel(x, c, ctx, c_ctx, w_ada, b_ada, norm1_g, norm2_g, w_in, ret_decay_logit, ret_gn_g,
           pool_w, pool_scale, w_out, ffn_w13, ffn_w2, router_w, moe_w13, moe_w2, final_norm_g):
    hc = _host_consts()
    in_maps = _prepare_inputs(x, c, ctx, c_ctx, w_ada, b_ada, norm1_g, norm2_g, w_in, ret_decay_logit, ret_gn_g,
                              pool_w, pool_scale, w_out, ffn_w13, ffn_w2, router_w, moe_w13, moe_w2, final_norm_g)
    if "nc" not in _PROG_CACHE:
        _PROG_CACHE["nc"] = build_program(plan=hc["plan"], nblk=hc["blocks"].shape[0])
    nc = _PROG_CACHE["nc"]
    res = run_bass_kernel_spmd(nc, in_maps, core_ids=list(range(len(in_maps))))
    out = np.stack([np.ascontiguousarray(np.asarray(r["outT"], np.float32).T) for r in res.results], axis=0)
    return out.astype(np.float32)
```

```python
import numpy as np
import concourse.bass as bass
import concourse.mybir as mybir
from concourse.bass_utils import run_bass_kernel_spmd

F32 = mybir.dt.float32
BF16 = mybir.dt.bfloat16
AF = mybir.ActivationFunctionType
ALU = mybir.AluOpType

D = 1024
NCTX = 256
NLAT = 4096
NTOK = NCTX + NLAT
NT = NTOK // 128
DEPTH = 4
DFF = 2816
NFC = DFF // 128
NEXP = 8
GRID = 64
WINS = (2, 4, 8, 16)
NORM_EPS = 1e-6
GN_EPS = 1e-5
SCALE = 128 ** -0.5

QUADS = [(0, 256, True)] + [(256 + 512 * i, 512, False) for i in range(8)]
FFN_BLOCKS = [[0, 1, 2], [3, 4], [5, 6], [7, 8]]
F_HALVES = [(0, 12), (12, 10)]


class Buf:
    __slots__ = ("name", "w", "r")

    def __init__(self, name=""):
        self.name = name
        self.w = None
        self.r = []


class Sched:
    ENGS = ("pe", "act", "dve", "pool", "sp")

    def __init__(self, nc, n_dma_sems=48):
        self.nc = nc
        self.ops = {e: [] for e in self.ENGS}
        self.cnt = {e: 0 for e in self.ENGS}
        self.pending = {e: False for e in self.ENGS}
        self.sem = {e: nc.alloc_semaphore("s_" + e) for e in self.ENGS}
        self.seen = {e: {} for e in self.ENGS}
        self.dma_sems = [nc.alloc_semaphore("d%d" % i) for i in range(n_dma_sems)]
        self.dma_val = [0] * n_dma_sems
        self.dma_rr = 0
        self.dma_rr_q = {}
        self.n_ops = 0
        self.n_wait = 0

    def _deps(self, eng, reads, writes):
        best = {}
        for b in reads:
            t = b.w
            if t is not None and t[1] > best.get(t[0], 0):
                best[t[0]] = t[1]
        for b in writes:
            t = b.w
            if t is not None and t[1] > best.get(t[0], 0):
                best[t[0]] = t[1]
            for t in b.r:
                if t[1] > best.get(t[0], 0):
                    best[t[0]] = t[1]
        waits = []
        seen = self.seen[eng]
        for key, val in best.items():
            if key == "pe" and eng == "pe":
                continue
            if seen.get(key, 0) >= val:
                continue
            seen[key] = val
            waits.append((key, val))
        return waits

    def _mark(self, tok, reads, writes):
        for b in reads:
            b.r.append(tok)
        for b in writes:
            b.w = tok
            b.r = []

    def op(self, eng, fn, reads=(), writes=(), signal=True):
        waits = self._deps(eng, reads, writes)
        if signal:
            self.cnt[eng] += 1
            tok = (eng, self.cnt[eng])
            self.pending[eng] = False
        else:
            tok = (eng, self.cnt[eng] + 1)
            self.pending[eng] = True
        self.ops[eng].append((fn, waits, ("eng", signal)))
        self._mark(tok, reads, writes)
        self.n_ops += 1
        self.n_wait += len(waits)
        return tok

    def dma(self, q, fn, reads=(), writes=()):
        half = len(self.dma_sems) // 2
        base = 0 if q == "sp" else half
        rr = self.dma_rr_q.get(q, 0)
        self.dma_rr_q[q] = (rr + 1) % half
        i = base + rr
        waits = self._deps(q, reads, writes)
        key = ("dma", i)
        prev = self.dma_val[i]
        if prev > 0 and self.seen[q].get(key, 0) < prev:
            self.seen[q][key] = prev
            waits.append((key, prev))
        self.dma_val[i] += 16
        tok = (key, self.dma_val[i])
        self.ops[q].append((fn, waits, ("dma", i)))
        self._mark(tok, reads, writes)
        self.n_ops += 1
        self.n_wait += len(waits)
        return tok

    def barrier(self):
        for e in self.ENGS:
            waits = []
            seen = self.seen[e]
            for e2 in self.ENGS:
                if e2 == e:
                    continue
                if self.pending[e2]:
                    raise RuntimeError("barrier with pending unsignalled op on " + e2)
                v = self.cnt[e2]
                if v > 0 and seen.get(e2, 0) < v:
                    seen[e2] = v
                    waits.append((e2, v))
            for i, v in enumerate(self.dma_val):
                key = ("dma", i)
                if v > 0 and seen.get(key, 0) < v:
                    seen[key] = v
                    waits.append((key, v))
            if waits:
                self.ops[e].append((None, waits, None))
                self.n_wait += len(waits)

    def _semof(self, key):
        if isinstance(key, tuple):
            return self.dma_sems[key[1]]
        return self.sem[key]

    def emit(self):
        nc = self.nc
        engobj = {"pe": "tensor", "act": "scalar", "dve": "vector", "pool": "gpsimd", "sp": "sync"}
        for e in self.ENGS:
            if self.pending[e]:
                raise RuntimeError("engine %s ends with unsignalled op" % e)
        with nc.Block() as block:
            for e in self.ENGS:
                ops = self.ops[e]
                if not ops:
                    continue

                def body(eng, ops=ops, e=e):
                    for fn, waits, kind in ops:
                        for key, val in waits:
                            eng.wait_ge(self._semof(key), val)
                        if fn is None:
                            continue
                        ins = fn(eng)
                        if kind[0] == "eng":
                            if kind[1]:
                                ins.then_inc(self.sem[e], 1)
                        else:
                            ins.then_inc(self.dma_sems[kind[1]], 16)

                getattr(block, engobj[e])(body)


class Arena:
    def __init__(self, nc, nwords):
        self.t = nc.alloc_sbuf_tensor("arena", [128, nwords], F32)
        self.n = nwords
        self.top = 0
        self.marks = []

    def mark(self):
        self.marks.append(self.top)

    def release(self):
        self.top = self.marks.pop()

    def f32(self, n):
        n = int(n)
        a = self.top
        self.top += (n + 7) // 8 * 8
        assert self.top <= self.n, ("SBUF arena overflow", self.top, self.n)
        return self.t[:, a:a + n]

    def bf16(self, n):
        n = int(n)
        w = (n + 1) // 2
        a = self.top
        self.top += (w + 7) // 8 * 8
        assert self.top <= self.n, ("SBUF arena overflow", self.top, self.n)
        return self.t[:, a:a + w].bitcast(BF16)[:, 0:n]


def _win_matrix(n, w):
    A = np.zeros((n, n), np.float64)
    for i in range(n):
        lo = min(max(i - w // 2, 0), n)
        hi = min(max(i - w // 2 + w, 0), n)
        A[i, lo:hi] = 1.0 / (hi - lo)
    return A


def _pool_blocks():
    blocks = []
    index = {}
    plan = [[None] * NT for _ in range(4)]

    def add(M):
        M = np.ascontiguousarray(M.astype(np.float32))
        key = M.tobytes()
        if key not in index:
            index[key] = len(blocks)
            blocks.append(M)
        return index[key]

    eye = np.eye(128)
    for g, w in enumerate(WINS):
        A = _win_matrix(NCTX, w) - np.eye(NCTX)
        for jo in range(2):
            lst = []
            for ji in range(2):
                M = A[jo * 128:(jo + 1) * 128, ji * 128:(ji + 1) * 128].T
                if np.any(M != 0):
                    lst.append((ji, add(M)))
            plan[g][jo] = lst
        Ar = _win_matrix(GRID, w)
        Ac = _win_matrix(GRID, w)
        for jo in range(32):
            lst = []
            for ji in range(32):
                sub = Ar[2 * jo:2 * jo + 2, 2 * ji:2 * ji + 2]
                if not np.any(sub != 0):
                    continue
                M = np.kron(sub, Ac)
                if ji == jo:
                    M = M - eye
                lst.append((2 + ji, add(M.T)))
            plan[g][2 + jo] = lst
    return np.stack(blocks), plan


def _rope_tables():
    half = 64
    inv = 10000.0 ** (-np.arange(0, half, 2, dtype=np.float32) / np.float32(half))
    inv = inv.astype(np.float32)
    cos = np.ones((128, NT, 2, 32), np.float32)
    sin = np.zeros((128, NT, 2, 32), np.float32)
    t = np.arange(NLAT)
    rows = (t // GRID).astype(np.float32)
    cols = (t % GRID).astype(np.float32)
    for hidx, pos in enumerate((rows, cols)):
        ang = (pos[:, None] * inv[None, :]).astype(np.float32)
        c = np.cos(ang).astype(np.float32).reshape(32, 128, 32)
        s = np.sin(ang).astype(np.float32).reshape(32, 128, 32)
        cos[:, 2:, hidx, :] = c.transpose(1, 0, 2)
        sin[:, 2:, hidx, :] = s.transpose(1, 0, 2)
    return cos.reshape(128, -1), sin.reshape(128, -1)


_CONST_CACHE = {}


def _host_consts():
    if "c" in _CONST_CACHE:
        return _CONST_CACHE["c"]
    blocks, plan = _pool_blocks()
    cos, sin = _rope_tables()
    p = np.arange(128, dtype=np.float32)
    i = np.arange(128, dtype=np.float32)
    E = i[None, :] - p[:, None]
    misc = np.zeros((128, 6, 128), np.float32)
    misc[:, 0] = np.eye(128)
    misc[:, 1] = 1.0
    misc[:, 2] = np.maximum(E, 0)
    misc[:, 3] = np.maximum(-E, 0)
    misc[:, 4] = (E >= 0)
    misc[:, 5] = (E < 0)
    small = np.zeros((128, 264), np.float32)
    small[:, 0:128] = i[None, :] + 1.0
    small[:, 128:256] = 128.0 - i[None, :]
    small[:, 256] = 127.0 - p
    small[:, 257] = p
    small[:, 258] = NORM_EPS
    small[:, 259] = GN_EPS
    small[:, 260] = 1.0
    small[:, 261] = 128.0
    small[:, 262] = -0.5
    c = dict(blocks=blocks, plan=plan, cos=cos, sin=sin, misc=misc.reshape(128, -1), small=small)
    _CONST_CACHE["c"] = c
    return c


VEC_PER_LAYER = 72
NVEC = VEC_PER_LAYER * DEPTH + 8


def build_program(n_layers=DEPTH, stop_after_mix=False, nblk=69, plan=None):
    nc = bass.Bass("TRN2", target_bir_lowering=False)
    S = Sched(nc)

    def din(name, shape, dt=F32):
        return nc.dram_tensor(name, list(shape), dt, kind="ExternalInput").ap()

    xT_in = din("xT", [D, NTOK])
    cc_in = din("cc", [128, 16])
    vecs_in = din("vecs", [128, NVEC])
    dlog_in = din("dlog", [1, 32])
    w_ada = din("w_ada", [DEPTH, D, 6 * D])
    w_in = din("w_in", [DEPTH, D, 2560])
    w_out = din("w_out", [DEPTH, D, D])
    pool_w = din("pool_w", [DEPTH, 4, 128, 128])
    ffn_w13 = din("ffn_w13", [2, D, 2 * DFF])
    ffn_w2 = din("ffn_w2", [2, DFF, D])
    router_w = din("router_w", [2, D, NEXP])
    moe_w13 = din("moe_w13", [2, NEXP, D, 2 * DFF])
    moe_w2 = din("moe_w2", [2, NEXP, DFF, D])
    blocks_in = din("blocks", [nblk, 128, 128])
    cos_in = din("cos", [128, NT * 64])
    sin_in = din("sin", [128, NT * 64])
    misc_in = din("misc", [128, 768])
    small_in = din("small", [128, 264])
    outT = nc.dram_tensor("outT", [D, NLAT], F32, kind="ExternalOutput").ap()
    xres = nc.dram_tensor("xres", [D, NTOK], F32, kind="Internal").ap()
    pscr = nc.dram_tensor("pscr", [NT, 128, 512], BF16, kind="Internal").ap()
    sbscr = nc.dram_tensor("sbscr", [NT, 128, 512], BF16, kind="Internal").ap()
    kscr = nc.dram_tensor("kscr", [NT, 128, 512], BF16, kind="Internal").ap()
    vscr = nc.dram_tensor("vscr", [NT, 128, 512], BF16, kind="Internal").ap()
    xres_v = xres.rearrange("(k p) t -> p k t", p=128)
    outT_v = outT.rearrange("(k p) t -> p k t", p=128)

    A = Arena(nc, 52000)
    banks = [nc.alloc_psum_tensor("bank%d" % i, [128, 512], F32) for i in range(8)]
    bankb = [Buf("bank%d" % i) for i in range(8)]

    def bk(i):
        return banks[i][:]

    def bkb16(i):
        return banks[i][:].bitcast(BF16)

    xseg_b = [Buf("xseg%d" % s) for s in range(17)]
    pscr_b = [Buf("pscr%d" % t) for t in range(NT)]
    sbscr_b = [Buf("sbscr%d" % t) for t in range(NT)]
    kscr_b = [Buf("kscr%d" % t) for t in range(NT)]
    vscr_b = [Buf("vscr%d" % t) for t in range(NT)]
    out_b = Buf("out")

    misc = A.f32(768).rearrange("p (a b) -> p a b", a=6)
    misc_b = Buf("misc")
    small = A.f32(264)
    small_b = Buf("small")
    vecs = A.f32(NVEC)
    vecs_b = Buf("vecs")
    ident_bf = A.bf16(128)
    ones_bf = A.bf16(128)
    cbf_b = Buf("cbf")
    mod = A.f32(DEPTH * 96).rearrange("p (l j s) -> p l j s", l=DEPTH, j=48)
    mod_b = Buf("mod")
    lg_all = A.f32(32)
    lg_b = Buf("lg")
    silc = A.f32(16)
    silc_b = Buf("silc")
    ident_f = misc[:, 0, :]
    ones_f = misc[:, 1, :]
    Epos, Eneg, Mf, Mb = misc[:, 2, :], misc[:, 3, :], misc[:, 4, :], misc[:, 5, :]
    eps_n = small[:, 258:259]
    eps_g = small[:, 259:260]

    S.dma("sp", lambda e: e.dma_start(out=misc.rearrange("p a b -> p (a b)"), in_=misc_in[:, :]), writes=[misc_b])
    S.dma("sp", lambda e: e.dma_start(out=small, in_=small_in[:, :]), writes=[small_b])
    S.dma("sp", lambda e: e.dma_start(out=vecs, in_=vecs_in[:, :]), writes=[vecs_b])
    S.dma("sp", lambda e: e.dma_start(out=silc, in_=cc_in[:, :]), writes=[silc_b])
    S.dma("sp", lambda e: e.dma_start(out=lg_all, in_=dlog_in.partition_broadcast(128).rearrange("p a b -> p (a b)")
                                      if len(dlog_in.partition_broadcast(128).shape) == 3 else dlog_in.partition_broadcast(128)),
          writes=[lg_b])
    S.op("dve", lambda e: e.tensor_copy(out=ident_bf, in_=ident_f), reads=[misc_b], writes=[cbf_b])
    S.op("dve", lambda e: e.tensor_copy(out=ones_bf, in_=ones_f), reads=[misc_b], writes=[cbf_b])
    for s in range(17):
        S.dma("sp", lambda e, s=s: e.dma_start(out=xres[:, s * 256:(s + 1) * 256], in_=xT_in[:, s * 256:(s + 1) * 256]),
              writes=[xseg_b[s]])

    S.op("act", lambda e: e.activation(out=lg_all, in_=lg_all, func=AF.Exp, scale=-1.0), reads=[lg_b], writes=[lg_b])
    S.op("dve", lambda e: e.tensor_scalar(out=lg_all, in0=lg_all, scalar1=1.0, scalar2=None, op0=ALU.add),
         reads=[lg_b], writes=[lg_b])
    S.op("act", lambda e: e.activation(out=lg_all, in_=lg_all, func=AF.Ln), reads=[lg_b], writes=[lg_b])
    S.op("dve", lambda e: e.tensor_scalar(out=lg_all, in0=lg_all, scalar1=-1.0, scalar2=None, op0=ALU.mult),
         reads=[lg_b], writes=[lg_b])
    S.op("act", lambda e: e.activation(out=silc, in_=silc, func=AF.Silu), reads=[silc_b], writes=[silc_b])

    A.mark()
    wa_st = [A.f32(8 * 1024).rearrange("p (k n) -> p k n", k=8) for _ in range(2)]
    wa_b = [Buf("wa0"), Buf("wa1")]
    silc_v = silc.rearrange("p (s k) -> p k s", s=2)
    it = 0
    for l in range(n_layers):
        for j6 in range(6):
            i = it % 2
            it += 1
            for half in range(2):
                S.dma("sp", lambda e, i=i, l=l, j6=j6, half=half: e.dma_start(
                    out=wa_st[i][:, half * 4:(half + 1) * 4, :],
                    in_=w_ada[l, half * 512:(half + 1) * 512, j6 * 1024:(j6 + 1) * 1024].rearrange("(k p) n -> p k n", p=128)),
                    writes=[wa_b[i]])
            pb = j6 % 2
            for n in range(8):
                for k in range(8):
                    S.op("pe", lambda e, i=i, n=n, k=k, pb=pb: e.matmul(
                        bk(pb)[:, n * 2:n * 2 + 2], lhsT=wa_st[i][:, k, n * 128:(n + 1) * 128], rhs=silc_v[:, k, :],
                        start=(k == 0), stop=(k == 7)),
                        reads=[wa_b[i], silc_b], writes=[bankb[pb]], signal=(k == 7))
            bofs = l * VEC_PER_LAYER + 24 + j6 * 8
            S.op("dve", lambda e, l=l, j6=j6, pb=pb, bofs=bofs: e.tensor_tensor(
                out=mod[:, l, j6 * 8:(j6 + 1) * 8, :],
                in0=bk(pb)[:, 0:16].rearrange("p (n s) -> p n s", s=2),
                in1=vecs[:, bofs:bofs + 8].unsqueeze(2).broadcast_to([128, 8, 2]), op=ALU.add),
                reads=[bankb[pb], vecs_b], writes=[mod_b])
    S.barrier()
    A.release()

    ctx = dict(nc=nc, S=S, A=A, bk=bk, bkb16=bkb16, bankb=bankb, xres_v=xres_v, xseg_b=xseg_b,
               pscr=pscr, pscr_b=pscr_b, sbscr=sbscr, sbscr_b=sbscr_b, kscr=kscr, kscr_b=kscr_b, vscr=vscr, vscr_b=vscr_b, misc=misc, misc_b=misc_b, small=small,
               small_b=small_b, vecs=vecs, vecs_b=vecs_b, ident_bf=ident_bf, ones_bf=ones_bf, cbf_b=cbf_b,
               mod=mod, mod_b=mod_b, lg_all=lg_all, lg_b=lg_b, ident_f=ident_f, ones_f=ones_f,
               Epos=Epos, Eneg=Eneg, Mf=Mf, Mb=Mb, eps_n=eps_n, eps_g=eps_g,
               w_in=w_in, w_out=w_out, pool_w=pool_w, blocks_in=blocks_in, cos_in=cos_in, sin_in=sin_in,
               ffn_w13=ffn_w13, ffn_w2=ffn_w2, router_w=router_w, moe_w13=moe_w13, moe_w2=moe_w2,
               plan=plan, nblk=nblk)

    for l in range(n_layers):
        emit_mixer(ctx, l)
        if stop_after_mix and l == n_layers - 1:
            break
        emit_ffn(ctx, l)

    emit_final(ctx, vecs[:, VEC_PER_LAYER * DEPTH:VEC_PER_LAYER * DEPTH + 8], outT_v, out_b,
               raw=(n_layers < DEPTH or stop_after_mix))
    S.barrier()
    S.emit()
    return nc


def emit_norm_seg(ctx, s, xq, xq_b, gvec, svec, hT_out, hT_b, nb, h32_out=None, h32_b=None, statbank=7):
    S, bk, bankb = ctx["S"], ctx["bk"], ctx["bankb"]
    sq, sq_b, rstd, rstd_b, tmp, tmp_b = nb
    S.op("act", lambda e: e.activation(out=sq, in_=xq, func=AF.Square), reads=[xq_b], writes=[sq_b])
    for k in range(8):
        S.op("pe", lambda e, k=k: e.matmul(bk(statbank)[:, 0:256], lhsT=ctx["ones_bf"], rhs=sq[:, k, :],
                                           start=(k == 0), stop=(k == 7)),
             reads=[sq_b, ctx["cbf_b"]], writes=[bankb[statbank]], signal=(k == 7))
    S.op("act", lambda e: e.activation(out=rstd, in_=bk(statbank)[:, 0:256], func=AF.Ln, bias=ctx["eps_n"], scale=1.0 / D),
         reads=[bankb[statbank], ctx["small_b"]], writes=[rstd_b])
    S.op("act", lambda e: e.activation(out=rstd, in_=rstd, func=AF.Exp, scale=-0.5), reads=[rstd_b], writes=[rstd_b])
    dst, dst_b = (hT_out, hT_b) if h32_out is None else (h32_out, h32_b)
    for k in range(8):
        i = k % 2
        S.op("dve", lambda e, k=k, i=i: e.tensor_tensor(out=tmp[i], in0=xq[:, k, :], in1=rstd, op=ALU.mult),
             reads=[xq_b, rstd_b], writes=[tmp_b[i]])
        if svec is not None:
            S.op("act", lambda e, k=k, i=i: e.activation(out=dst[:, k, :], in_=tmp[i], func=AF.Identity,
                                                         bias=svec[:, k:k + 1], scale=gvec[:, k:k + 1]),
                 reads=[tmp_b[i], ctx["mod_b"], ctx["vecs_b"]], writes=[dst_b])
        else:
            S.op("act", lambda e, k=k, i=i: e.activation(out=dst[:, k, :], in_=tmp[i], func=AF.Identity,
                                                         scale=gvec[:, k:k + 1]),
                 reads=[tmp_b[i], ctx["mod_b"], ctx["vecs_b"]], writes=[dst_b])
    if h32_out is not None and hT_out is not None:
        S.op("pool", lambda e: e.tensor_copy(out=hT_out, in_=h32_out), reads=[h32_b], writes=[hT_b])


def emit_layer_vectors(ctx, l, which):
    S, A = ctx["S"], ctx["A"]
    mod, vecs = ctx["mod"], ctx["vecs"]
    G = A.f32(16).rearrange("p (k s) -> p k s", s=2)
    g_ofs = l * VEC_PER_LAYER + (0 if which == 1 else 8)
    j_sc = 8 if which == 1 else 32
    S.op("dve", lambda e: e.scalar_tensor_tensor(
        out=G, in0=mod[:, l, j_sc:j_sc + 8, :], scalar=1.0,
        in1=vecs[:, g_ofs:g_ofs + 8].unsqueeze(2).broadcast_to([128, 8, 2]), op0=ALU.add, op1=ALU.mult),
        reads=[ctx["mod_b"], ctx["vecs_b"]], writes=[ctx["mod_b"]])
    return G


def emit_rope(ctx, src_view, dst_view, cs, sn, nh, tmps, tmps_b, src_b, dst_b, tab_b):
    S = ctx["S"]
    u1, u2 = src_view[:, :, :, 0, :], src_view[:, :, :, 1, :]
    o1, o2 = dst_view[:, :, :, 0, :], dst_view[:, :, :, 1, :]
    cb = cs.unsqueeze(1).broadcast_to([128, nh, 2, 32])
    sb = sn.unsqueeze(1).broadcast_to([128, nh, 2, 32])
    t = [x[:, 0:nh * 64].rearrange("p (h a f) -> p h a f", h=nh, a=2) for x in tmps]
    S.op("dve", lambda e: e.tensor_tensor(out=t[0], in0=u1, in1=cb, op=ALU.mult), reads=[src_b, tab_b], writes=[tmps_b[0]])
    S.op("dve", lambda e: e.tensor_tensor(out=t[1], in0=u2, in1=sb, op=ALU.mult), reads=[src_b, tab_b], writes=[tmps_b[1]])
    S.op("dve", lambda e: e.tensor_tensor(out=t[2], in0=u1, in1=sb, op=ALU.mult), reads=[src_b, tab_b], writes=[tmps_b[2]])
    S.op("dve", lambda e: e.tensor_tensor(out=t[3], in0=u2, in1=cb, op=ALU.mult), reads=[src_b, tab_b], writes=[tmps_b[3]])
    S.op("pool", lambda e: e.tensor_tensor(out=o1, in0=t[0], in1=t[1], op=ALU.subtract),
         reads=[tmps_b[0], tmps_b[1]], writes=[dst_b])
    S.op("pool", lambda e: e.tensor_tensor(out=o2, in0=t[2], in1=t[3], op=ALU.add),
         reads=[tmps_b[2], tmps_b[3]], writes=[dst_b])


def emit_mixer(ctx, l):
    S, A, bk, bkb16, bankb = ctx["S"], ctx["A"], ctx["bk"], ctx["bkb16"], ctx["bankb"]
    mod, vecs, small, lg_all = ctx["mod"], ctx["vecs"], ctx["small"], ctx["lg_all"]
    plan = ctx["plan"]
    nblk = ctx["nblk"]
    last = (l == DEPTH - 1)
    S.barrier()
    A.mark()
    win = A.bf16(8 * 2560).rearrange("p (k n) -> p k n", k=8)
    win_b = Buf("win")
    wout = A.bf16(8 * 1024).rearrange("p (k n) -> p k n", k=8)
    wout_b = Buf("wout")
    poolw = A.bf16(4 * 128).rearrange("p (g n) -> p g n", g=4)
    poolw_b = Buf("poolw")
    blocks = A.bf16(nblk * 128).rearrange("p (b n) -> p b n", b=nblk)
    blocks_b = Buf("blocks")
    for k in range(8):
        for hf in range(2):
            S.dma("pool", lambda e, k=k, hf=hf: e.dma_start(
                out=win[:, k, hf * 1280:(hf + 1) * 1280], in_=ctx["w_in"][l, k * 128:(k + 1) * 128, hf * 1280:(hf + 1) * 1280]),
                writes=[win_b])
    S.dma("pool", lambda e: e.dma_start(out=wout, in_=ctx["w_out"][l].rearrange("(k p) n -> p k n", p=128)), writes=[wout_b])
    S.dma("pool", lambda e: e.dma_start(out=poolw, in_=ctx["pool_w"][l].rearrange("g c d -> c g d")), writes=[poolw_b])
    for b0 in range(0, nblk, 16):
        b1 = min(nblk, b0 + 16)
        S.dma("pool", lambda e, b0=b0, b1=b1: e.dma_start(
            out=blocks[:, b0:b1, :], in_=ctx["blocks_in"][b0:b1].rearrange("b p n -> p b n")), writes=[blocks_b])

    G1 = emit_layer_vectors(ctx, l, 1)
    S1 = mod[:, l, 0:8, :]
    GT1 = mod[:, l, 16:24, :]
    gn_ofs = l * VEC_PER_LAYER + 16
    rsc = vecs[:, gn_ofs:gn_ofs + 8]

    Dcomb = A.f32(512).rearrange("p (h n) -> p h n", h=4)
    decqf = A.f32(512).rearrange("p (h n) -> p h n", h=4)
    decqb = A.f32(512).rearrange("p (h n) -> p h n", h=4)
    dk = A.f32(16)
    dtmp = [A.f32(128), A.f32(128)]
    dec_b = Buf("dec")
    dtmp_b = [Buf("dtmp0"), Buf("dtmp1")]
    lgf = lg_all[:, l * 8:l * 8 + 4]
    lgb = lg_all[:, l * 8 + 4:l * 8 + 8]
    rd = [ctx["lg_b"], ctx["misc_b"], ctx["small_b"]]
    for h in range(4):
        S.op("act", lambda e, h=h: e.activation(out=dtmp[0], in_=ctx["Epos"], func=AF.Exp, scale=lgf[:, h:h + 1]),
             reads=rd, writes=[dtmp_b[0]])
        S.op("dve", lambda e, h=h: e.scalar_tensor_tensor(out=Dcomb[:, h, :], in0=dtmp[0], scalar=SCALE, in1=ctx["Mf"],
                                                          op0=ALU.mult, op1=ALU.mult),
             reads=[dtmp_b[0]] + rd, writes=[dec_b])
        S.op("act", lambda e, h=h: e.activation(out=dtmp[1], in_=ctx["Eneg"], func=AF.Exp, scale=lgb[:, h:h + 1]),
             reads=rd, writes=[dtmp_b[1]])
        S.op("dve", lambda e, h=h: e.scalar_tensor_tensor(out=dtmp[1], in0=dtmp[1], scalar=SCALE, in1=ctx["Mb"],
                                                          op0=ALU.mult, op1=ALU.mult),
             reads=[dtmp_b[1]] + rd, writes=[dtmp_b[1]])
        S.op("dve", lambda e, h=h: e.tensor_tensor(out=Dcomb[:, h, :], in0=Dcomb[:, h, :], in1=dtmp[1], op=ALU.add),
             reads=[dtmp_b[1], dec_b], writes=[dec_b])
        S.op("act", lambda e, h=h: e.activation(out=decqf[:, h, :], in_=small[:, 0:128], func=AF.Exp, scale=lgf[:, h:h + 1]),
             reads=rd, writes=[dec_b])
        S.op("act", lambda e, h=h: e.activation(out=decqb[:, h, :], in_=small[:, 128:256], func=AF.Exp, scale=lgb[:, h:h + 1]),
             reads=rd, writes=[dec_b])
    S.op("dve", lambda e: e.tensor_scalar(out=dk[:, 0:4], in0=lgf, scalar1=small[:, 256:257], scalar2=None, op0=ALU.mult),
         reads=rd, writes=[dec_b])
    S.op("dve", lambda e: e.tensor_scalar(out=dk[:, 4:8], in0=lgb, scalar1=small[:, 257:258], scalar2=None, op0=ALU.mult),
         reads=rd, writes=[dec_b])
    S.op("dve", lambda e: e.tensor_scalar(out=dk[:, 8:12], in0=lgf, scalar1=128.0, scalar2=None, op0=ALU.mult),
         reads=rd, writes=[dec_b])
    S.op("dve", lambda e: e.tensor_scalar(out=dk[:, 12:16], in0=lgb, scalar1=128.0, scalar2=None, op0=ALU.mult),
         reads=rd, writes=[dec_b])
    S.op("act", lambda e: e.activation(out=dk, in_=dk, func=AF.Exp), reads=[dec_b], writes=[dec_b])
    S.op("dve", lambda e: e.tensor_scalar(out=dk[:, 0:8], in0=dk[:, 0:8], scalar1=SCALE, scalar2=None, op0=ALU.mult),
         reads=[dec_b], writes=[dec_b])
    dkf, dkb, gcf, gcb = dk[:, 0:4], dk[:, 4:8], dk[:, 8:12], dk[:, 12:16]

    xq = [A.f32(8 * 256).rearrange("p (k n) -> p k n", k=8) for _ in range(3)]
    xq_b = [Buf("xq%d" % i) for i in range(3)]
    sq = A.bf16(8 * 256).rearrange("p (k n) -> p k n", k=8)
    nb = (sq, Buf("sq"), A.f32(256), Buf("rstd"), [A.f32(256), A.f32(256)], [Buf("tmp0"), Buf("tmp1")])
    hT = [A.bf16(8 * 256).rearrange("p (k n) -> p k n", k=8) for _ in range(2)]
    hT_b = [Buf("hT0"), Buf("hT1")]
    cst = [A.f32(128).rearrange("p (j a f) -> p j a f", j=2, a=2) for _ in range(3)]
    snt = [A.f32(128).rearrange("p (j a f) -> p j a f", j=2, a=2) for _ in range(3)]
    tab_b = [Buf("tab0"), Buf("tab1"), Buf("tab2")]
    rtmp = [A.f32(512) for _ in range(4)]
    rtmp_b = [Buf("rt%d" % i) for i in range(4)]
    P2 = range(2)
    qk_tm = [A.bf16(1024) for _ in P2]
    qk_b = [Buf("qk_tm%d" % i) for i in P2]
    v_tm = [A.bf16(512) for _ in P2]
    v_b = [Buf("v_tm%d" % i) for i in P2]
    sg = [A.f32(512) for _ in P2]
    sg_b = [Buf("sg%d" % i) for i in P2]
    kT = [A.bf16(512).rearrange("p (h n) -> p h n", h=4) for _ in P2]
    qT = [A.bf16(512).rearrange("p (h n) -> p h n", h=4) for _ in P2]
    qfT = [A.bf16(512).rearrange("p (h n) -> p h n", h=4) for _ in P2]
    qbT = [A.bf16(512).rearrange("p (h n) -> p h n", h=4) for _ in P2]
    qkT_b = [Buf("qkT%d" % i) for i in P2]
    ktil = [A.bf16(512).rearrange("p (h n) -> p h n", h=4) for _ in P2]
    ktil_b = [Buf("ktil%d" % i) for i in P2]
    pst = [A.bf16(512) for _ in P2]
    pst_b = [Buf("pst%d" % i) for i in P2]
    sbst = [A.bf16(512) for _ in P2]
    sbst_b = [Buf("sbst%d" % i) for i in P2]
    kt3 = [pst[0], pst[1], sbst[0]]
    kt3_b = [pst_b[0], pst_b[1], sbst_b[0]]
    vt3 = [sbst[1], A.bf16(512), A.bf16(512)]
    vt3_b = [sbst_b[1], Buf("vt3_1"), Buf("vt3_2")]
    PT = A.bf16(512).rearrange("p (h n) -> p h n", h=4)
    PT_b = Buf("PT")
    St = A.f32(512).rearrange("p (h n) -> p h n", h=4)
    St_bf = A.bf16(512).rearrange("p (h n) -> p h n", h=4)
    St_b = Buf("St")
    Stbf_b = Buf("Stbf")
    pslot = [A.bf16(512) for _ in range(12)]
    pslot_b = [Buf("pslot%d" % i) for i in range(12)]
    sbslot = [A.bf16(512).rearrange("p (h n) -> p h n", h=4) for _ in range(4)]
    sbslot_b = [Buf("sbslot%d" % i) for i in range(4)]
    stats = A.f32(24).rearrange("p (h s) -> p h s", h=4)
    mv = A.f32(8).rearrange("p (h s) -> p h s", h=4)
    rs4 = A.f32(4)
    gn_b = Buf("gn")
    on = A.f32(512).rearrange("p (h n) -> p h n", h=4)
    on_b = Buf("on")
    ret_tm = A.bf16(512)
    ret_b = Buf("ret_tm")
    mixT = [A.bf16(1024).rearrange("p (k n) -> p k n", k=8) for _ in P2]
    mixr_b = [Buf("mixr%d" % i) for i in P2]
    mixp_b = [Buf("mixp%d" % i) for i in P2]
    dT = A.bf16(512).rearrange("p (g n) -> p g n", g=4)
    dT_b = Buf("dT")
    xnew = [A.f32(1024).rearrange("p (k n) -> p k n", k=8) for _ in P2]
    xnew_b = [Buf("xnew%d" % i) for i in P2]
    cos_v = ctx["cos_in"].rearrange("p (t a f) -> p t a f", t=NT, a=2)
    sin_v = ctx["sin_in"].rearrange("p (t a f) -> p t a f", t=NT, a=2)
    STATB = 3

    def load_seg(s, n):
        i3 = n % 3
        S.dma("sp", lambda e: e.dma_start(out=xq[i3], in_=ctx["xres_v"][:, :, s * 256:(s + 1) * 256]),
              reads=[ctx["xseg_b"][s]], writes=[xq_b[i3]])
        S.dma("sp", lambda e: e.dma_start(out=cst[i3], in_=cos_v[:, 2 * s:2 * s + 2]), writes=[tab_b[i3]])
        S.dma("sp", lambda e: e.dma_start(out=snt[i3], in_=sin_v[:, 2 * s:2 * s + 2]), writes=[tab_b[i3]])

    def norm_seg(s, n):
        sidx = 1 if s == 0 else 0
        emit_norm_seg(ctx, s, xq[n % 3], xq_b[n % 3], G1[:, :, sidx], S1[:, :, sidx], hT[n % 2], hT_b[n % 2], nb, statbank=STATB)

    def project(n, tl, col0, ncols, bank0):
        h_, hb_ = hT[n % 2], hT_b[n % 2]
        for c in range(ncols // 512):
            for k in range(8):
                S.op("pe", lambda e, c=c, k=k: e.matmul(
                    bk(bank0 + c), lhsT=h_[:, k, tl * 128:(tl + 1) * 128], rhs=win[:, k, col0 + c * 512:col0 + (c + 1) * 512],
                    start=(k == 0), stop=(k == 7)),
                    reads=[hb_, win_b], writes=[bankb[bank0 + c]], signal=(k == 7))

    def make_ktil(p, dkv, ksrc=None, ksrc_b=None):
        if ksrc is None:
            ksrc, ksrc_b = qk_tm[p][:, 512:1024], qk_b[p]
        kview = ksrc.rearrange("p (h n) -> p h n", h=4)
        S.op("pool", lambda e: e.tensor_tensor(out=ktil[p], in0=kview, in1=dkv.unsqueeze(2).broadcast_to([128, 4, 128]), op=ALU.mult),
             reads=[ksrc_b, dec_b], writes=[ktil_b[p]])

    def state_update(p, gcv, vsrc=None, vsrc_b=None):
        if vsrc is None:
            vsrc, vsrc_b = v_tm[p], v_b[p]
        for h in range(4):
            S.op("pe", lambda e, h=h: e.matmul(bk(7)[:, h * 128:(h + 1) * 128], lhsT=ktil[p][:, h, :], rhs=vsrc[:, h * 128:(h + 1) * 128],
                                               start=True, stop=True),
                 reads=[ktil_b[p], vsrc_b], writes=[bankb[7]], signal=(h == 3))
        S.op("pool", lambda e: e.tensor_tensor(out=St, in0=St, in1=gcv.unsqueeze(2).broadcast_to([128, 4, 128]), op=ALU.mult),
             reads=[St_b, dec_b], writes=[St_b])
        S.op("dve", lambda e: e.tensor_tensor(out=St, in0=St, in1=bk(7).rearrange("p (h n) -> p h n", h=4), op=ALU.add),
             reads=[St_b, bankb[7]], writes=[St_b])
        S.op("act", lambda e: e.copy(out=St_bf, in_=St), reads=[St_b], writes=[Stbf_b])

    def zero_state():
        S.op("pool", lambda e: e.memset(St.rearrange("p h n -> p (h n)"), 0.0), writes=[St_b])
        S.op("pool", lambda e: e.memset(St_bf.rearrange("p h n -> p (h n)"), 0.0), writes=[Stbf_b])

    zero_state()
    seg_order = [0] + list(range(16, 0, -1))
    tiles = []
    for n, s in enumerate(seg_order):
        for tl in (1, 0):
            tiles.append((2 * s + tl, n, s, tl))

    def pre_front(j):
        t, n, s, tl = tiles[j]
        p = j % 2
        project(n, tl, 512, 512, 0)
        project(n, tl, 1024, 512, 2)
        project(n, tl, 2048, 512, 3)
        yield
        kv5 = bk(0).rearrange("p (h a b f) -> p h a b f", h=4, a=2, b=2)
        kd5 = qk_tm[p][:, 512:1024].rearrange("p (h a b f) -> p h a b f", h=4, a=2, b=2)
        emit_rope(ctx, kv5, kd5, cst[n % 3][:, tl], snt[n % 3][:, tl], 4, rtmp, rtmp_b, bankb[0], qk_b[p], tab_b[n % 3])
        S.op("act", lambda e: e.copy(out=v_tm[p], in_=bk(2)), reads=[bankb[2]], writes=[v_b[p]])
        S.op("act", lambda e: e.copy(out=pst[p], in_=bk(3)), reads=[bankb[3]], writes=[pst_b[p]])
        S.dma("pool", lambda e: e.dma_start(out=ctx["pscr"][t], in_=pst[p]), reads=[pst_b[p]], writes=[ctx["pscr_b"][t]])
        S.dma("pool", lambda e: e.dma_start(out=ctx["vscr"][t], in_=v_tm[p]), reads=[v_b[p]], writes=[ctx["vscr_b"][t]])
        S.dma("pool", lambda e: e.dma_start(out=ctx["kscr"][t], in_=qk_tm[p][:, 512:1024]), reads=[qk_b[p]], writes=[ctx["kscr_b"][t]])
        make_ktil(p, dkb)
        if tl == 0:
            if n + 2 < len(seg_order):
                load_seg(seg_order[n + 2], n + 2)
            if n + 1 < len(seg_order):
                norm_seg(seg_order[n + 1], n + 1)

    def pre_back(j):
        t, n, s, tl = tiles[j]
        p = j % 2
        S.op("act", lambda e: e.copy(out=sbst[p], in_=St_bf.rearrange("p h n -> p (h n)")), reads=[Stbf_b], writes=[sbst_b[p]])
        S.dma("pool", lambda e: e.dma_start(out=ctx["sbscr"][t], in_=sbst[p]), reads=[sbst_b[p]], writes=[ctx["sbscr_b"][t]])
        state_update(p, gcb)
        yield

    def interleave(gens):
        active = list(gens)
        while active:
            for g_ in list(active):
                try:
                    next(g_)
                except StopIteration:
                    active.remove(g_)

    load_seg(seg_order[0], 0)
    load_seg(seg_order[1], 1)
    norm_seg(seg_order[0], 0)
    interleave([pre_front(0)])
    for j in range(len(tiles)):
        gens = [pre_back(j)]
        if j + 1 < len(tiles):
            gens.append(pre_front(j + 1))
        interleave(gens)

    zero_state()

    def p_needed(t):
        r = set()
        for g in range(4):
            for (ti, _) in plan[g][t]:
                r.add(ti)
        return r

    loaded_p = set()

    def prefetch_p(t):
        if t in loaded_p or t >= NT:
            return
        loaded_p.add(t)
        S.dma("sp", lambda e: e.dma_start(out=pslot[t % 12], in_=ctx["pscr"][t]), reads=[ctx["pscr_b"][t]], writes=[pslot_b[t % 12]])

    def prefetch_sb(t):
        if t < NT:
            S.dma("sp", lambda e: e.dma_start(out=sbslot[t % 4].rearrange("p h n -> p (h n)"), in_=ctx["sbscr"][t]),
                  reads=[ctx["sbscr_b"][t]], writes=[sbslot_b[t % 4]])

    def main_front(t):
        s, tl = t // 2, t % 2
        n = s
        p = t % 2
        prefetch_sb(t + 2)
        for tt in sorted(p_needed(t) | (p_needed(t + 1) if t + 1 < NT else set())):
            prefetch_p(tt)
        k3, k3b, v3, v3b = kt3[t % 3], kt3_b[t % 3], vt3[t % 3], vt3_b[t % 3]
        if t + 1 < NT:
            S.dma("sp", lambda e: e.dma_start(out=kt3[(t + 1) % 3], in_=ctx["kscr"][t + 1]), reads=[ctx["kscr_b"][t + 1]], writes=[kt3_b[(t + 1) % 3]])
            S.dma("sp", lambda e: e.dma_start(out=vt3[(t + 1) % 3], in_=ctx["vscr"][t + 1]), reads=[ctx["vscr_b"][t + 1]], writes=[vt3_b[(t + 1) % 3]])
        project(n, tl, 0, 512, 0)
        project(n, tl, 1536, 512, 3)
        yield
        sv = bk(0).rearrange("p (h a b f) -> p h a b f", h=4, a=2, b=2)
        dv = qk_tm[p][:, 0:512].rearrange("p (h a b f) -> p h a b f", h=4, a=2, b=2)
        emit_rope(ctx, sv, dv, cst[n % 3][:, tl], snt[n % 3][:, tl], 4, rtmp, rtmp_b, bankb[0], qk_b[p], tab_b[n % 3])
        S.op("act", lambda e: e.activation(out=sg[p], in_=bk(3), func=AF.Silu), reads=[bankb[3]], writes=[sg_b[p]])
        make_ktil(p, dkf, k3, k3b)
        yield
        if tl == 1:
            if s + 2 < 17:
                load_seg(s + 2, s + 2)
            if s + 1 < 17:
                norm_seg(s + 1, s + 1)
        b4 = bkb16(4).rearrange("p (j n) -> p j n", j=8)
        for j in range(8):
            src_j = qk_tm[p][:, j * 128:(j + 1) * 128] if j < 4 else k3[:, (j - 4) * 128:(j - 3) * 128]
            S.op("pe", lambda e, j=j, src_j=src_j: e.transpose(out=b4[:, j, :], in_=src_j, identity=ctx["ident_bf"]),
                 reads=[qk_b[p], k3b, ctx["cbf_b"]], writes=[bankb[4]], signal=(j == 7))
        S.op("act", lambda e: e.copy(out=kT[p], in_=b4[:, 4:8, :]), reads=[bankb[4]], writes=[qkT_b[p]])
        S.op("act", lambda e: e.copy(out=qT[p], in_=b4[:, 0:4, :]), reads=[bankb[4]], writes=[qkT_b[p]])
        S.op("dve", lambda e: e.tensor_tensor(out=qfT[p], in0=b4[:, 0:4, :], in1=decqf, op=ALU.mult),
             reads=[bankb[4], dec_b], writes=[qkT_b[p]])
        S.op("dve", lambda e: e.tensor_tensor(out=qbT[p], in0=b4[:, 0:4, :], in1=decqb, op=ALU.mult),
             reads=[bankb[4], dec_b], writes=[qkT_b[p]])
        yield
        for g in range(4):
            lst = plan[g][t]
            for n_i, (ti, bi) in enumerate(lst):
                S.op("pe", lambda e, g=g, ti=ti, bi=bi, n_i=n_i, L=len(lst): e.matmul(
                    bk(0)[:, g * 128:(g + 1) * 128], lhsT=pslot[ti % 12][:, g * 128:(g + 1) * 128], rhs=blocks[:, bi, :],
                    start=(n_i == 0), stop=(n_i == L - 1)),
                    reads=[pslot_b[ti % 12], blocks_b], writes=[bankb[0]], signal=(g == 3 and n_i == len(lst) - 1))
        S.op("act", lambda e: e.copy(out=dT, in_=bk(0).rearrange("p (g n) -> p g n", g=4)), reads=[bankb[0]], writes=[dT_b])
        yield
        for g in range(4):
            S.op("pe", lambda e, g=g: e.matmul(bk(1)[:, g * 128:(g + 1) * 128], lhsT=poolw[:, g, :], rhs=dT[:, g, :], start=True, stop=True),
                 reads=[poolw_b, dT_b], writes=[bankb[1]], signal=(g == 3))
        S.op("dve", lambda e: e.tensor_tensor(out=mixT[p][:, 4:8, :], in0=bk(1).rearrange("p (g n) -> p g n", g=4),
                                              in1=rsc[:, 4:8].unsqueeze(2).broadcast_to([128, 4, 128]), op=ALU.mult),
             reads=[bankb[1], ctx["vecs_b"]], writes=[mixp_b[p]])

    def main_back(t):
        s, tl = t // 2, t % 2
        p = t % 2
        sidx = 1 if s == 0 else 0
        xi = s % 3
        for h in range(4):
            S.op("pe", lambda e, h=h: e.matmul(bk(5)[:, h * 128:(h + 1) * 128], lhsT=kT[p][:, h, :], rhs=qT[p][:, h, :], start=True, stop=True),
                 reads=[qkT_b[p]], writes=[bankb[5]], signal=(h == 3))
        S.op("dve", lambda e: e.tensor_tensor(out=PT, in0=bk(5).rearrange("p (h n) -> p h n", h=4), in1=Dcomb, op=ALU.mult),
             reads=[bankb[5], dec_b], writes=[PT_b])
        yield
        sbs = sbslot[t % 4]
        for h in range(4):
            o_h = bk(6)[:, h * 128:(h + 1) * 128]
            S.op("pe", lambda e, h=h, o_h=o_h: e.matmul(o_h, lhsT=PT[:, h, :], rhs=vt3[t % 3][:, h * 128:(h + 1) * 128], start=True, stop=False),
                 reads=[PT_b, vt3_b[t % 3]], writes=[bankb[6]], signal=False)
            S.op("pe", lambda e, h=h, o_h=o_h: e.matmul(o_h, lhsT=qfT[p][:, h, :], rhs=St_bf[:, h, :], start=False, stop=False),
                 reads=[qkT_b[p], Stbf_b], writes=[bankb[6]], signal=False)
            S.op("pe", lambda e, h=h, o_h=o_h: e.matmul(o_h, lhsT=qbT[p][:, h, :], rhs=sbs[:, h, :], start=False, stop=True),
                 reads=[qkT_b[p], sbslot_b[t % 4]], writes=[bankb[6]], signal=(h == 3))
        state_update(p, gcf, vt3[t % 3], vt3_b[t % 3])
        yield
        if last and s == 0:
            return
        o3 = bk(6).rearrange("p (h n) -> p h n", h=4)
        for h in range(4):
            S.op("dve", lambda e, h=h: e.bn_stats(out=stats[:, h, :], in_=o3[:, h, :]), reads=[bankb[6]], writes=[gn_b])
        for h in range(4):
            S.op("dve", lambda e, h=h: e.bn_aggr(out=mv[:, h, :], in_=stats[:, h, :]), reads=[gn_b], writes=[gn_b])
        S.op("dve", lambda e: e.tensor_scalar(out=rs4, in0=mv[:, :, 1], scalar1=GN_EPS, scalar2=None, op0=ALU.add),
             reads=[gn_b], writes=[gn_b])
        S.op("pool", lambda e: e.tensor_tensor(out=rs4, in0=rs4, in1=ctx["small"][:, 262:263].broadcast_to([128, 4]), op=ALU.pow),
             reads=[gn_b, ctx["small_b"]], writes=[gn_b])
        for h in range(4):
            S.op("dve", lambda e, h=h: e.tensor_scalar(out=on[:, h, :], in0=o3[:, h, :], scalar1=mv[:, h, 0:1], scalar2=rs4[:, h:h + 1],
                                                       op0=ALU.subtract, op1=ALU.mult),
                 reads=[bankb[6], gn_b], writes=[on_b])
        S.op("pool", lambda e: e.tensor_tensor(out=ret_tm, in0=on.rearrange("p h n -> p (h n)"), in1=sg[p], op=ALU.mult),
             reads=[on_b, sg_b[p]], writes=[ret_b])
        yield
        b7r = bkb16(7)[:, 0:512].rearrange("p (j n) -> p j n", j=4)
        for h in range(4):
            S.op("pe", lambda e, h=h: e.transpose(out=b7r[:, h, :], in_=ret_tm[:, h * 128:(h + 1) * 128], identity=ctx["ident_bf"]),
                 reads=[ret_b, ctx["cbf_b"]], writes=[bankb[7]], signal=(h == 3))
        S.op("dve", lambda e: e.tensor_tensor(out=mixT[p][:, 0:4, :], in0=b7r, in1=rsc[:, 0:4].unsqueeze(2).broadcast_to([128, 4, 128]), op=ALU.mult),
             reads=[bankb[7], ctx["vecs_b"]], writes=[mixr_b[p]])
        yield
        for n_ in range(8):
            for k in range(8):
                S.op("pe", lambda e, n_=n_, k=k: e.matmul(
                    bk(5 + n_ // 4)[:, (n_ % 4) * 128:(n_ % 4 + 1) * 128], lhsT=wout[:, k, n_ * 128:(n_ + 1) * 128], rhs=mixT[p][:, k, :],
                    start=(k == 0), stop=(k == 7)),
                    reads=[wout_b, mixr_b[p], mixp_b[p]], writes=[bankb[5 + n_ // 4]], signal=(k == 7 and n_ % 4 == 3))
        if not (last and s == 0):
            for n_ in range(8):
                S.op("dve", lambda e, n_=n_: e.scalar_tensor_tensor(
                    out=xnew[p][:, n_, :], in0=bk(5 + n_ // 4)[:, (n_ % 4) * 128:(n_ % 4 + 1) * 128], scalar=GT1[:, n_, sidx:sidx + 1],
                    in1=xq[xi][:, n_, tl * 128:(tl + 1) * 128], op0=ALU.mult, op1=ALU.add),
                    reads=[bankb[5 + n_ // 4], xq_b[xi], ctx["mod_b"]], writes=[xnew_b[p]])
            S.dma("pool", lambda e: e.dma_start(out=ctx["xres_v"][:, :, t * 128:(t + 1) * 128], in_=xnew[p]),
                  reads=[xnew_b[p]], writes=[ctx["xseg_b"][s]])

    load_seg(0, 0)
    load_seg(1, 1)
    norm_seg(0, 0)
    S.dma("sp", lambda e: e.dma_start(out=kt3[0], in_=ctx["kscr"][0]), reads=[ctx["kscr_b"][0]], writes=[kt3_b[0]])
    S.dma("sp", lambda e: e.dma_start(out=vt3[0], in_=ctx["vscr"][0]), reads=[ctx["vscr_b"][0]], writes=[vt3_b[0]])
    prefetch_sb(0)
    prefetch_sb(1)
    for t0 in (0, 1):
        prefetch_p(t0)
    interleave([main_front(0)])
    for t in range(NT):
        gens = [main_back(t)]
        if t + 1 < NT:
            gens.append(main_front(t + 1))
        interleave(gens)
    S.barrier()
    A.release()


FFN_SEG_BLOCKS = [list(range(0, 5)), list(range(5, 9)), list(range(9, 13)), list(range(13, 17))]


def emit_ffn(ctx, l):
    S, A, bk, bankb = ctx["S"], ctx["A"], ctx["bk"], ctx["bankb"]
    mod, vecs = ctx["mod"], ctx["vecs"]
    is_moe = (l % 2 == 1)
    li = l // 2
    last = (l == DEPTH - 1)
    S.barrier()
    A.mark()
    G2 = emit_layer_vectors(ctx, l, 2)
    S2 = mod[:, l, 24:32, :]
    GT2 = mod[:, l, 40:48, :]
    TBMAX = 1280
    h2T = A.bf16(8 * TBMAX).rearrange("p (k n) -> p k n", k=8)
    h2T_b = Buf("h2T")
    abuf = A.bf16(12 * TBMAX).rearrange("p (c n) -> p c n", c=12)
    a_b = Buf("a")
    acc = A.f32(8 * TBMAX).rearrange("p (k n) -> p k n", k=8)
    acc_b = Buf("acc")
    wst = [A.bf16(2 * 8 * 512).rearrange("p (u k n) -> p u k n", u=2, k=8) for _ in range(2)]
    wst_b = [Buf("wst%d" % i) for i in range(2)]
    w2sb = A.bf16(12 * 1024).rearrange("p (c n) -> p c n", c=12)
    w2_b = Buf("w2sb")
    xq = [A.f32(8 * 256).rearrange("p (k n) -> p k n", k=8) for _ in range(2)]
    xq_b = [Buf("fxq0"), Buf("fxq1")]
    sq = A.bf16(8 * 256).rearrange("p (k n) -> p k n", k=8)
    nb = (sq, Buf("fsq"), A.f32(256), Buf("frstd"), [A.f32(256), A.f32(256)], [Buf("ftmp0"), Buf("ftmp1")])
    sgb = [A.f32(512), A.f32(512)]
    sgb_b = [Buf("sgb0"), Buf("sgb1")]
    xnew = A.f32(8 * 256).rearrange("p (k n) -> p k n", k=8)
    xnew_b = Buf("fxnew")
    if is_moe:
        h2f = xnew
        h2f_b = xnew_b
        rw = A.f32(64).rearrange("p (k e) -> p k e", k=8)
        rw_b = Buf("rw")
        Gt = [A.f32(10 * 8).rearrange("p (t e) -> p t e", t=10) for _ in range(2)]
        Gt_b = [Buf("Gt0"), Buf("Gt1")]
        gsm = A.f32(48)
        gsm_b = Buf("gsm")
        dg = [A.f32(128), A.f32(128)]
        dg_b = [Buf("dg0"), Buf("dg1")]
        gbc = A.f32(512)
        gbc_b = Buf("gbc")
        tmpg = [A.f32(512), A.f32(512)]
        tmpg_b = [Buf("tmpg0"), Buf("tmpg1")]
        S.dma("sp", lambda e: e.dma_start(out=rw, in_=ctx["router_w"][li].rearrange("(k p) e -> p k e", p=128)), writes=[rw_b])
        items = [(e_, hf) for e_ in range(NEXP) for hf in range(2)]
    else:
        items = [(None, 0), (None, 1)]

    def w13_of(e_):
        return ctx["moe_w13"][li, e_] if is_moe else ctx["ffn_w13"][li]

    def w2_of(e_):
        return ctx["moe_w2"][li, e_] if is_moe else ctx["ffn_w2"][li]

    ugrot = [0]
    w2rot = [0]
    strot = [0]

    blocks_list = []
    for segs in FFN_SEG_BLOCKS:
        if last and segs[0] == 0:
            segs = segs[1:]
        blocks_list.append(segs)

    def interleave(gens):
        active = list(gens)
        while active:
            for g_ in list(active):
                try:
                    next(g_)
                except StopIteration:
                    active.remove(g_)

    def norm_block(bi):
        segs = blocks_list[bi]
        for n, s in enumerate(segs):
            i = n % 2
            sidx = 1 if s == 0 else 0
            S.dma("sp", lambda e, s=s, i=i: e.dma_start(out=xq[i], in_=ctx["xres_v"][:, :, s * 256:(s + 1) * 256]),
                  reads=[ctx["xseg_b"][s]], writes=[xq_b[i]])
            c0 = (s - segs[0]) * 256
            if not is_moe:
                emit_norm_seg(ctx, s, xq[i], xq_b[i], G2[:, :, sidx], S2[:, :, sidx], h2T[:, :, c0:c0 + 256], h2T_b, nb)
                yield
                continue
            Gtb = Gt[bi % 2]
            emit_norm_seg(ctx, s, xq[i], xq_b[i], G2[:, :, sidx], S2[:, :, sidx], h2T[:, :, c0:c0 + 256], h2T_b, nb,
                          h32_out=h2f, h32_b=h2f_b)
            yield
            for tl in range(2):
                tb = (s - segs[0]) * 2 + tl
                lgp = bk(7)[:, 256 + tl * 8:256 + tl * 8 + 8]
                for k in range(8):
                    S.op("pe", lambda e, k=k, tl=tl, lgp=lgp: e.matmul(lgp, lhsT=h2f[:, k, tl * 128:(tl + 1) * 128], rhs=rw[:, k, :],
                                                                       start=(k == 0), stop=(k == 7)),
                         reads=[h2f_b, rw_b], writes=[bankb[7]], signal=(k == 7))
                lg8, mx, dlt, w1, w2_, g1 = gsm[:, 0:8], gsm[:, 8:16], gsm[:, 16:17], gsm[:, 17:18], gsm[:, 18:19], gsm[:, 24:32]
                S.op("act", lambda e, lgp=lgp: e.copy(out=lg8, in_=lgp), reads=[bankb[7]], writes=[gsm_b])
                yield
                S.op("dve", lambda e: e.max(out=mx, in_=lg8), reads=[gsm_b], writes=[gsm_b])
                S.op("dve", lambda e: e.tensor_tensor(out=dlt, in0=mx[:, 1:2], in1=mx[:, 0:1], op=ALU.subtract), reads=[gsm_b], writes=[gsm_b])
                S.op("act", lambda e: e.activation(out=dlt, in_=dlt, func=AF.Exp), reads=[gsm_b], writes=[gsm_b])
                S.op("dve", lambda e: e.tensor_scalar(out=w1, in0=dlt, scalar1=1.0, scalar2=None, op0=ALU.add), reads=[gsm_b], writes=[gsm_b])
                S.op("dve", lambda e: e.reciprocal(out=w1, in_=w1), reads=[gsm_b], writes=[gsm_b])
                S.op("dve", lambda e: e.tensor_tensor(out=w2_, in0=dlt, in1=w1, op=ALU.mult), reads=[gsm_b], writes=[gsm_b])
                S.op("dve", lambda e: e.tensor_scalar(out=g1, in0=lg8, scalar1=mx[:, 0:1], scalar2=w1, op0=ALU.is_equal, op1=ALU.mult),
                     reads=[gsm_b], writes=[gsm_b])
                S.op("dve", lambda e, tb=tb, Gtb=Gtb: e.tensor_scalar(out=Gtb[:, tb, :], in0=lg8, scalar1=mx[:, 1:2], scalar2=w2_, op0=ALU.is_equal, op1=ALU.mult),
                     reads=[gsm_b], writes=[Gt_b[bi % 2]])
                S.op("dve", lambda e, tb=tb, Gtb=Gtb: e.tensor_tensor(out=Gtb[:, tb, :], in0=Gtb[:, tb, :], in1=g1, op=ALU.add),
                     reads=[gsm_b, Gt_b[bi % 2]], writes=[Gt_b[bi % 2]])
                yield

    def w2_phase(bi, it_i, e_, nfc, cgs):
        for (cc0, cn) in cgs:
            if is_moe:
                ntl = cn // 128
                for tl in range(ntl):
                    tb = cc0 // 128 + tl
                    di = tl % 2
                    S.op("dve", lambda e, tb=tb, di=di: e.tensor_scalar(out=dg[di], in0=ctx["ident_f"], scalar1=Gt[bi % 2][:, tb, e_:e_ + 1],
                                                                        scalar2=None, op0=ALU.mult),
                         reads=[Gt_b[bi % 2], ctx["misc_b"]], writes=[dg_b[di]])
                    S.op("pe", lambda e, tl=tl, di=di: e.matmul(bk(7)[:, tl * 128:(tl + 1) * 128], lhsT=ctx["ones_f"], rhs=dg[di], start=True, stop=True),
                         reads=[dg_b[di], ctx["misc_b"]], writes=[bankb[7]], signal=True)
                S.op("act", lambda e, cn=cn: e.copy(out=gbc[:, 0:cn], in_=bk(7)[:, 0:cn]), reads=[bankb[7]], writes=[gbc_b])
            for n_ in range(8):
                ob = 4 + (w2rot[0] % 3)
                w2rot[0] += 1
                for c in range(nfc):
                    S.op("pe", lambda e, ob=ob, c=c, n_=n_, cc0=cc0, cn=cn: e.matmul(
                        bk(ob)[:, 0:cn], lhsT=w2sb[:, c, n_ * 128:(n_ + 1) * 128], rhs=abuf[:, c, cc0:cc0 + cn],
                        start=(c == 0), stop=(c == nfc - 1)),
                        reads=[w2_b, a_b], writes=[bankb[ob]], signal=(c == nfc - 1))
                dst = acc[:, n_, cc0:cc0 + cn]
                if not is_moe:
                    if it_i == 0:
                        S.op("act", lambda e, ob=ob, cn=cn, dst=dst: e.copy(out=dst, in_=bk(ob)[:, 0:cn]), reads=[bankb[ob]], writes=[acc_b])
                    else:
                        S.op("dve", lambda e, ob=ob, cn=cn, dst=dst: e.tensor_tensor(out=dst, in0=dst, in1=bk(ob)[:, 0:cn], op=ALU.add),
                             reads=[bankb[ob], acc_b], writes=[acc_b])
                else:
                    if it_i == 0:
                        S.op("dve", lambda e, ob=ob, cn=cn, dst=dst: e.tensor_tensor(out=dst, in0=bk(ob)[:, 0:cn], in1=gbc[:, 0:cn], op=ALU.mult),
                             reads=[bankb[ob], gbc_b], writes=[acc_b])
                    else:
                        tg = tmpg[w2rot[0] % 2]
                        tg_b = tmpg_b[w2rot[0] % 2]
                        S.op("dve", lambda e, ob=ob, cn=cn, tg=tg: e.tensor_tensor(out=tg[:, 0:cn], in0=bk(ob)[:, 0:cn], in1=gbc[:, 0:cn], op=ALU.mult),
                             reads=[bankb[ob], gbc_b], writes=[tg_b])
                        S.op("pool", lambda e, cn=cn, dst=dst, tg=tg: e.tensor_tensor(out=dst, in0=dst, in1=tg[:, 0:cn], op=ALU.add),
                             reads=[tg_b, acc_b], writes=[acc_b])
                yield

    interleave([norm_block(0)])
    prefetched = [False]
    for bi, segs in enumerate(blocks_list):
        TB = 256 * len(segs)
        if segs[0] == 0:
            cgs = [(0, 256), (256, 512), (768, 512)]
        else:
            cgs = [(0, 512), (512, 512)]

        flat = []
        for it_i, (e_, hf) in enumerate(items):
            fc0, nfc = F_HALVES[hf]
            for c4 in range(0, nfc, 4):
                flat.append((it_i, e_, hf, c4, min(4, nfc - c4)))

        def w13_dma(gi):
            it_i, e_, hf, c4, ng = flat[gi]
            fc0, nfc = F_HALVES[hf]
            w13 = w13_of(e_)
            si = gi % 2
            col_u = (fc0 + c4) * 128
            col_g = DFF + (fc0 + c4) * 128
            for uu, col in enumerate((col_u, col_g)):
                S.dma("pool", lambda e, si=si, uu=uu, col=col, w13=w13, ng=ng: e.dma_start(
                    out=wst[si][:, uu, :, 0:ng * 128], in_=w13[:, col:col + ng * 128].rearrange("(k p) n -> p k n", p=128)),
                    writes=[wst_b[si]])

        def w2_dma(it_i):
            e_, hf = items[it_i]
            fc0, nfc = F_HALVES[hf]
            w2 = w2_of(e_)
            for c2 in range(0, nfc, 2):
                S.dma("pool", lambda e, c2=c2, fc0=fc0, w2=w2: e.dma_start(
                    out=w2sb[:, c2:c2 + 2, :], in_=w2[(fc0 + c2) * 128:(fc0 + c2 + 2) * 128, :].rearrange("(c p) n -> p c n", p=128)),
                    writes=[w2_b])

        if not prefetched[0]:
            w13_dma(0)
        w2_dma(0)
        if len(flat) > 1 and not prefetched[0]:
            w13_dma(1)
        prefetched[0] = False
        for gi, (it_i, e_, hf, c4, ng) in enumerate(flat):
            fc0, nfc = F_HALVES[hf]
            si = gi % 2
            for (cc0, cn) in cgs:
                for cl in range(ng):
                    pr = (ugrot[0] % 2) * 2
                    ugrot[0] += 1
                    for uu in range(2):
                        for k in range(8):
                            S.op("pe", lambda e, si=si, uu=uu, k=k, cl=cl, pr=pr, cc0=cc0, cn=cn: e.matmul(
                                bk(pr + uu)[:, 0:cn], lhsT=wst[si][:, uu, k, cl * 128:(cl + 1) * 128], rhs=h2T[:, k, cc0:cc0 + cn],
                                start=(k == 0), stop=(k == 7)),
                                reads=[wst_b[si], h2T_b], writes=[bankb[pr + uu]], signal=(k == 7))
                    sgi = (ugrot[0]) % 2
                    S.op("act", lambda e, pr=pr, cn=cn, sgi=sgi: e.activation(out=sgb[sgi][:, 0:cn], in_=bk(pr + 1)[:, 0:cn], func=AF.Silu),
                         reads=[bankb[pr + 1]], writes=[sgb_b[sgi]])
                    S.op("dve", lambda e, pr=pr, cn=cn, sgi=sgi, c4=c4, cl=cl, cc0=cc0: e.tensor_tensor(
                        out=abuf[:, c4 + cl, cc0:cc0 + cn], in0=bk(pr)[:, 0:cn], in1=sgb[sgi][:, 0:cn], op=ALU.mult),
                        reads=[bankb[pr], sgb_b[sgi]], writes=[a_b])
            if gi + 2 < len(flat):
                w13_dma(gi + 2)
            last_of_item = (gi + 1 == len(flat)) or (flat[gi + 1][0] != it_i)
            if not last_of_item:
                continue
            gens = [w2_phase(bi, it_i, e_, nfc, cgs)]
            if it_i + 1 == len(items) and bi + 1 < len(blocks_list):
                w13_dma(0)
                w13_dma(1)
                prefetched[0] = True
                gens.append(norm_block(bi + 1))
            interleave(gens)
            if it_i + 1 < len(items):
                w2_dma(it_i + 1)
        for n, s in enumerate(segs):
            if last and s == 0:
                continue
            i = n % 2
            sidx = 1 if s == 0 else 0
            c0 = (s - segs[0]) * 256
            S.dma("sp", lambda e, s=s, i=i: e.dma_start(out=xq[i], in_=ctx["xres_v"][:, :, s * 256:(s + 1) * 256]),
                  reads=[ctx["xseg_b"][s]], writes=[xq_b[i]])
            for n_ in range(8):
                S.op("dve", lambda e, n_=n_, i=i, c0=c0, sidx=sidx: e.scalar_tensor_tensor(
                    out=xnew[:, n_, :], in0=acc[:, n_, c0:c0 + 256], scalar=GT2[:, n_, sidx:sidx + 1], in1=xq[i][:, n_, :],
                    op0=ALU.mult, op1=ALU.add),
                    reads=[acc_b, xq_b[i], ctx["mod_b"]], writes=[xnew_b])
            S.dma("sp", lambda e, s=s: e.dma_start(out=ctx["xres_v"][:, :, s * 256:(s + 1) * 256], in_=xnew),
                  reads=[xnew_b], writes=[ctx["xseg_b"][s]])
    S.barrier()
    A.release()


def emit_final(ctx, gfin, outT_v, out_b, raw=False):
    S, A = ctx["S"], ctx["A"]
    S.barrier()
    A.mark()
    xq = [A.f32(8 * 256).rearrange("p (k n) -> p k n", k=8) for _ in range(2)]
    xq_b = [Buf("oxq0"), Buf("oxq1")]
    sq = A.bf16(8 * 256).rearrange("p (k n) -> p k n", k=8)
    nb = (sq, Buf("osq"), A.f32(256), Buf("orstd"), [A.f32(256), A.f32(256)], [Buf("otmp0"), Buf("otmp1")])
    yo = [A.f32(8 * 256).rearrange("p (k n) -> p k n", k=8) for _ in range(2)]
    yo_b = [Buf("yo0"), Buf("yo1")]
    for s in range(1, 17):
        i = s % 2
        S.dma("sp", lambda e, s=s, i=i: e.dma_start(out=xq[i], in_=ctx["xres_v"][:, :, s * 256:(s + 1) * 256]),
              reads=[ctx["xseg_b"][s]], writes=[xq_b[i]])
        if raw:
            S.dma("sp", lambda e, s=s, i=i: e.dma_start(out=outT_v[:, :, (s - 1) * 256:s * 256], in_=xq[i]),
                  reads=[xq_b[i]], writes=[out_b])
            continue
        emit_norm_seg(ctx, s, xq[i], xq_b[i], gfin, None, None, None, nb, h32_out=yo[i], h32_b=yo_b[i])
        S.dma("sp", lambda e, s=s, i=i: e.dma_start(out=outT_v[:, :, (s - 1) * 256:s * 256], in_=yo[i]),
              reads=[yo_b[i]], writes=[out_b])
    A.release()


def _pk(v):
    v = np.asarray(v, np.float32)
    return np.ascontiguousarray(v.reshape(-1, 128).T)


_PROG_CACHE = {}


def _prepare_inputs(x, c, ctx, c_ctx, w_ada, b_ada, norm1_g, norm2_g, w_in, ret_decay_logit, ret_gn_g,
                    pool_w, pool_scale, w_out, ffn_w13, ffn_w2, router_w, moe_w13, moe_w2, final_norm_g):
    hc = _host_consts()
    f = lambda a: np.ascontiguousarray(np.asarray(a, np.float32))
    vecs = np.zeros((128, NVEC), np.float32)
    for l in range(DEPTH):
        o = l * VEC_PER_LAYER
        vecs[:, o:o + 8] = _pk(norm1_g[l])
        vecs[:, o + 8:o + 16] = _pk(norm2_g[l])
        vecs[:, o + 16:o + 20] = _pk(ret_gn_g[l])
        vecs[:, o + 20:o + 24] = _pk(pool_scale[l])
        vecs[:, o + 24:o + 72] = _pk(b_ada[l])
    vecs[:, VEC_PER_LAYER * DEPTH:] = _pk(final_norm_g)
    shared = dict(vecs=vecs, dlog=f(ret_decay_logit).reshape(1, 32), w_ada=f(w_ada), w_in=f(w_in), w_out=f(w_out),
                  pool_w=f(pool_w), ffn_w13=f(ffn_w13), ffn_w2=f(ffn_w2), router_w=f(router_w), moe_w13=f(moe_w13),
                  moe_w2=f(moe_w2), blocks=hc["blocks"], cos=hc["cos"], sin=hc["sin"], misc=hc["misc"], small=hc["small"])
    x = np.asarray(x, np.float32)
    ctx = np.asarray(ctx, np.float32)
    c = np.asarray(c, np.float32)
    c_ctx = np.asarray(c_ctx, np.float32)
    in_maps = []
    for b in range(x.shape[0]):
        xT = np.ascontiguousarray(np.concatenate([ctx[b], x[b]], axis=0).T)
        cc = np.ascontiguousarray(np.concatenate([_pk(c[b]), _pk(c_ctx)], axis=1))
        m = dict(shared)
        m["xT"] = xT
        m["cc"] = cc
        in_maps.append(m)
    return in_maps


def kernel(x, c, ctx, c_ctx, w_ada, b_ada, norm1_g, norm2_g, w_in, ret_decay_logit, ret_gn_g,
           pool_w, pool_scale, w_out, ffn_w13, ffn_w2, router_w, moe_w13, moe_w2, final_norm_g):
    hc = _host_consts()
    in_maps = _prepare_inputs(x, c, ctx, c_ctx, w_ada, b_ada, norm1_g, norm2_g, w_in, ret_decay_logit, ret_gn_g,
                              pool_w, pool_scale, w_out, ffn_w13, ffn_w2, router_w, moe_w13, moe_w2, final_norm_g)
    if "nc" not in _PROG_CACHE:
        _PROG_CACHE["nc"] = build_program(plan=hc["plan"], nblk=hc["blocks"].shape[0])
    nc = _PROG_CACHE["nc"]
    res = run_bass_kernel_spmd(nc, in_maps, core_ids=list(range(len(in_maps))))
    out = np.stack([np.ascontiguousarray(np.asarray(r["outT"], np.float32).T) for r in res.results], axis=0)
    return out.astype(np.float32)
```
